# Optimizing a Trainium2 kernel written in Bass

```python
import jax
import jax.numpy as jnp
from jax import lax
import numpy as np

D_MODEL = 2048
BATCH = 4
SEQ = 4096
DEPTH = 2

MEM_LEN = 256
HEAD_DIM = 128
FOX_HEADS = 8
NSA_HEADS = 8
NSA_KV_HEADS = 2
NSA_GROUP = NSA_HEADS // NSA_KV_HEADS
MEM_HEADS = 4
MEM_HEAD_DIM = 256
FOX_W = FOX_HEADS * HEAD_DIM
NSA_W = NSA_HEADS * HEAD_DIM
NSA_KV_W = NSA_KV_HEADS * HEAD_DIM
MEM_W = MEM_HEADS * MEM_HEAD_DIM
N_BRANCHES = 3
CMP_LEN = 32
CMP_STRIDE = 16
SLC_BLOCK = 64
SLC_TOPK = 16
WINDOW = 512
Q_BLOCK = 128
SLC_Q_BLOCK = 64
ROPE_THETA = 500000.0
ROPE_DIM = HEAD_DIM // 4
D_FF = 4 * D_MODEL
RMS_EPS = 1e-6
NEG = -1e30
FORCED_SCORE = 1e6
IN_SPLITS = (FOX_W, FOX_W, FOX_W, FOX_HEADS, NSA_W, NSA_KV_W, NSA_KV_W, NSA_KV_W, NSA_KV_W, NSA_KV_W, NSA_KV_W, 3 * NSA_HEADS, MEM_W, N_BRANCHES * D_MODEL)
D_IN = 3 * FOX_W + FOX_HEADS + NSA_W + 6 * NSA_KV_W + 3 * NSA_HEADS + MEM_W + N_BRANCHES * D_MODEL

kernel_name = "hybrid_fox_nsa_memory_block"


def rms_norm(x, g):
    xf = x.astype(jnp.float32)
    y = xf * lax.rsqrt(jnp.mean(xf * xf, axis=-1, keepdims=True) + RMS_EPS)
    return (y * g.astype(jnp.float32)).astype(x.dtype)


def to_heads(x, n_heads, head_dim):
    b, t, _ = x.shape
    return x.reshape(b, t, n_heads, head_dim).transpose(0, 2, 1, 3)


def from_heads(x):
    b, h, t, d = x.shape
    return x.transpose(0, 2, 1, 3).reshape(b, t, h * d)


def rope_partial(x, pos):
    half = ROPE_DIM // 2
    inv_freq = ROPE_THETA ** (-jnp.arange(half, dtype=jnp.float32) / half)
    ang = pos.astype(jnp.float32)[:, None] * inv_freq[None, :]
    cos, sin = jnp.cos(ang), jnp.sin(ang)
    xr = x[..., :ROPE_DIM].astype(jnp.float32)
    x1, x2 = xr[..., :half], xr[..., half:]
    rot = jnp.concatenate([x1 * cos - x2 * sin, x1 * sin + x2 * cos], axis=-1)
    return jnp.concatenate([rot.astype(x.dtype), x[..., ROPE_DIM:]], axis=-1)


def fox_attention(q, k, v, log_f):
    b, h, t, d = q.shape
    c = jnp.cumsum(log_f, axis=-1)
    kpos = jnp.arange(t)
    scale = d ** -0.5

    def block(i):
        s0 = i * Q_BLOCK
        qb = lax.dynamic_slice_in_dim(q, s0, Q_BLOCK, axis=2)
        cb = lax.dynamic_slice_in_dim(c, s0, Q_BLOCK, axis=2)
        qpos = s0 + jnp.arange(Q_BLOCK)
        logits = jnp.einsum('bhqd,bhkd->bhqk', qb, k, preferred_element_type=jnp.float32) * scale
        logits = logits + (cb[..., :, None] - c[..., None, :])
        logits = jnp.where(kpos[None, :] <= qpos[:, None], logits, -jnp.inf)
        p = jax.nn.softmax(logits, axis=-1)
        return jnp.einsum('bhqk,bhkd->bhqd', p.astype(v.dtype), v)

    out = lax.map(block, jnp.arange(t // Q_BLOCK))
    return jnp.moveaxis(out, 0, 2).reshape(b, h, t, d)


def compress(x, w1, w2, pe):
    b, g, t, d = x.shape
    nc = (t - CMP_LEN) // CMP_STRIDE + 1
    idx = np.arange(nc)[:, None] * CMP_STRIDE + np.arange(CMP_LEN)[None, :]
    blocks = x[:, :, idx, :] + pe
    hdn = jax.nn.gelu(blocks.reshape(b, g, nc, CMP_LEN * d) @ w1)
    return hdn @ w2


def nsa_attention(q, kc, vc, ks, vs, kw, vw, gate_logits, pos):
    b, hq, t, d = q.shape
    g = NSA_KV_HEADS
    scale = d ** -0.5
    qg = q.reshape(b, g, NSA_GROUP, t, d)

    nc = kc.shape[2]
    cmp_end = np.arange(nc) * CMP_STRIDE + CMP_LEN - 1
    cmp_valid = jnp.asarray(cmp_end)[None, :] <= pos[:, None]
    logits = jnp.einsum('bgqtd,bgnd->bgqtn', qg, kc, preferred_element_type=jnp.float32) * scale
    logits = jnp.where(cmp_valid, logits, NEG)
    p_cmp = jnp.where(cmp_valid, jax.nn.softmax(logits, axis=-1), 0.0)
    o_cmp = jnp.einsum('bgqtn,bgnd->bgqtd', p_cmp.astype(vc.dtype), vc).reshape(b, hq, t, d)

    nsel = t // SLC_BLOCK
    k_top = min(SLC_TOPK, nsel)
    ci = np.arange(nc)[:, None] * CMP_STRIDE
    sj = np.arange(nsel)[None, :] * SLC_BLOCK
    overlap = jnp.asarray(((ci <= sj + SLC_BLOCK - 1) & (ci + CMP_LEN - 1 >= sj)).astype(np.float32))
    p_slc = jnp.einsum('bgtn,ns->bgts', p_cmp.sum(axis=2), overlap)
    blk = jnp.arange(nsel)[None, :]
    cur = (pos // SLC_BLOCK)[:, None]
    sel_valid = blk <= cur
    forced = (blk == 0) | (blk == cur) | (blk == cur - 1)
    score = jnp.where(forced, FORCED_SCORE, jnp.where(sel_valid, p_slc, -1.0))
    _, sel_idx = lax.top_k(score, k_top)

    kb = ks.reshape(b, g, nsel, SLC_BLOCK, d)
    vb = vs.reshape(b, g, nsel, SLC_BLOCK, d)
    gather = jax.vmap(jax.vmap(lambda blocks, ix: blocks[ix]))
    n_tok = k_top * SLC_BLOCK

    def slc_block(i):
        s0 = i * SLC_Q_BLOCK
        qc = lax.dynamic_slice_in_dim(qg, s0, SLC_Q_BLOCK, axis=3)
        ic = lax.dynamic_slice_in_dim(sel_idx, s0, SLC_Q_BLOCK, axis=2)
        kg = gather(kb, ic).reshape(b, g, SLC_Q_BLOCK, n_tok, d)
        vg = gather(vb, ic).reshape(b, g, SLC_Q_BLOCK, n_tok, d)
        tok = (ic[..., None] * SLC_BLOCK + jnp.arange(SLC_BLOCK)).reshape(b, g, SLC_Q_BLOCK, n_tok)
        qpos = s0 + jnp.arange(SLC_Q_BLOCK)
        mask = (tok <= qpos[:, None])[:, :, None]
        lg = jnp.einsum('bgqtd,bgtnd->bgqtn', qc, kg, preferred_element_type=jnp.float32) * scale
        p = jax.nn.softmax(jnp.where(mask, lg, -jnp.inf), axis=-1)
        return jnp.einsum('bgqtn,bgtnd->bgqtd', p.astype(vg.dtype), vg)

    o_slc = jnp.moveaxis(lax.map(slc_block, jnp.arange(t // SLC_Q_BLOCK)), 0, 3).reshape(b, hq, t, d)

    kp = jnp.pad(kw, ((0, 0), (0, 0), (WINDOW, 0), (0, 0)))
    vp = jnp.pad(vw, ((0, 0), (0, 0), (WINDOW, 0), (0, 0)))
    span = WINDOW + Q_BLOCK

    def win_block(i):
        s0 = i * Q_BLOCK
        qb = lax.dynamic_slice_in_dim(qg, s0, Q_BLOCK, axis=3)
        kbk = lax.dynamic_slice_in_dim(kp, s0, span, axis=2)
        vbk = lax.dynamic_slice_in_dim(vp, s0, span, axis=2)
        qpos = s0 + jnp.arange(Q_BLOCK)
        kpos = s0 - WINDOW + jnp.arange(span)
        rel = qpos[:, None] - kpos[None, :]
        mask = (rel >= 0) & (rel < WINDOW) & (kpos[None, :] >= 0)
        lg = jnp.einsum('bgqtd,bgkd->bgqtk', qb, kbk, preferred_element_type=jnp.float32) * scale
        p = jax.nn.softmax(jnp.where(mask, lg, -jnp.inf), axis=-1)
        return jnp.einsum('bgqtk,bgkd->bgqtd', p.astype(vbk.dtype), vbk)

    o_win = jnp.moveaxis(lax.map(win_block, jnp.arange(t // Q_BLOCK)), 0, 3).reshape(b, hq, t, d)

    gates = jax.nn.sigmoid(gate_logits).reshape(b, t, 3, hq).transpose(2, 0, 3, 1)[..., None]
    return gates[0] * o_cmp + gates[1] * o_slc + gates[2] * o_win


def memory_attention(q, mk, mv):
    scale = q.shape[-1] ** -0.5
    lg = jnp.einsum('bhtd,bhmd->bhtm', q, mk, preferred_element_type=jnp.float32) * scale
    p = jax.nn.softmax(lg, axis=-1)
    return jnp.einsum('bhtm,bhmd->bhtd', p.astype(mv.dtype), mv)


def token_mixer(h, mem, w_in, b_f, cmp_k_params, cmp_v_params, w_mem_kv, g_mem,
                w_up_fox, w_up_nsa, w_up_mem, w_o, g_pre, g_post):
    b, t, _ = h.shape
    pos = jnp.arange(t, dtype=jnp.int32)
    u = rms_norm(h, g_pre)
    proj = u @ w_in
    (fq, fk, fv, ff, nq, ck, cv, sk, sv, wk, wv, ng, mq, bg) = jnp.split(
        proj, np.cumsum(IN_SPLITS)[:-1].tolist(), axis=-1)

    log_f = jax.nn.log_sigmoid(ff.astype(jnp.float32) + b_f.astype(jnp.float32)).transpose(0, 2, 1)
    o_fox = fox_attention(to_heads(fq, FOX_HEADS, HEAD_DIM), to_heads(fk, FOX_HEADS, HEAD_DIM),
                          to_heads(fv, FOX_HEADS, HEAD_DIM), log_f)

    def kvh(z):
        return to_heads(z, NSA_KV_HEADS, HEAD_DIM)
    q_nsa = rope_partial(to_heads(nq, NSA_HEADS, HEAD_DIM), pos)
    kc = compress(rope_partial(kvh(ck), pos), *cmp_k_params)
    vc = compress(kvh(cv), *cmp_v_params)
    o_nsa = nsa_attention(q_nsa, kc, vc, rope_partial(kvh(sk), pos), kvh(sv),
                          rope_partial(kvh(wk), pos), kvh(wv), ng, pos)

    mkv = rms_norm(mem, g_mem) @ w_mem_kv
    mk, mv = jnp.split(mkv, 2, axis=-1)
    o_mem = memory_attention(to_heads(mq, MEM_HEADS, MEM_HEAD_DIM), to_heads(mk, MEM_HEADS, MEM_HEAD_DIM),
                             to_heads(mv, MEM_HEADS, MEM_HEAD_DIM))

    gate = jax.nn.sigmoid(bg).reshape(b, t, N_BRANCHES, D_MODEL)
    merged = (gate[:, :, 0] * (from_heads(o_fox) @ w_up_fox)
              + gate[:, :, 1] * (from_heads(o_nsa) @ w_up_nsa)
              + gate[:, :, 2] * (from_heads(o_mem) @ w_up_mem))
    return rms_norm(merged @ w_o, g_post)


def channel_mixer(h, g_pre, w1, w2, g_post):
    u = rms_norm(h, g_pre)
    a = jnp.square(jax.nn.relu(u @ w1))
    return rms_norm(a @ w2, g_post)


def setup_inputs(seed: int = 0) -> dict:
    key = jax.random.key(seed)
    ks = jax.random.split(key, 24)
    L = DEPTH

    def nrm(k, shape, fan_in):
        return jax.random.normal(k, shape, jnp.float32) * (fan_in ** -0.5)

    def gain(k, shape):
        return 1.0 + 0.05 * jax.random.normal(k, shape, jnp.float32)

    return {
        "x": jax.random.normal(ks[0], (BATCH, SEQ, D_MODEL), jnp.float32),
        "mem": jax.random.normal(ks[1], (BATCH, MEM_LEN, D_MODEL), jnp.float32),
        "w_in": nrm(ks[2], (L, D_MODEL, D_IN), D_MODEL),
        "b_f": jax.random.uniform(ks[3], (L, FOX_HEADS), jnp.float32, 1.0, 6.0),
        "w_cmp1_k": nrm(ks[4], (L, CMP_LEN * HEAD_DIM, HEAD_DIM), CMP_LEN * HEAD_DIM),
        "w_cmp2_k": nrm(ks[5], (L, HEAD_DIM, HEAD_DIM), HEAD_DIM),
        "pe_cmp_k": 0.1 * jax.random.normal(ks[6], (L, CMP_LEN, HEAD_DIM), jnp.float32),
        "w_cmp1_v": nrm(ks[7], (L, CMP_LEN * HEAD_DIM, HEAD_DIM), CMP_LEN * HEAD_DIM),
        "w_cmp2_v": nrm(ks[8], (L, HEAD_DIM, HEAD_DIM), HEAD_DIM),
        "pe_cmp_v": 0.1 * jax.random.normal(ks[9], (L, CMP_LEN, HEAD_DIM), jnp.float32),
        "w_mem_kv": nrm(ks[10], (L, D_MODEL, 2 * MEM_W), D_MODEL),
        "g_mem": gain(ks[11], (L, D_MODEL)),
        "w_up_fox": nrm(ks[12], (L, FOX_W, D_MODEL), FOX_W),
        "w_up_nsa": nrm(ks[13], (L, NSA_W, D_MODEL), NSA_W),
        "w_up_mem": nrm(ks[14], (L, MEM_W, D_MODEL), MEM_W),
        "w_o": nrm(ks[15], (L, D_MODEL, D_MODEL), D_MODEL),
        "g_pre_mix": gain(ks[16], (L, D_MODEL)),
        "g_post_mix": gain(ks[17], (L, D_MODEL)),
        "g_pre_mlp": gain(ks[18], (L, D_MODEL)),
        "g_post_mlp": gain(ks[19], (L, D_MODEL)),
        "w_mlp1": nrm(ks[20], (L, D_MODEL, D_FF), D_MODEL),
        "w_mlp2": nrm(ks[21], (L, D_FF, D_MODEL), D_FF),
    }


def reference(x, mem, w_in, b_f, w_cmp1_k, w_cmp2_k, pe_cmp_k, w_cmp1_v, w_cmp2_v, pe_cmp_v,
              w_mem_kv, g_mem, w_up_fox, w_up_nsa, w_up_mem, w_o, g_pre_mix, g_post_mix,
              g_pre_mlp, g_post_mlp, w_mlp1, w_mlp2):
    h = x
    for l in range(DEPTH):
        h = h + token_mixer(h, mem, w_in[l], b_f[l],
                            (w_cmp1_k[l], w_cmp2_k[l], pe_cmp_k[l]),
                            (w_cmp1_v[l], w_cmp2_v[l], pe_cmp_v[l]),
                            w_mem_kv[l], g_mem[l], w_up_fox[l], w_up_nsa[l], w_up_mem[l], w_o[l],
                            g_pre_mix[l], g_post_mix[l])
        h = h + channel_mixer(h, g_pre_mlp[l], w_mlp1[l], w_mlp2[l], g_post_mlp[l])
    return h
```

```python
import contextlib
import numpy as np
import ml_dtypes
import concourse.bass as bass
import concourse.mybir as mybir

F32 = mybir.dt.float32
BF16 = mybir.dt.bfloat16
AF = mybir.ActivationFunctionType
ALU = mybir.AluOpType
NPBF = ml_dtypes.bfloat16

ENGS = ["pe", "act", "dve", "pool", "sp"]
EPOCH = 20000
NDSEM = {"sp": 28, "pool": 20, "act": 8}


class Op:
    __slots__ = ("eng", "fn", "deps", "dma", "sig", "sigord", "dsem", "dval", "prev")

    def __init__(self, eng, fn, deps, dma):
        self.eng = eng
        self.fn = fn
        self.deps = deps
        self.dma = dma
        self.sig = False
        self.sigord = 0
        self.dsem = None
        self.dval = 0
        self.prev = None


class Prog:
    def __init__(self, nc):
        self.nc = nc
        self.ops = {e: [] for e in ENGS}
        self.last_w = {}
        self.rd_c = {}
        self.rd_d = {}
        self.stack = contextlib.ExitStack()
        self.dma_since = []

    def op(self, eng, fn, reads=(), writes=(), dma=False):
        ops = self.ops[eng]
        idx = len(ops)
        deps = set()
        for k in reads:
            w = self.last_w.get(k)
            if w is not None:
                deps.add(w)
        for k in writes:
            w = self.last_w.get(k)
            if w is not None:
                deps.add(w)
            rc = self.rd_c.get(k)
            if rc:
                for e, i in rc.items():
                    deps.add((e, i))
            rd = self.rd_d.get(k)
            if rd:
                deps.update(rd)
        ops.append(Op(eng, fn, deps, dma))
        me = (eng, idx)
        if dma:
            self.dma_since.append(me)
        for k in reads:
            if dma:
                self.rd_d.setdefault(k, []).append(me)
            else:
                self.rd_c.setdefault(k, {})[eng] = idx
        for k in writes:
            self.last_w[k] = me
            self.rd_c[k] = {}
            self.rd_d[k] = []
        return me

    def dma(self, eng, out, in_, reads=(), writes=()):
        return self.op(eng, lambda e: e.dma_start(out=out, in_=in_), reads, writes, dma=True)

    def barrier(self):
        deps = set(self.dma_since)
        for e in ENGS:
            for i in range(len(self.ops[e]) - 1, -1, -1):
                if not self.ops[e][i].dma:
                    deps.add((e, i))
                    break
        for e in ENGS:
            self.ops[e].append(Op(e, lambda eng: eng.nop(), set(deps), False))
        self.dma_since = []

    def emit(self, final_waits=()):
        nc = self.nc
        ops = self.ops
        for e in ENGS:
            for o in ops[e]:
                for (de, di) in o.deps:
                    d = ops[de][di]
                    if not d.dma:
                        if de == "pe" and e == "pe":
                            continue
                        d.sig = True
        for (de, di) in final_waits:
            if not ops[de][di].dma:
                ops[de][di].sig = True
        nsig = {}
        for e in ENGS:
            c = 0
            for o in ops[e]:
                if o.dma:
                    continue
                if o.sig:
                    c += 1
                    o.sigord = c
            nsig[e] = c
        st = self.stack
        csem = {}
        for e in ENGS:
            n = (nsig[e] + EPOCH - 1) // EPOCH
            csem[e] = [st.enter_context(nc.semaphore(f"c_{e}_{i}")) for i in range(max(n, 1))]
        dsem = {}
        for e in ("sp", "pool", "act"):
            dsem[e] = [st.enter_context(nc.semaphore(f"d_{e}_{i}")) for i in range(NDSEM[e])]
        for e in ("sp", "pool", "act"):
            cnt = [0] * NDSEM[e]
            k = 0
            for o in ops[e]:
                if o.dma:
                    s = k % NDSEM[e]
                    k += 1
                    cnt[s] += 1
                    o.dsem = dsem[e][s]
                    o.dval = 16 * cnt[s]
        for e in ("pe", "dve"):
            for o in ops[e]:
                assert not o.dma

        def target(de, di):
            d = ops[de][di]
            if d.dma:
                return d.dsem, d.dval
            so = d.sigord
            return csem[de][(so - 1) // EPOCH], (so - 1) % EPOCH + 1

        engobj = {"pe": "tensor", "act": "scalar", "dve": "vector", "pool": "gpsimd", "sp": "sync"}
        stats = {}
        with nc.Block() as block:
            def body(ename):
                def run(eng):
                    seen = {}
                    nw = 0
                    for idx, o in enumerate(ops[ename]):
                        waits = []
                        for (de, di) in o.deps:
                            if de == "pe" and ename == "pe":
                                continue
                            waits.append(target(de, di))
                        if o.dma and o.dval > 16:
                            waits.append((o.dsem, o.dval - 16))
                        for (s, v) in waits:
                            key = id(s)
                            if seen.get(key, 0) >= v:
                                continue
                            seen[key] = v
                            eng.wait_ge(s, v)
                            nw += 1
                        ins = o.fn(eng)
                        if o.dma:
                            ins.then_inc(o.dsem, 16)
                        elif o.sig:
                            so = o.sigord
                            ins.then_inc(csem[ename][(so - 1) // EPOCH], 1)
                    if ename == "sp":
                        for (de, di) in final_waits:
                            s, v = target(de, di)
                            eng.wait_ge(s, v)
                    stats[ename] = (len(ops[ename]), nw)
                return run
            block.tensor(body("pe"))
            block.scalar(body("act"))
            block.vector(body("dve"))
            block.gpsimd(body("pool"))
            block.sync(body("sp"))
        return stats


import math

D = 2048
T = 4096
NCH = 16
DIN = 12832
NL = 2
BIG = 30000.0
EPS = 1e-6
TS = 512
NT = T // TS
NB = T // 128

SEG = {}
_o = 0
for _n, _w in [("fq", 1024), ("fk", 1024), ("fv", 1024), ("ff", 8), ("nq", 1024), ("ck", 256), ("cv", 256),
               ("sk", 256), ("sv", 256), ("wk", 256), ("wv", 256), ("ng", 24), ("mq", 1024), ("bg", 6144)]:
    SEG[_n] = (_o, _w)
    _o += _w
assert _o == DIN

VG_PRE_MIX, VG_POST_MIX, VG_PRE_MLP, VG_POST_MLP, VG_MEM, V_NEGBF = 0, 16, 32, 48, 64, 80
VPL = 81


def host_consts():
    c = {}
    half = 16
    inv = (500000.0 ** (-np.arange(half, dtype=np.float32) / half)).astype(np.float32)
    ang = np.arange(T, dtype=np.float32)[:, None] * inv[None, :]
    cos, sin = np.cos(ang).astype(np.float32), np.sin(ang).astype(np.float32)
    cs = np.zeros((2, 32, T), np.float32)
    cs[0, :16] = cos.T; cs[0, 16:] = cos.T
    cs[1, :16] = sin.T; cs[1, 16:] = sin.T
    c["ropecs"] = cs
    pm = np.zeros((32, 32), np.float32)
    for m in range(16):
        pm[m + 16, m] = -1.0
        pm[m, m + 16] = 1.0
    c["pm"] = pm.astype(NPBF)
    c["ident"] = np.eye(128, dtype=np.float32).astype(NPBF)
    c["ones"] = np.ones((128, 128), np.float32).astype(NPBF)
    k = np.arange(128)[:, None]
    q = np.arange(512)[None, :]
    cz = np.zeros((128, 4, 512), np.float32)
    for r in range(4):
        cz[:, r, :] = np.where(r * 128 + k <= q, 0.0, -BIG)
    c["causal"] = cz.astype(NPBF)
    wz = np.zeros((128, 8, 512), np.float32)
    for i in range(8):
        rel = q - ((i - 4) * 128 + k)
        wz[:, i, :] = np.where((rel >= 0) & (rel < 512), 0.0, -BIG)
    c["winmask"] = wz.astype(NPBF)
    n = np.arange(256)
    tt = np.arange(T)
    valid = (n[:, None] * 16 + 31 <= tt[None, :]) & (n[:, None] < 255)
    cm = np.where(valid, 0.0, -BIG).astype(np.float32)
    cm = cm.reshape(2, 128, NT, 512).transpose(1, 2, 0, 3)
    c["cmpmask"] = np.ascontiguousarray(cm).astype(NPBF)
    ci = n[:, None] * 16
    sj = np.arange(64)[None, :] * 64
    ov = ((ci <= sj + 63) & (ci + 31 >= sj) & (n[:, None] < 255)).astype(np.float32)
    ovo = np.concatenate([ov, np.ones((256, 1), np.float32)], axis=1)
    ovo[255, :] = 0.0
    c["ov"] = np.ascontiguousarray(ovo.reshape(2, 128, 65).transpose(1, 0, 2)).astype(NPBF)
    a = np.zeros((64, 32, 128), np.float32)
    for kb in range(32):
        a[2 * kb, kb, 0:64] = 1.0
        a[2 * kb + 1, kb, 64:128] = 1.0
    c["asel"] = a.astype(NPBF)
    blk = np.arange(64)[None, :]
    cur = (tt // 64)[:, None]
    sel_valid = blk <= cur
    forced = (blk == 0) | (blk == cur) | (blk == cur - 1)
    vt = (sel_valid & ~forced).astype(np.float32)
    ct = np.where(forced, 1e6, np.where(sel_valid, 0.0, -1.0)).astype(np.float32)
    def tm(x):
        return np.ascontiguousarray(x.reshape(32, 128, 64).transpose(1, 0, 2)).astype(np.float32)
    c["seltab"] = np.stack([tm(vt), tm(ct), tm(sel_valid.astype(np.float32))], axis=1)
    sg = np.zeros((24, 24, 128), np.float32)
    for r in range(24):
        sg[r, r, :] = 1.0
    c["selg"] = sg.astype(NPBF)
    return c


def host_vecs(inp):
    v = np.zeros((128, NL * VPL), np.float32)
    for l in range(NL):
        b = l * VPL
        for nm, off in [("g_pre_mix", VG_PRE_MIX), ("g_post_mix", VG_POST_MIX), ("g_pre_mlp", VG_PRE_MLP),
                        ("g_post_mlp", VG_POST_MLP), ("g_mem", VG_MEM)]:
            v[:, b + off:b + off + 16] = np.asarray(inp[nm][l], np.float32).reshape(16, 128).T
        v[:8, b + V_NEGBF] = np.asarray(inp["b_f"][l], np.float32)
    return v


CONST_SPECS = {
    "ropecs": ([2, 32, T], F32), "pm": ([32, 32], BF16), "ident": ([128, 128], BF16), "ones": ([128, 128], BF16),
    "causal": ([128, 4, 512], BF16), "winmask": ([128, 8, 512], BF16), "cmpmask": ([128, NT, 2, 512], BF16),
    "ov": ([128, 2, 65], BF16), "asel": ([64, 32, 128], BF16), "seltab": ([128, 3, 32, 64], F32),
    "selg": ([24, 24, 128], BF16), "vecs": ([128, NL * VPL], F32),
}
WEIGHT_SPECS = {
    "w_in": [NL, D, DIN], "w_cmp1_k": [NL, 4096, 128], "w_cmp2_k": [NL, 128, 128], "pe_cmp_k": [NL, 32, 128],
    "w_cmp1_v": [NL, 4096, 128], "w_cmp2_v": [NL, 128, 128], "pe_cmp_v": [NL, 32, 128],
    "w_mem_kv": [NL, D, 2048], "w_up_fox": [NL, 1024, D], "w_up_nsa": [NL, 1024, D], "w_up_mem": [NL, 1024, D],
    "w_o": [NL, D, D], "w_mlp1": [NL, D, 8192], "w_mlp2": [NL, 8192, D],
}
SCRATCH = {
    "hT": ([NCH, 128, T], F32),
    "qfT": ([8, 128, T], BF16), "kfT": ([8, 128, T], BF16), "vf": ([T, 1024], BF16), "logf": ([8, T], F32),
    "qnT": ([8, 128, T], BF16), "ckT": ([2, 128, T], BF16), "cvT": ([2, 128, T], BF16),
    "skT": ([2, 128, T], BF16), "sv": ([T, 256], BF16), "wkT": ([2, 128, T], BF16), "wv": ([T, 256], BF16),
    "gnT": ([24, T], BF16), "mqT": ([8, 128, T], BF16), "bgT": ([48, 128, T], BF16),
    "oT": ([24, 128, T], BF16), "cs3": ([8, 6, T], BF16),
    "wcu": ([12, 128, 8, 512], BF16), "wcb": ([36, 128, 16, 512], BF16),
}


DBG = {"d_acc": ([3, 128, 512], F32), "d_pslc": ([3, 128, 4, 64], F32), "d_gn": ([3, 24, 512], BF16),
       "d_cmk": ([3, 128, 2, 512], BF16), "d_q4": ([3, 128, 4, 512], BF16), "d_stb": ([3, 128, 3, 4, 64], F32),
       "d_selT": ([64, T], BF16)}


class Builder:
    def __init__(self, ext_in=(), ext_out=(), dbg=False):
        self.nc = nc = bass.Bass("TRN2", target_bir_lowering=False)
        self.P = Prog(nc)
        self.dr = {}
        self.dr["xT"] = nc.dram_tensor("xT", [NCH, 128, T], F32, kind="ExternalInput").ap()
        self.dr["memT"] = nc.dram_tensor("memT", [NCH, 128, 256], F32, kind="ExternalInput").ap()
        for k, (shp, dt) in CONST_SPECS.items():
            self.dr[k] = nc.dram_tensor(k, shp, dt, kind="ExternalInput").ap()
        for k, shp in WEIGHT_SPECS.items():
            self.dr[k] = nc.dram_tensor(k, shp, F32, kind="ExternalInput").ap()
        for k, (shp, dt) in SCRATCH.items():
            kind = "Internal"
            if k in ext_in:
                kind = "ExternalInput"
            elif k in ext_out:
                kind = "ExternalOutput"
            self.dr[k] = nc.dram_tensor(k, shp, dt, kind=kind).ap()
        self.dr["outT"] = nc.dram_tensor("outT", [NCH, 128, T], F32, kind="ExternalOutput").ap()
        self.dbg = dbg
        if dbg:
            for k, (shp, dt) in DBG.items():
                self.dr[k] = nc.dram_tensor(k, shp, dt, kind="ExternalOutput").ap()
        self.st = self.P.stack
        self.final = []
        self.sb = {}
        for k in ("pm", "ident", "ones", "vecs", "selg"):
            shp, dt = CONST_SPECS[k]
            self.sb[k] = self.st.enter_context(nc.sbuf_tensor("c_" + k, shp, dt))
            src = self.dr[k]
            self.P.dma("sp", self.sb[k][:], src[:], writes=[("c", k)])
        self.ps = [self.st.enter_context(nc.psum_tensor(f"ps{i}", [128, 512], F32)) for i in range(7)]
        self.psb = self.st.enter_context(nc.psum_tensor("psb", [128, 1024], BF16))
        self.psi = 0
        self._uid = 0

    def uid(self):
        self._uid += 1
        return self._uid

    def sbuf(self, stack, name, shape, dt):
        return stack.enter_context(self.nc.sbuf_tensor(f"{name}_{self.uid()}", shape, dt))

    def cast_begin(self, l, stage):
        dr = self.dr
        jobs = []
        for br, wn in enumerate(("w_up_fox", "w_up_nsa", "w_up_mem")):
            for cg in range(4):
                jobs.append((dr[wn][l, :, cg * 512:(cg + 1) * 512].rearrange("(c p) n -> p c n", p=128), "wcu", br * 4 + cg, 8))
        for cg in range(4):
            jobs.append((dr["w_o"][l, :, cg * 512:(cg + 1) * 512].rearrange("(c p) n -> p c n", p=128), "wcb", cg, NCH))
        for i in range(16):
            jobs.append((dr["w_mlp1"][l, :, i * 512:(i + 1) * 512].rearrange("(c p) n -> p c n", p=128), "wcb", 4 + i, NCH))
        for g in range(4):
            for cg in range(4):
                jobs.append((dr["w_mlp2"][l, g * 2048:(g + 1) * 2048, cg * 512:(cg + 1) * 512].rearrange("(c p) n -> p c n", p=128),
                             "wcb", 20 + g * 4 + cg, NCH))
        self.cjobs = jobs
        self.cstage = stage
        self.cpos = 0

    def cast_step(self):
        P = self.P
        i = self.cpos
        n = len(self.cjobs)
        if i > n:
            return
        if i < n:
            src, dn, idx, nk = self.cjobs[i]
            sb_ = self.cstage[i % 2]
            P.dma("pool", sb_[:, 0:nk, :], src, writes=[("cstage", i % 2)])
        if i >= 1:
            src, dn, idx, nk = self.cjobs[i - 1]
            sb_ = self.cstage[(i - 1) % 2]
            P.dma("pool", self.dr[dn][idx], sb_[:, 0:nk, :], reads=[("cstage", (i - 1) % 2)], writes=[("wc", dn, idx)])
        self.cpos += 1

    def nextps(self, lo=0, hi=7):
        i = lo + (self.psi % (hi - lo))
        self.psi += 1
        return i

    def rmsnorm_stats(self, src, srckey, sq, rstd, tmp, nfree=512):
        P = self.P
        pi = self.nextps()
        ps = self.ps[pi]
        P.op("act", lambda e: e.activation(out=sq[:, :, 0:nfree], in_=src[:, :, 0:nfree], func=AF.Square),
             reads=[srckey], writes=[("sq",)])
        for c in range(NCH):
            P.op("pe", lambda e, c=c: e.matmul(ps[:, 0:nfree], lhsT=self.sb["ones"][:], rhs=sq[:, c, 0:nfree],
                                               start=(c == 0), stop=(c == NCH - 1)),
                 reads=[("sq",), ("c", "ones")], writes=[("ps", pi)])
        P.op("dve", lambda e: e.tensor_scalar(out=tmp[:, 0:nfree], in0=ps[:, 0:nfree], scalar1=1.0 / D, scalar2=EPS,
                                              op0=ALU.mult, op1=ALU.add),
             reads=[("ps", pi)], writes=[("nt",)])
        P.op("act", lambda e: e.activation(out=tmp[:, 0:nfree], in_=tmp[:, 0:nfree], func=AF.Sqrt),
             reads=[("nt",)], writes=[("nt",)])
        P.op("dve", lambda e: e.reciprocal(out=rstd[:, 0:nfree], in_=tmp[:, 0:nfree]),
             reads=[("nt",)], writes=[("rstd",)])

    def phase_A(self, l, src_name):
        nc, P, dr, sb = self.nc, self.P, self.dr, self.sb
        RC = 1024
        NTC = RC // TS
        vb = l * VPL
        with contextlib.ExitStack() as st:
            uT = self.sbuf(st, "A_uT", [128, NCH, RC], BF16)
            hT = self.sbuf(st, "A_hT", [128, NCH, TS], F32)
            sq = self.sbuf(st, "A_sq", [128, NCH, TS], BF16)
            rstd = self.sbuf(st, "A_rstd", [128, TS], F32)
            ntmp = self.sbuf(st, "A_ntmp", [128, TS], F32)
            wb = [self.sbuf(st, f"A_w{i}", [128, NCH, 512], BF16) for i in range(2)]
            NSTG = 6
            stg = [self.sbuf(st, f"A_stg{i}", [128, 512], BF16) for i in range(NSTG)]
            rcs = [self.sbuf(st, f"A_rcs{i}", [32, 2, TS], F32) for i in range(NTC)]
            rt1 = self.sbuf(st, "A_rt1", [32, TS], F32)
            rt2 = self.sbuf(st, "A_rt2", [32, TS], F32)
            ft = self.sbuf(st, "A_ft", [8, TS], F32)
            tiles = []
            for nm in ["fk", "fv", "ff", "ck", "cv", "sk", "sv", "wk", "wv", "fq", "nq", "ng", "mq", "bg"]:
                c0, w = SEG[nm]
                for o in range(0, w, 512):
                    tiles.append((nm, c0 + o, min(512, w - o), o))
            sgi = [0]

            def load_w(i):
                nm, c0, w, o = tiles[i]
                b = i % 2
                P.dma("pool", wb[b][:, :, 0:w],
                      dr["w_in"][l, :, c0:c0 + w].rearrange("(c p) n -> p c n", p=128),
                      writes=[("A_w", b)])

            for ch in range(T // RC):
                t0 = ch * RC
                for tl in range(NTC):
                    ts0 = t0 + tl * TS
                    P.dma("sp", hT[:], dr[src_name][:, :, ts0:ts0 + TS].rearrange("c p t -> p c t"),
                          reads=[(src_name, ts0 // TS)], writes=[("A_hT",)])
                    P.dma("sp", rcs[tl][:], dr["ropecs"][:, :, ts0:ts0 + TS].rearrange("a p t -> p a t"),
                          writes=[("A_rcs", tl)])
                    self.rmsnorm_stats(hT, ("A_hT",), sq, rstd, ntmp)
                    for c in range(NCH):
                        P.op("dve", lambda e, c=c, tl=tl: e.scalar_tensor_tensor(
                            out=uT[:, c, tl * TS:(tl + 1) * TS], in0=hT[:, c, :],
                            scalar=sb["vecs"][:, vb + VG_PRE_MIX + c:vb + VG_PRE_MIX + c + 1], in1=rstd[:],
                            op0=ALU.mult, op1=ALU.mult),
                            reads=[("A_hT",), ("rstd",), ("c", "vecs")], writes=[("A_uT", tl)])
                load_w(0)
                for i, (nm, c0, w, o) in enumerate(tiles):
                    if i + 1 < len(tiles):
                        load_w(i + 1)
                    b = i % 2
                    W = wb[b]
                    tokmajor = nm in ("fv", "sv", "wv")
                    if tokmajor:
                        dst = {"fv": "vf", "sv": "sv", "wv": "wv"}[nm]
                        for tb in range(RC // 128):
                            pi = self.nextps(); ps = self.ps[pi]
                            for c in range(NCH):
                                P.op("pe", lambda e, c=c, tb=tb, ps=ps, W=W, w=w: e.matmul(
                                    ps[:, 0:w], lhsT=uT[:, c, tb * 128:(tb + 1) * 128], rhs=W[:, c, 0:w],
                                    start=(c == 0), stop=(c == NCH - 1)),
                                    reads=[("A_uT", tb // 4), ("A_w", b)], writes=[("ps", pi)])
                            si = sgi[0] % NSTG; sgi[0] += 1
                            S = stg[si]
                            P.op("act", lambda e, S=S, ps=ps, w=w: e.activation(out=S[:, 0:w], in_=ps[:, 0:w], func=AF.Copy),
                                 reads=[("ps", pi)], writes=[("A_stg", si)])
                            r0 = t0 + tb * 128
                            P.dma("sp", dr[dst][r0:r0 + 128, o:o + w], S[:, 0:w],
                                  reads=[("A_stg", si)], writes=[(dst, r0 // 128, o // 512)])
                        continue
                    nblk = (w + 127) // 128
                    for cb in range(nblk):
                        m = min(128, w - cb * 128)
                        for tl in range(NTC):
                            ts0 = t0 + tl * TS
                            tj = ts0 // TS
                            pi = self.nextps(); ps = self.ps[pi]
                            for c in range(NCH):
                                P.op("pe", lambda e, c=c, tl=tl, ps=ps, W=W, cb=cb, m=m: e.matmul(
                                    ps[0:m, :], lhsT=W[:, c, cb * 128:cb * 128 + m], rhs=uT[:, c, tl * TS:(tl + 1) * TS],
                                    start=(c == 0), stop=(c == NCH - 1)),
                                    reads=[("A_uT", tl), ("A_w", b)], writes=[("ps", pi)])
                            gcb = (o // 128) + cb
                            if nm == "ff":
                                P.op("dve", lambda e, ps=ps: e.tensor_scalar(out=ft[:], in0=ps[0:8, :], scalar1=sb["vecs"][0:8, vb + V_NEGBF:vb + V_NEGBF + 1],
                                                                             scalar2=None, op0=ALU.add),
                                     reads=[("ps", pi), ("c", "vecs")], writes=[("A_ft",)])
                                P.op("act", lambda e: e.activation(out=ft[:], in_=ft[:], func=AF.Exp, scale=-1.0),
                                     reads=[("A_ft",)], writes=[("A_ft",)])
                                P.op("act", lambda e: e.activation(out=ft[:], in_=ft[:], func=AF.Ln, bias=1.0),
                                     reads=[("A_ft",)], writes=[("A_ft",)])
                                P.op("dve", lambda e: e.tensor_scalar(out=ft[:], in0=ft[:], scalar1=-1.0, scalar2=None, op0=ALU.mult),
                                     reads=[("A_ft",)], writes=[("A_ft",)])
                                P.dma("sp", dr["logf"][:, ts0:ts0 + TS], ft[:], reads=[("A_ft",)], writes=[("logf", tj)])
                                continue
                            si = sgi[0] % NSTG; sgi[0] += 1
                            S = stg[si]
                            if nm in ("bg", "ng"):
                                P.op("act", lambda e, S=S, ps=ps, m=m: e.activation(out=S[0:m, :], in_=ps[0:m, :], func=AF.Sigmoid),
                                     reads=[("ps", pi)], writes=[("A_stg", si)])
                            else:
                                sc = 1.0
                                if nm in ("fq", "nq"):
                                    sc = 128.0 ** -0.5
                                elif nm == "mq":
                                    sc = 256.0 ** -0.5
                                P.op("act", lambda e, S=S, ps=ps, m=m, sc=sc: e.activation(out=S[0:m, :], in_=ps[0:m, :], func=AF.Copy, scale=sc),
                                     reads=[("ps", pi)], writes=[("A_stg", si)])
                            if nm in ("nq", "ck", "sk", "wk"):
                                pj = self.nextps(); ps2 = self.ps[pj]
                                P.op("pe", lambda e, S=S, ps2=ps2: e.matmul(ps2[0:32, :], lhsT=sb["pm"][:], rhs=S[0:32, :], start=True, stop=True),
                                     reads=[("A_stg", si), ("c", "pm")], writes=[("ps", pj)])
                                P.op("dve", lambda e, S=S, tl=tl: e.tensor_tensor(out=rt1[:], in0=S[0:32, :], in1=rcs[tl][:, 0, :], op=ALU.mult),
                                     reads=[("A_stg", si), ("A_rcs", tl)], writes=[("A_rt1",)])
                                P.op("dve", lambda e, ps2=ps2, tl=tl: e.tensor_tensor(out=rt2[:], in0=ps2[0:32, :], in1=rcs[tl][:, 1, :], op=ALU.mult),
                                     reads=[("ps", pj), ("A_rcs", tl)], writes=[("A_rt2",)])
                                P.op("dve", lambda e, S=S: e.tensor_tensor(out=S[0:32, :], in0=rt1[:], in1=rt2[:], op=ALU.add),
                                     reads=[("A_rt1",), ("A_rt2",)], writes=[("A_stg", si)])
                            dstn = {"fq": "qfT", "fk": "kfT", "nq": "qnT", "ck": "ckT", "cv": "cvT", "sk": "skT", "wk": "wkT",
                                    "ng": "gnT", "mq": "mqT", "bg": "bgT"}[nm]
                            if nm == "ng":
                                P.dma("sp", dr["gnT"][:, ts0:ts0 + TS], S[0:24, :], reads=[("A_stg", si)], writes=[("gnT", tj)])
                            else:
                                P.dma("sp", dr[dstn][gcb, :, ts0:ts0 + TS], S[:, :], reads=[("A_stg", si)], writes=[(dstn, gcb, tj)])
        P.barrier()


def _phase_B(self, l):
    nc, P, dr, sb = self.nc, self.P, self.dr, self.sb
    vb = l * VPL
    ps = self.ps
    ident, ones = sb["ident"], sb["ones"]
    KI, KO = ("c", "ident"), ("c", "ones")

    with contextlib.ExitStack() as stB:
        kcT = [self.sbuf(stB, f"B_kcT{g}", [128, 256], BF16) for g in range(2)]
        vc = [self.sbuf(stB, f"B_vc{g}", [128, 2, 128], BF16) for g in range(2)]
        mkT = self.sbuf(stB, "B_mkT", [128, 8, 256], BF16)
        mv = self.sbuf(stB, "B_mv", [128, 2, 1024], BF16)
        NP_ = 6
        Pt = [self.sbuf(stB, f"B_P{i}", [128, 512], BF16) for i in range(NP_)]
        zacc = [self.sbuf(stB, f"B_zacc{i}", [128, 512], F32) for i in range(2)]
        zb16 = [self.sbuf(stB, f"B_zb16{i}", [128, 512], BF16) for i in range(2)]
        SB = [0, 1, 4]
        zt = self.sbuf(stB, "B_zt", [128, 512], F32)
        rz = self.sbuf(stB, "B_rz", [128, 512], F32)
        wt = self.sbuf(stB, "B_wt", [128, 512], F32)
        ctr = self.sbuf(stB, "B_ctr", [128, 512], F32)
        ostg = [self.sbuf(stB, f"B_ostg{i}", [128, 512], BF16) for i in range(4)]
        cnt = {"p": 0, "o": 0, "s": 0, "oz": 0, "z": 0}
        cstage = [self.sbuf(stB, f"B_cst{i}", [128, NCH, 512], BF16) for i in range(2)]
        self.cast_begin(l, cstage)

        with contextlib.ExitStack() as st:
            lf = self.sbuf(st, "B0_lf", [8, T], F32)
            on8 = self.sbuf(st, "B0_on8", [8, T], F32)
            cc = self.sbuf(st, "B0_c", [8, T], F32)
            cb16 = self.sbuf(st, "B0_cb", [8, 6, T], BF16)
            cf = on8
            P.dma("sp", lf[:], dr["logf"][:, :], reads=[("logf", j) for j in range(NT)], writes=[("B0_lf",)])
            P.op("pool", lambda e: e.memset(on8[:], 1.0), writes=[("B0_on8",)])
            P.op("dve", lambda e: e.tensor_tensor_scan(out=cc[:], data0=on8[:], data1=lf[:], initial=0.0,
                                                       op0=ALU.mult, op1=ALU.add),
                 reads=[("B0_lf",), ("B0_on8",)], writes=[("B0_c",)])
            for i in range(3):
                P.op("dve", lambda e, i=i: e.tensor_copy(out=cb16[:, i, :], in_=cc[:]), reads=[("B0_c",)], writes=[("B0_cb", i)])
                P.op("dve", lambda e, i=i: e.tensor_scalar(out=cb16[:, 3 + i, :], in0=cb16[:, i, :], scalar1=-1.0, scalar2=None, op0=ALU.mult),
                     reads=[("B0_cb", i)], writes=[("B0_cb", 3 + i)])
                if i < 2:
                    P.op("dve", lambda e, i=i: e.tensor_copy(out=cf[:], in_=cb16[:, i, :]), reads=[("B0_cb", i)], writes=[("B0_on8",)])
                    P.op("dve", lambda e: e.tensor_tensor(out=cc[:], in0=cc[:], in1=cf[:], op=ALU.subtract),
                         reads=[("B0_c",), ("B0_on8",)], writes=[("B0_c",)])
            P.dma("sp", dr["cs3"][:, :, :], cb16[:], reads=[("B0_cb", i) for i in range(6)], writes=[("cs3",)])
        P.barrier()
        with contextlib.ExitStack() as st:
            mT = self.sbuf(st, "B0_mT", [128, NCH, 256], F32)
            msq = self.sbuf(st, "B0_msq", [128, NCH, 256], BF16)
            umT = self.sbuf(st, "B0_umT", [128, NCH, 256], BF16)
            wbm = [self.sbuf(st, f"B0_wm{i}", [128, NCH, 512], BF16) for i in range(2)]
            P.dma("sp", mT[:], dr["memT"].rearrange("c p t -> p c t"), writes=[("B0_mT",)])
            self.rmsnorm_stats(mT, ("B0_mT",), msq, rz, zt, nfree=256)
            for c in range(NCH):
                P.op("dve", lambda e, c=c: e.scalar_tensor_tensor(
                    out=umT[:, c, :], in0=mT[:, c, :], scalar=sb["vecs"][:, vb + VG_MEM + c:vb + VG_MEM + c + 1],
                    in1=rz[:, 0:256], op0=ALU.mult, op1=ALU.mult),
                    reads=[("B0_mT",), ("rstd",), ("c", "vecs")], writes=[("B0_umT",)])
            for ti in range(4):
                b = ti % 2
                P.dma("pool", wbm[b][:], dr["w_mem_kv"][l, :, ti * 512:(ti + 1) * 512].rearrange("(c p) n -> p c n", p=128),
                      writes=[("B0_w", b)])
                if ti < 2:
                    for cbk in range(4):
                        pi = self.nextps(); pp = ps[pi]
                        for c in range(NCH):
                            P.op("pe", lambda e, c=c, pp=pp, b=b, cbk=cbk: e.matmul(
                                pp[:, 0:256], lhsT=wbm[b][:, c, cbk * 128:(cbk + 1) * 128], rhs=umT[:, c, :],
                                start=(c == 0), stop=(c == NCH - 1)),
                                reads=[("B0_umT",), ("B0_w", b)], writes=[("ps", pi)])
                        P.op("act", lambda e, pp=pp, ti=ti, cbk=cbk: e.activation(out=mkT[:, ti * 4 + cbk, :], in_=pp[:, 0:256], func=AF.Copy),
                             reads=[("ps", pi)], writes=[("B_mkT",)])
                else:
                    for mt in range(2):
                        pi = self.nextps(); pp = ps[pi]
                        for c in range(NCH):
                            P.op("pe", lambda e, c=c, pp=pp, b=b, mt=mt: e.matmul(
                                pp[:, :], lhsT=umT[:, c, mt * 128:(mt + 1) * 128], rhs=wbm[b][:, c, :],
                                start=(c == 0), stop=(c == NCH - 1)),
                                reads=[("B0_umT",), ("B0_w", b)], writes=[("ps", pi)])
                        P.op("act", lambda e, pp=pp, ti=ti, mt=mt: e.activation(out=mv[:, mt, (ti - 2) * 512:(ti - 1) * 512], in_=pp[:, :], func=AF.Copy),
                             reads=[("ps", pi)], writes=[("B_mv",)])

            xc = self.sbuf(st, "B0_xc", [128, T], BF16)
            w1 = self.sbuf(st, "B0_w1", [128, 32, 128], BF16)
            w2 = self.sbuf(st, "B0_w2", [128, 128], BF16)
            peT = self.sbuf(st, "B0_peT", [128, 32], BF16)
            bsb = self.sbuf(st, "B0_bsb", [128, 1], F32)
            xg = self.sbuf(st, "B0_xg", [128, 256], F32)
            x2 = self.sbuf(st, "B0_x2", [128, 256], F32)
            hd = self.sbuf(st, "B0_hd", [128, 256], BF16)
            P.op("pool", lambda e: e.memset(xg[:], 0.0), writes=[("B0_xg",)])
            for kv in range(2):
                sfx = "k" if kv == 0 else "v"
                P.dma("pool", w1[:], dr["w_cmp1_" + sfx][l].rearrange("(j d) o -> d j o", d=128), writes=[("B0_w1",)])
                P.dma("pool", w2[:], dr["w_cmp2_" + sfx][l], writes=[("B0_w2",)])
                P.op("pool", lambda e, sfx=sfx: e.dma_start(out=peT[:], in_=dr["pe_cmp_" + sfx][l].rearrange("j d -> d j"),
                                                            allow_slow_non_contiguous=True), writes=[("B0_peT",)], dma=True)
                for g in range(2):
                    srcn = "ckT" if kv == 0 else "cvT"
                    P.dma("sp", xc[:], dr[srcn][g, :, :], reads=[(srcn, g, j) for j in range(NT)], writes=[("B0_xc",)])
                    pi = self.nextps(); pp = ps[pi]
                    for j in range(32):
                        P.op("pe", lambda e, j=j, pp=pp: e.matmul(pp[:, 0:255], lhsT=w1[:, j, :], rhs=xc[:, j:j + 16 * 254 + 1:16],
                                                                  start=(j == 0), stop=(j == 31)),
                             reads=[("B0_xc",), ("B0_w1",)], writes=[("ps", pi)])
                    pj = self.nextps(); pp2 = ps[pj]
                    for j in range(32):
                        P.op("pe", lambda e, j=j, pp2=pp2: e.matmul(pp2[:, 0:1], lhsT=w1[:, j, :], rhs=peT[:, j:j + 1],
                                                                    start=(j == 0), stop=(j == 31)),
                             reads=[("B0_peT",), ("B0_w1",)], writes=[("ps", pj)])
                    P.op("dve", lambda e, pp2=pp2: e.tensor_copy(out=bsb[:], in_=pp2[:, 0:1]), reads=[("ps", pj)], writes=[("B0_bsb",)])
                    P.op("dve", lambda e, pp=pp: e.tensor_scalar(out=xg[:, 0:255], in0=pp[:, 0:255], scalar1=bsb[:, 0:1], scalar2=None, op0=ALU.add),
                         reads=[("ps", pi), ("B0_bsb",)], writes=[("B0_xg",)])
                    P.op("dve", lambda e: e.tensor_tensor(out=x2[:], in0=xg[:], in1=xg[:], op=ALU.mult), reads=[("B0_xg",)], writes=[("B0_x2",)])
                    P.op("dve", lambda e: e.tensor_scalar(out=x2[:], in0=x2[:], scalar1=0.044715, scalar2=1.0, op0=ALU.mult, op1=ALU.add),
                         reads=[("B0_x2",)], writes=[("B0_x2",)])
                    P.op("dve", lambda e: e.tensor_tensor(out=x2[:], in0=x2[:], in1=xg[:], op=ALU.mult), reads=[("B0_x2",), ("B0_xg",)], writes=[("B0_x2",)])
                    P.op("act", lambda e: e.activation(out=x2[:], in_=x2[:], func=AF.Tanh, scale=0.7978845608028654),
                         reads=[("B0_x2",)], writes=[("B0_x2",)])
                    P.op("dve", lambda e: e.tensor_scalar(out=x2[:], in0=x2[:], scalar1=1.0, scalar2=0.5, op0=ALU.add, op1=ALU.mult),
                         reads=[("B0_x2",)], writes=[("B0_x2",)])
                    P.op("dve", lambda e: e.tensor_tensor(out=hd[:], in0=x2[:], in1=xg[:], op=ALU.mult), reads=[("B0_x2",), ("B0_xg",)], writes=[("B0_hd",)])
                    if kv == 0:
                        pk = self.nextps(); pp3 = ps[pk]
                        P.op("pe", lambda e, pp3=pp3: e.matmul(pp3[:, 0:256], lhsT=w2[:], rhs=hd[:], start=True, stop=True),
                             reads=[("B0_hd",), ("B0_w2",)], writes=[("ps", pk)])
                        P.op("act", lambda e, pp3=pp3, g=g: e.activation(out=kcT[g][:], in_=pp3[:, 0:256], func=AF.Copy),
                             reads=[("ps", pk)], writes=[("B_kcT", g)])
                    else:
                        for nt in range(2):
                            pk = self.nextps(); pp3 = ps[pk]
                            P.op("pe", lambda e, pp3=pp3, nt=nt: e.matmul(pp3[:, 0:128], lhsT=hd[:, nt * 128:(nt + 1) * 128], rhs=w2[:], start=True, stop=True),
                                 reads=[("B0_hd",), ("B0_w2",)], writes=[("ps", pk)])
                            P.op("act", lambda e, pp3=pp3, g=g, nt=nt: e.activation(out=vc[g][:, nt, :], in_=pp3[:, 0:128], func=AF.Copy),
                                 reads=[("ps", pk)], writes=[("B_vc", g)])
        P.barrier()

        deferred = []

        def flush_deferred():
            while deferred:
                deferred.pop(0)()

        def store_later(dst_ap, src_ap, rkeys, wkeys):
            deferred.append(lambda: P.dma("pool", dst_ap, src_ap, reads=rkeys, writes=wkeys))

        def attn(blocks, pv_sets, obanks, zbank):
            nb = len(blocks)
            pend = []
            zi = cnt["z"] % 2; cnt["z"] += 1
            for bi, blk in enumerate(blocks):
                si = SB[cnt["s"] % 3]; cnt["s"] += 1
                S = ps[si]
                ns = len(blk["s"])
                for mi, (lt, rh, keys) in enumerate(blk["s"]):
                    P.op("pe", lambda e, S=S, lt=lt, rh=rh, mi=mi, ns=ns: e.matmul(S[:, :], lhsT=lt, rhs=rh, start=(mi == 0), stop=(mi == ns - 1)),
                         reads=keys, writes=[("ps", si)])
                pidx = cnt["p"] % NP_; cnt["p"] += 1
                Pb = Pt[pidx]
                P.op("act", lambda e, S=S, Pb=Pb: e.activation(out=Pb[:], in_=S[:, :], func=AF.Exp),
                     reads=[("ps", si)], writes=[("B_P", pidx)])
                if bi == 0:
                    P.op("pool", lambda e, Pb=Pb, zi=zi: e.tensor_copy(out=zacc[zi][:], in_=Pb[:]),
                         reads=[("B_P", pidx)], writes=[("B_zacc", zi)])
                else:
                    P.op("pool", lambda e, Pb=Pb, zi=zi: e.tensor_tensor(out=zacc[zi][:], in0=zacc[zi][:], in1=Pb[:], op=ALU.add),
                         reads=[("B_P", pidx), ("B_zacc", zi)], writes=[("B_zacc", zi)])
                if bi == min(4, nb - 1):
                    flush_deferred()
                pend.append((bi, blk, Pb, pidx))
                if len(pend) == 3:
                    emit_pv(pend.pop(0), nb, obanks, zbank)
            while pend:
                emit_pv(pend.pop(0), nb, obanks, zbank)
            P.op("pool", lambda e, zi=zi: e.tensor_copy(out=zb16[zi][:], in_=zacc[zi][:]),
                 reads=[("B_zacc", zi)], writes=[("B_zb16", zi)])
            P.op("pe", lambda e, zi=zi, zbank=zbank: e.matmul(ps[zbank][:, :], lhsT=ones[:], rhs=zb16[zi][:], start=True, stop=True),
                 reads=[("B_zb16", zi), KO], writes=[("ps", zbank)])

        def emit_pv(item, nb, obanks, zbank):
            bi, blk, Pb, pidx = item
            for oi, ob in enumerate(obanks):
                lt = blk["v"][oi]
                P.op("pe", lambda e, ob=ob, lt=lt, Pb=Pb, bi=bi, nb=nb: e.matmul(ps[ob][:, :], lhsT=lt, rhs=Pb[:], start=(bi == 0), stop=(bi == nb - 1)),
                     reads=[("B_P", pidx)] + blk["vkeys"], writes=[("ps", ob)])

        def next_oz():
            k = cnt["oz"] % 2; cnt["oz"] += 1
            return 2 + k, 5

        def next_ostg():
            k = cnt["o"] % 4; cnt["o"] += 1
            return k

        with contextlib.ExitStack() as st:
            causal = self.sbuf(st, "N_causal", [128, 4, 512], BF16)
            winm = self.sbuf(st, "N_winm", [128, 8, 512], BF16)
            ov = self.sbuf(st, "N_ov", [128, 2, 65], BF16)
            asel = self.sbuf(st, "N_asel", [64, 32, 128], BF16)
            P.dma("sp", causal[:], dr["causal"][:], writes=[("N_causal",)])
            P.dma("sp", winm[:], dr["winmask"][:], writes=[("N_winm",)])
            P.dma("sp", ov[:], dr["ov"][:], writes=[("N_ov",)])
            P.dma("sp", asel[:], dr["asel"][:], writes=[("N_asel",)])
            skT = self.sbuf(st, "N_skT", [128, T], BF16)
            svs = self.sbuf(st, "N_sv", [128, NB, 128], BF16)
            wkT = self.sbuf(st, "N_wkT", [128, T], BF16)
            wvs = self.sbuf(st, "N_wv", [128, NB, 128], BF16)
            selT = self.sbuf(st, "N_selT", [64, T], BF16)
            q4 = [self.sbuf(st, f"N_q4_{i}", [128, 4, 512], BF16) for i in range(2)]
            cmk = [self.sbuf(st, f"N_cmk{i}", [128, 2, 512], BF16) for i in range(2)]
            stb = [self.sbuf(st, f"N_stb{i}", [128, 3, 4, 64], F32) for i in range(2)]
            gnt = [self.sbuf(st, f"N_gn{i}", [24, 512], BF16) for i in range(2)]
            acc = [self.sbuf(st, f"N_acc{i}", [128, 512], F32) for i in range(4)]
            pslc = self.sbuf(st, "N_pslc", [128, 4, 64], F32)
            z4 = self.sbuf(st, "N_z4", [128, 4], F32)
            sc = self.sbuf(st, "N_sc", [128, 64], F32)
            sc2 = self.sbuf(st, "N_sc2", [128, 64], F32)
            m8 = self.sbuf(st, "N_m8", [128, 8], F32)
            m8b = self.sbuf(st, "N_m8b", [128, 8], F32)
            sbb = self.sbuf(st, "N_sbb", [128, 64], BF16)

            def gate_w(row, jb, zb):
                P.op("pe", lambda e, row=row, jb=jb: e.matmul(ps[6][:, :], lhsT=sb["selg"][:, row, :], rhs=gnt[jb][:], start=True, stop=True),
                     reads=[("N_gn", jb), ("c", "selg")], writes=[("ps", 6)])
                P.op("dve", lambda e, zb=zb: e.tensor_scalar(out=zt[:], in0=ps[zb][:, :], scalar1=1e-30, scalar2=None, op0=ALU.add),
                     reads=[("ps", zb)], writes=[("B_zt",)])
                P.op("dve", lambda e: e.reciprocal(out=rz[:], in_=zt[:]), reads=[("B_zt",)], writes=[("B_rz",)])
                P.op("dve", lambda e: e.tensor_tensor(out=wt[:], in0=ps[6][:, :], in1=rz[:], op=ALU.mult),
                     reads=[("ps", 6), ("B_rz",)], writes=[("B_wt",)])

            for g in range(2):
                P.dma("sp", skT[:], dr["skT"][g, :, :], reads=[("skT", g, j) for j in range(NT)], writes=[("N_skT",)])
                P.dma("sp", wkT[:], dr["wkT"][g, :, :], reads=[("wkT", g, j) for j in range(NT)], writes=[("N_wkT",)])
                P.dma("sp", svs[:], dr["sv"][:, g * 128:(g + 1) * 128].rearrange("(b p) d -> p b d", p=128),
                      reads=[("sv", r, 0) for r in range(NB)], writes=[("N_sv",)])
                P.dma("sp", wvs[:], dr["wv"][:, g * 128:(g + 1) * 128].rearrange("(b p) d -> p b d", p=128),
                      reads=[("wv", r, 0) for r in range(NB)], writes=[("N_wv",)])
                for j in range(NT):
                    jb = j % 2
                    t0 = j * TS
                    Q = q4[jb]
                    for hh in range(4):
                        h = 4 * g + hh
                        P.dma("sp", Q[:, hh, :], dr["qnT"][h, :, t0:t0 + TS], reads=[("qnT", h, j)], writes=[("N_q4", jb, hh)])
                    P.dma("sp", cmk[jb][:], dr["cmpmask"][:, j, :, :], writes=[("N_cmk", jb)])
                    P.dma("sp", stb[jb][:], dr["seltab"][:, :, 4 * j:4 * j + 4, :], writes=[("N_stb", jb)])
                    P.dma("sp", gnt[jb][:], dr["gnT"][:, t0:t0 + TS], reads=[("gnT", j)], writes=[("N_gn", jb)])
                    nts = [0] if j < 4 else [0, 1]
                    for hh in range(4):
                        h = 4 * g + hh
                        qa = Q[:, hh, :]
                        qk = ("N_q4", jb, hh)
                        ob, zb = next_oz()
                        blocks = []
                        for nt in nts:
                            blocks.append(dict(
                                s=[(kcT[g][:, nt * 128:(nt + 1) * 128], qa, [("B_kcT", g), qk]),
                                   (ident[:], cmk[jb][:, nt, :], [KI, ("N_cmk", jb)])],
                                v=[vc[g][:, nt, :]], vkeys=[("B_vc", g)]))
                        p_start = cnt["p"]
                        attn(blocks, 1, [ob], zb)
                        Es = [((p_start + i) % NP_) for i in range(len(nts))]
                        for r in range(4):
                            for i, nt in enumerate(nts):
                                Eb = Pt[Es[i]]
                                P.op("pe", lambda e, r=r, Eb=Eb, nt=nt, i=i, n=len(nts): e.matmul(
                                    ps[6][:, r * 128:r * 128 + 65], lhsT=Eb[:, r * 128:(r + 1) * 128], rhs=ov[:, nt, :],
                                    start=(i == 0), stop=(i == n - 1)),
                                    reads=[("B_P", Es[i]), ("N_ov",)], writes=[("ps", 6)])
                        p6 = ps[6][:, :].rearrange("p (r k) -> p r k", k=128)
                        P.op("dve", lambda e, p6=p6: e.tensor_scalar(out=z4[:], in0=p6[:, :, 64], scalar1=1e-30, scalar2=None, op0=ALU.add),
                             reads=[("ps", 6)], writes=[("N_z4",)])
                        P.op("dve", lambda e: e.reciprocal(out=z4[:], in_=z4[:]), reads=[("N_z4",)], writes=[("N_z4",)])
                        for r in range(4):
                            if hh == 0:
                                P.op("dve", lambda e, r=r, p6=p6: e.tensor_scalar(out=pslc[:, r, :], in0=p6[:, r, 0:64], scalar1=z4[:, r:r + 1], scalar2=None, op0=ALU.mult),
                                     reads=[("ps", 6), ("N_z4",)], writes=[("N_pslc",)])
                            else:
                                P.op("dve", lambda e, r=r, p6=p6: e.scalar_tensor_tensor(out=pslc[:, r, :], in0=p6[:, r, 0:64], scalar=z4[:, r:r + 1], in1=pslc[:, r, :],
                                                                                         op0=ALU.mult, op1=ALU.add),
                                     reads=[("ps", 6), ("N_z4",), ("N_pslc",)], writes=[("N_pslc",)])
                        gate_w(0 * 8 + h, jb, zb)
                        P.op("dve", lambda e, ob=ob, hh=hh: e.tensor_tensor(out=acc[hh][:], in0=ps[ob][:, :], in1=wt[:], op=ALU.mult),
                             reads=[("ps", ob), ("B_wt",)], writes=[("N_acc", hh)])
                    self.cast_step()
                    if getattr(self, "dbg", False) and g == 0 and j in (0, 1, 2):
                        dj = j
                        P.dma("sp", dr["d_acc"][dj], acc[0][:], reads=[("N_acc", 0)])
                        P.dma("sp", dr["d_pslc"][dj], pslc[:], reads=[("N_pslc",)])
                        P.dma("sp", dr["d_gn"][dj], gnt[jb][:], reads=[("N_gn", jb)])
                        P.dma("sp", dr["d_cmk"][dj], cmk[jb][:], reads=[("N_cmk", jb)])
                        P.dma("sp", dr["d_q4"][dj], Q[:], reads=[("N_q4", jb, hh) for hh in range(4)])
                        P.dma("sp", dr["d_stb"][dj], stb[jb][:], reads=[("N_stb", jb)])
                    for r in range(4):
                        qb = 4 * j + r
                        P.op("dve", lambda e, r=r, jb=jb: e.tensor_tensor(out=sc[:], in0=pslc[:, r, :], in1=stb[jb][:, 0, r, :], op=ALU.mult),
                             reads=[("N_pslc",), ("N_stb", jb)], writes=[("N_sc",)])
                        P.op("dve", lambda e, r=r, jb=jb: e.tensor_tensor(out=sc[:], in0=sc[:], in1=stb[jb][:, 1, r, :], op=ALU.add),
                             reads=[("N_sc",), ("N_stb", jb)], writes=[("N_sc",)])
                        P.op("dve", lambda e: e.max(out=m8[:], in_=sc[:]), reads=[("N_sc",)], writes=[("N_m8",)])
                        P.op("dve", lambda e: e.match_replace(out=sc2[:], in_to_replace=m8[:], in_values=sc[:], imm_value=-1e9),
                             reads=[("N_sc",), ("N_m8",)], writes=[("N_sc2",)])
                        P.op("dve", lambda e: e.max(out=m8b[:], in_=sc2[:]), reads=[("N_sc2",)], writes=[("N_m8b",)])
                        P.op("dve", lambda e: e.tensor_scalar(out=sc2[:], in0=sc[:], scalar1=m8b[:, 7:8], scalar2=None, op0=ALU.is_ge),
                             reads=[("N_sc",), ("N_m8b",)], writes=[("N_sc2",)])
                        P.op("dve", lambda e, r=r, jb=jb: e.tensor_tensor(out=sc2[:], in0=sc2[:], in1=stb[jb][:, 2, r, :], op=ALU.mult),
                             reads=[("N_sc2",), ("N_stb", jb)], writes=[("N_sc2",)])
                        P.op("dve", lambda e: e.tensor_scalar(out=sbb[:], in0=sc2[:], scalar1=BIG, scalar2=-BIG, op0=ALU.mult, op1=ALU.add),
                             reads=[("N_sc2",)], writes=[("N_sbb",)])
                        P.op("pe", lambda e: e.transpose(out=self.psb[0:64, 0:128], in_=sbb[:], identity=ident[:]),
                             reads=[("N_sbb",), KI], writes=[("psb",)])
                        P.op("act", lambda e, qb=qb: e.activation(out=selT[:, qb * 128:(qb + 1) * 128], in_=self.psb[0:64, 0:128], func=AF.Copy),
                             reads=[("psb",)], writes=[("N_selT", qb)])
                    for hh in range(4):
                        h = 4 * g + hh
                        qa = Q[:, hh, :]
                        qk = ("N_q4", jb, hh)
                        ob, zb = next_oz()
                        blocks = []
                        for kb in range(4 * j + 4):
                            s = [(skT[:, kb * 128:(kb + 1) * 128], qa, [("N_skT",), qk]),
                                 (asel[:, kb, :], selT[:, t0:t0 + TS], [("N_asel",)] + [("N_selT", 4 * j + r) for r in range(4)])]
                            if kb >= 4 * j:
                                s.append((ident[:], causal[:, kb - 4 * j, :], [KI, ("N_causal",)]))
                            blocks.append(dict(s=s, v=[svs[:, kb, :]], vkeys=[("N_sv",)]))
                        attn(blocks, 1, [ob], zb)
                        gate_w(1 * 8 + h, jb, zb)
                        P.op("dve", lambda e, ob=ob: e.tensor_tensor(out=ctr[:], in0=ps[ob][:, :], in1=wt[:], op=ALU.mult),
                             reads=[("ps", ob), ("B_wt",)], writes=[("B_ctr",)])
                        P.op("pool", lambda e, hh=hh: e.tensor_tensor(out=acc[hh][:], in0=acc[hh][:], in1=ctr[:], op=ALU.add),
                             reads=[("N_acc", hh), ("B_ctr",)], writes=[("N_acc", hh)])
                    self.cast_step()
                    for hh in range(4):
                        h = 4 * g + hh
                        qa = Q[:, hh, :]
                        qk = ("N_q4", jb, hh)
                        ob, zb = next_oz()
                        blocks = []
                        for kb in range(max(0, 4 * j - 4), 4 * j + 4):
                            s = [(wkT[:, kb * 128:(kb + 1) * 128], qa, [("N_wkT",), qk]),
                                 (ident[:], winm[:, kb - 4 * j + 4, :], [KI, ("N_winm",)])]
                            blocks.append(dict(s=s, v=[wvs[:, kb, :]], vkeys=[("N_wv",)]))
                        attn(blocks, 1, [ob], zb)
                        gate_w(2 * 8 + h, jb, zb)
                        P.op("dve", lambda e, ob=ob: e.tensor_tensor(out=ctr[:], in0=ps[ob][:, :], in1=wt[:], op=ALU.mult),
                             reads=[("ps", ob), ("B_wt",)], writes=[("B_ctr",)])
                        oi = next_ostg()
                        P.op("pool", lambda e, hh=hh, oi=oi: e.tensor_tensor(out=ostg[oi][:], in0=acc[hh][:], in1=ctr[:], op=ALU.add),
                             reads=[("N_acc", hh), ("B_ctr",)], writes=[("B_ostg", oi)])
                        store_later(dr["oT"][8 + h, :, t0:t0 + TS], ostg[oi][:], [("B_ostg", oi)], [("oT", 8 + h, j)])
                    self.cast_step()
                if getattr(self, "dbg", False) and g == 0:
                    P.dma("sp", dr["d_selT"][:, :], selT[:], reads=[("N_selT", q) for q in range(NB)])
        flush_deferred()
        P.barrier()

        with contextlib.ExitStack() as st:
            causal = self.sbuf(st, "F_causal", [128, 4, 512], BF16)
            P.dma("sp", causal[:], dr["causal"][:], writes=[("F_causal",)])
            kT = [self.sbuf(st, f"F_kT{i}", [128, T], BF16) for i in range(2)]
            vs = [self.sbuf(st, f"F_v{i}", [128, NB, 128], BF16) for i in range(2)]
            q6 = [self.sbuf(st, f"F_q6{i}", [6, T], BF16) for i in range(2)]
            k6 = [self.sbuf(st, f"F_k6{i}", [6, T], BF16) for i in range(2)]
            qt = [self.sbuf(st, f"F_q{i}", [128, 512], BF16) for i in range(2)]
            qc = 0
            for h in range(8):
                hb = h % 2
                P.dma("sp", kT[hb][:], dr["kfT"][h, :, :], reads=[("kfT", h, j) for j in range(NT)], writes=[("F_kT", hb)])
                P.dma("sp", vs[hb][:], dr["vf"][:, h * 128:(h + 1) * 128].rearrange("(b p) d -> p b d", p=128),
                      reads=[("vf", r, h // 4) for r in range(NB)], writes=[("F_v", hb)])
                P.op("pool", lambda e, hb=hb: e.memset(q6[hb][:], 1.0), writes=[("F_q6", hb)])
                P.op("pool", lambda e, hb=hb: e.memset(k6[hb][:], 1.0), writes=[("F_k6", hb)])
                P.dma("sp", q6[hb][0:3, :], dr["cs3"][h, 0:3, :], reads=[("cs3",)], writes=[("F_q6", hb)])
                P.dma("sp", k6[hb][3:6, :], dr["cs3"][h, 3:6, :], reads=[("cs3",)], writes=[("F_k6", hb)])
                for j in range(NT):
                    t0 = j * TS
                    qb_ = qc % 2; qc += 1
                    P.dma("sp", qt[qb_][:], dr["qfT"][h, :, t0:t0 + TS], reads=[("qfT", h, j)], writes=[("F_q", qb_)])
                    ob, zb = next_oz()
                    blocks = []
                    for kb in range(4 * j + 4):
                        s = [(kT[hb][:, kb * 128:(kb + 1) * 128], qt[qb_][:], [("F_kT", hb), ("F_q", qb_)]),
                             (k6[hb][:, kb * 128:(kb + 1) * 128], q6[hb][:, t0:t0 + TS], [("F_k6", hb), ("F_q6", hb)])]
                        if kb >= 4 * j:
                            s.append((ident[:], causal[:, kb - 4 * j, :], [KI, ("F_causal",)]))
                        blocks.append(dict(s=s, v=[vs[hb][:, kb, :]], vkeys=[("F_v", hb)]))
                    attn(blocks, 1, [ob], zb)
                    P.op("dve", lambda e, zb=zb: e.reciprocal(out=rz[:], in_=ps[zb][:, :]), reads=[("ps", zb)], writes=[("B_rz",)])
                    oi = next_ostg()
                    P.op("dve", lambda e, ob=ob, oi=oi: e.tensor_tensor(out=ostg[oi][:], in0=ps[ob][:, :], in1=rz[:], op=ALU.mult),
                         reads=[("ps", ob), ("B_rz",)], writes=[("B_ostg", oi)])
                    store_later(dr["oT"][h, :, t0:t0 + TS], ostg[oi][:], [("B_ostg", oi)], [("oT", h, j)])
                    self.cast_step()
        flush_deferred()
        P.barrier()

        with contextlib.ExitStack() as st:
            mq = [self.sbuf(st, f"M_q{i}", [128, 2, 512], BF16) for i in range(2)]
            qc = 0
            for h in range(4):
                for j in range(NT):
                    t0 = j * TS
                    qb_ = qc % 2; qc += 1
                    for c in range(2):
                        P.dma("sp", mq[qb_][:, c, :], dr["mqT"][h * 2 + c, :, t0:t0 + TS], reads=[("mqT", h * 2 + c, j)], writes=[("M_q", qb_, c)])
                    blocks = []
                    for mt in range(2):
                        s = [(mkT[:, h * 2 + c, mt * 128:(mt + 1) * 128], mq[qb_][:, c, :], [("B_mkT",), ("M_q", qb_, c)]) for c in range(2)]
                        blocks.append(dict(s=s, v=[mv[:, mt, h * 256 + c * 128:h * 256 + (c + 1) * 128] for c in range(2)], vkeys=[("B_mv",)]))
                    attn(blocks, 2, [2, 3], 5)
                    P.op("dve", lambda e: e.reciprocal(out=rz[:], in_=ps[5][:, :]), reads=[("ps", 5)], writes=[("B_rz",)])
                    for c in range(2):
                        oi = next_ostg()
                        P.op("dve", lambda e, c=c, oi=oi: e.tensor_tensor(out=ostg[oi][:], in0=ps[2 + c][:, :], in1=rz[:], op=ALU.mult),
                             reads=[("ps", 2 + c), ("B_rz",)], writes=[("B_ostg", oi)])
                        store_later(dr["oT"][16 + h * 2 + c, :, t0:t0 + TS], ostg[oi][:], [("B_ostg", oi)], [("oT", 16 + h * 2 + c, j)])
                    self.cast_step()
        flush_deferred()
    P.barrier()


Builder.phase_B = _phase_B


def _phase_C(self, l, src_name, dst_name):
    nc, P, dr, sb = self.nc, self.P, self.dr, self.sb
    vb = l * VPL
    ps = self.ps
    vec = sb["vecs"]
    with contextlib.ExitStack() as st:
        oTt = self.sbuf(st, "C_oT", [128, 24, TS], BF16)
        NG = 4
        gts = [self.sbuf(st, f"C_g{i}", [128, TS], BF16) for i in range(NG)]
        NWB = 4
        Wb = [self.sbuf(st, f"C_W{i}", [128, NCH, 512], BF16) for i in range(NWB)]
        mrg = self.sbuf(st, "C_mrg", [128, NCH, TS], BF16)
        y = self.sbuf(st, "C_y", [128, NCH, TS], F32)
        h = self.sbuf(st, "C_h", [128, NCH, TS], F32)
        ag = self.sbuf(st, "C_ag", [128, NCH, TS], BF16)
        tmp = [self.sbuf(st, f"C_t{i}", [128, TS], F32) for i in range(3)]
        rl = [self.sbuf(st, f"C_rl{i}", [128, TS], BF16) for i in range(2)]
        rstd = self.sbuf(st, "C_rstd", [128, TS], F32)
        ntmp = self.sbuf(st, "C_ntmp", [128, TS], F32)
        wi = [0]
        gi = [0]

        def loadW(dn, idx, nk):
            b = wi[0] % NWB; wi[0] += 1
            P.dma("sp", Wb[b][:, 0:nk, :], dr[dn][idx], reads=[("wc", dn, idx)], writes=[("C_W", b)])
            return b

        def norm_apply(srcbuf, srckey, gcol, dstfn):
            self.rmsnorm_stats(srcbuf, srckey, ag, rstd, ntmp)

        for j in range(NT):
            t0 = j * TS
            P.dma("sp", oTt[:], dr["oT"][:, :, t0:t0 + TS].rearrange("c p t -> p c t"),
                  reads=[("oT", c, j) for c in range(24)], writes=[("C_oT",)])
            for cg in range(4):
                bs = []
                for br, wn in enumerate(("w_up_fox", "w_up_nsa", "w_up_mem")):
                    bs.append(loadW("wcu", br * 4 + cg, 8))
                for cbl in range(4):
                    cb = cg * 4 + cbl
                    gb = []
                    for br in range(3):
                        k = gi[0] % NG; gi[0] += 1
                        P.dma("sp", gts[k][:], dr["bgT"][br * 16 + cb, :, t0:t0 + TS], reads=[("bgT", br * 16 + cb, j)], writes=[("C_g", k)])
                        gb.append(k)
                    pis = []
                    for br in range(3):
                        pi = self.nextps(); pis.append(pi)
                        for k in range(8):
                            P.op("pe", lambda e, pi=pi, br=br, k=k, cbl=cbl, b=bs[br]: e.matmul(
                                ps[pi][:, :], lhsT=Wb[b][:, k, cbl * 128:(cbl + 1) * 128], rhs=oTt[:, br * 8 + k, :],
                                start=(k == 0), stop=(k == 7)),
                                reads=[("C_W", bs[br]), ("C_oT",)], writes=[("ps", pi)])
                    for br in range(3):
                        P.op("dve", lambda e, br=br, pi=pis[br], k=gb[br]: e.tensor_tensor(out=tmp[br][:], in0=ps[pi][:, :], in1=gts[k][:], op=ALU.mult),
                             reads=[("ps", pis[br]), ("C_g", gb[br])], writes=[("C_t", br)])
                    P.op("pool", lambda e: e.tensor_tensor(out=tmp[0][:], in0=tmp[0][:], in1=tmp[1][:], op=ALU.add),
                         reads=[("C_t", 0), ("C_t", 1)], writes=[("C_t", 0)])
                    P.op("pool", lambda e, cb=cb: e.tensor_tensor(out=mrg[:, cb, :], in0=tmp[0][:], in1=tmp[2][:], op=ALU.add),
                         reads=[("C_t", 0), ("C_t", 2)], writes=[("C_mrg", cb)])
            MK = [("C_mrg", c) for c in range(NCH)]
            for cg in range(4):
                b = loadW("wcb", cg, NCH)
                for cbl in range(4):
                    cb = cg * 4 + cbl
                    pi = self.nextps()
                    for k in range(NCH):
                        P.op("pe", lambda e, pi=pi, k=k, cbl=cbl, b=b: e.matmul(
                            ps[pi][:, :], lhsT=Wb[b][:, k, cbl * 128:(cbl + 1) * 128], rhs=mrg[:, k, :],
                            start=(k == 0), stop=(k == NCH - 1)),
                            reads=[("C_W", b)] + MK, writes=[("ps", pi)])
                    P.op("act", lambda e, pi=pi, cb=cb: e.activation(out=y[:, cb, :], in_=ps[pi][:, :], func=AF.Copy),
                         reads=[("ps", pi)], writes=[("C_y", cb)])
            YK = [("C_y", c) for c in range(NCH)]
            P.dma("sp", h[:], dr[src_name][:, :, t0:t0 + TS].rearrange("c p t -> p c t"),
                  reads=[(src_name, j)], writes=[("C_h",)])
            P.op("act", lambda e: e.activation(out=ag[:], in_=y[:], func=AF.Square), reads=YK, writes=[("sq",)] + [("C_ag", c) for c in range(NCH)])
            self._stats_from_sq(ag, rstd, ntmp)
            for cb in range(NCH):
                tb = cb % 2
                P.op("dve", lambda e, cb=cb, tb=tb: e.scalar_tensor_tensor(
                    out=tmp[tb][:], in0=y[:, cb, :], scalar=vec[:, vb + VG_POST_MIX + cb:vb + VG_POST_MIX + cb + 1], in1=rstd[:],
                    op0=ALU.mult, op1=ALU.mult),
                    reads=[("C_y", cb), ("rstd",), ("c", "vecs")], writes=[("C_t", tb)])
                P.op("pool", lambda e, cb=cb, tb=tb: e.tensor_tensor(out=h[:, cb, :], in0=h[:, cb, :], in1=tmp[tb][:], op=ALU.add),
                     reads=[("C_h",), ("C_t", tb)], writes=[("C_h",)])
            P.op("act", lambda e: e.activation(out=ag[:], in_=h[:], func=AF.Square), reads=[("C_h",)], writes=[("sq",)] + [("C_ag", c) for c in range(NCH)])
            self._stats_from_sq(ag, rstd, ntmp)
            for cb in range(NCH):
                P.op("dve", lambda e, cb=cb: e.scalar_tensor_tensor(
                    out=mrg[:, cb, :], in0=h[:, cb, :], scalar=vec[:, vb + VG_PRE_MLP + cb:vb + VG_PRE_MLP + cb + 1], in1=rstd[:],
                    op0=ALU.mult, op1=ALU.mult),
                    reads=[("C_h",), ("rstd",), ("c", "vecs")], writes=[("C_mrg", cb)])
            for g in range(4):
                for wt_ in range(4):
                    b = loadW("wcb", 4 + g * 4 + wt_, NCH)
                    for cbl in range(4):
                        pi = self.nextps()
                        for k in range(NCH):
                            P.op("pe", lambda e, pi=pi, k=k, cbl=cbl, b=b: e.matmul(
                                ps[pi][:, :], lhsT=Wb[b][:, k, cbl * 128:(cbl + 1) * 128], rhs=mrg[:, k, :],
                                start=(k == 0), stop=(k == NCH - 1)),
                                reads=[("C_W", b)] + MK, writes=[("ps", pi)])
                        ri = (wt_ * 4 + cbl) % 2
                        P.op("act", lambda e, pi=pi, ri=ri: e.activation(out=rl[ri][:], in_=ps[pi][:, :], func=AF.Relu),
                             reads=[("ps", pi)], writes=[("C_rl", ri)])
                        ac = wt_ * 4 + cbl
                        P.op("pool", lambda e, ri=ri, ac=ac: e.tensor_tensor(out=ag[:, ac, :], in0=rl[ri][:], in1=rl[ri][:], op=ALU.mult),
                             reads=[("C_rl", ri)], writes=[("C_ag", ac)])
                AK = [("C_ag", c) for c in range(NCH)]
                for cg in range(4):
                    b = loadW("wcb", 20 + g * 4 + cg, NCH)
                    for cbl in range(4):
                        cb = cg * 4 + cbl
                        pi = self.nextps()
                        for k in range(NCH):
                            P.op("pe", lambda e, pi=pi, k=k, cbl=cbl, b=b: e.matmul(
                                ps[pi][:, :], lhsT=Wb[b][:, k, cbl * 128:(cbl + 1) * 128], rhs=ag[:, k, :],
                                start=(k == 0), stop=(k == NCH - 1)),
                                reads=[("C_W", b)] + AK, writes=[("ps", pi)])
                        if g == 0:
                            P.op("act", lambda e, pi=pi, cb=cb: e.activation(out=y[:, cb, :], in_=ps[pi][:, :], func=AF.Copy),
                                 reads=[("ps", pi)], writes=[("C_y", cb)])
                        else:
                            P.op("dve", lambda e, pi=pi, cb=cb: e.tensor_tensor(out=y[:, cb, :], in0=ps[pi][:, :], in1=y[:, cb, :], op=ALU.add),
                                 reads=[("ps", pi), ("C_y", cb)], writes=[("C_y", cb)])
            P.op("act", lambda e: e.activation(out=ag[:], in_=y[:], func=AF.Square), reads=YK, writes=[("sq",)] + AK)
            self._stats_from_sq(ag, rstd, ntmp)
            for cb in range(NCH):
                tb = cb % 2
                P.op("dve", lambda e, cb=cb, tb=tb: e.scalar_tensor_tensor(
                    out=tmp[tb][:], in0=y[:, cb, :], scalar=vec[:, vb + VG_POST_MLP + cb:vb + VG_POST_MLP + cb + 1], in1=rstd[:],
                    op0=ALU.mult, op1=ALU.mult),
                    reads=[("C_y", cb), ("rstd",), ("c", "vecs")], writes=[("C_t", tb)])
                P.op("pool", lambda e, cb=cb, tb=tb: e.tensor_tensor(out=h[:, cb, :], in0=h[:, cb, :], in1=tmp[tb][:], op=ALU.add),
                     reads=[("C_h",), ("C_t", tb)], writes=[("C_h",)])
            d = P.dma("act", dr[dst_name][:, :, t0:t0 + TS].rearrange("c p t -> p c t"), h[:],
                      reads=[("C_h",)], writes=[(dst_name, j)])
            if dst_name == "outT":
                self.final.append(d)
    P.barrier()


def _stats_from_sq(self, sq, rstd, tmp, nfree=512):
    P = self.P
    pi = self.nextps()
    ps = self.ps[pi]
    for c in range(NCH):
        P.op("pe", lambda e, c=c: e.matmul(ps[:, 0:nfree], lhsT=self.sb["ones"][:], rhs=sq[:, c, 0:nfree],
                                           start=(c == 0), stop=(c == NCH - 1)),
             reads=[("sq",), ("c", "ones")], writes=[("ps", pi)])
    P.op("dve", lambda e: e.tensor_scalar(out=tmp[:, 0:nfree], in0=ps[:, 0:nfree], scalar1=1.0 / D, scalar2=EPS,
                                          op0=ALU.mult, op1=ALU.add),
         reads=[("ps", pi)], writes=[("nt",)])
    P.op("act", lambda e: e.activation(out=tmp[:, 0:nfree], in_=tmp[:, 0:nfree], func=AF.Sqrt),
         reads=[("nt",)], writes=[("nt",)])
    P.op("dve", lambda e: e.reciprocal(out=rstd[:, 0:nfree], in_=tmp[:, 0:nfree]),
         reads=[("nt",)], writes=[("rstd",)])


Builder.phase_C = _phase_C
Builder._stats_from_sq = _stats_from_sq


from concourse.bass_utils import run_bass_kernel_spmd

N_ACTIVE = 4


def build_fused():
    B = Builder()
    src = "xT"
    for l in range(NL):
        B.phase_A(l, src)
        B.phase_B(l)
        dst = "outT" if l == NL - 1 else "hT"
        B.phase_C(l, src, dst)
        src = dst
    B.P.emit(final_waits=B.final)
    return B


def kernel(**inputs):
    inp = {k: np.asarray(v) for k, v in inputs.items()}
    consts = host_consts()
    consts["vecs"] = host_vecs(inp)
    B = build_fused()
    in_maps = []
    for b in range(N_ACTIVE):
        m = {"xT": np.ascontiguousarray(inp["x"][b].T.reshape(NCH, 128, T)).astype(np.float32),
             "memT": np.ascontiguousarray(inp["mem"][b].T.reshape(NCH, 128, 256)).astype(np.float32)}
        m.update(consts)
        for k in WEIGHT_SPECS:
            m[k] = np.ascontiguousarray(inp[k], dtype=np.float32)
        in_maps.append(m)
    res = run_bass_kernel_spmd(B.nc, in_maps, core_ids=list(range(N_ACTIVE)))
    out = np.empty((N_ACTIVE, T, D), np.float32)
    for b in range(N_ACTIVE):
        oT = np.asarray(res.results[b]["outT"], dtype=np.float32)
        out[b] = oT.reshape(D, T).T
    return out
```

```python
import contextlib
import numpy as np
import ml_dtypes
import concourse.bass as bass
import concourse.mybir as mybir

F32 = mybir.dt.float32
BF16 = mybir.dt.bfloat16
AF = mybir.ActivationFunctionType
ALU = mybir.AluOpType
NPBF = ml_dtypes.bfloat16

ENGS = ["pe", "act", "dve", "pool", "sp"]
EPOCH = 20000
NDSEM = {"sp": 28, "pool": 20, "act": 8}


class Op:
    __slots__ = ("eng", "fn", "deps", "dma", "sig", "sigord", "dsem", "dval", "prev")

    def __init__(self, eng, fn, deps, dma):
        self.eng = eng
        self.fn = fn
        self.deps = deps
        self.dma = dma
        self.sig = False
        self.sigord = 0
        self.dsem = None
        self.dval = 0
        self.prev = None


class Prog:
    def __init__(self, nc):
        self.nc = nc
        self.ops = {e: [] for e in ENGS}
        self.last_w = {}
        self.rd_c = {}
        self.rd_d = {}
        self.stack = contextlib.ExitStack()
        self.dma_since = []

    def op(self, eng, fn, reads=(), writes=(), dma=False):
        ops = self.ops[eng]
        idx = len(ops)
        deps = set()
        for k in reads:
            w = self.last_w.get(k)
            if w is not None:
                deps.add(w)
        for k in writes:
            w = self.last_w.get(k)
            if w is not None:
                deps.add(w)
            rc = self.rd_c.get(k)
            if rc:
                for e, i in rc.items():
                    deps.add((e, i))
            rd = self.rd_d.get(k)
            if rd:
                deps.update(rd)
        ops.append(Op(eng, fn, deps, dma))
        me = (eng, idx)
        if dma:
            self.dma_since.append(me)
        for k in reads:
            if dma:
                self.rd_d.setdefault(k, []).append(me)
            else:
                self.rd_c.setdefault(k, {})[eng] = idx
        for k in writes:
            self.last_w[k] = me
            self.rd_c[k] = {}
            self.rd_d[k] = []
        return me

    def dma(self, eng, out, in_, reads=(), writes=()):
        return self.op(eng, lambda e: e.dma_start(out=out, in_=in_), reads, writes, dma=True)

    def barrier(self):
        deps = set(self.dma_since)
        for e in ENGS:
            for i in range(len(self.ops[e]) - 1, -1, -1):
                if not self.ops[e][i].dma:
                    deps.add((e, i))
                    break
        for e in ENGS:
            self.ops[e].append(Op(e, lambda eng: eng.nop(), set(deps), False))
        self.dma_since = []

    def emit(self, final_waits=()):
        nc = self.nc
        ops = self.ops
        for e in ENGS:
            for o in ops[e]:
                for (de, di) in o.deps:
                    d = ops[de][di]
                    if not d.dma:
                        if de == "pe" and e == "pe":
                            continue
                        d.sig = True
        for (de, di) in final_waits:
            if not ops[de][di].dma:
                ops[de][di].sig = True
        nsig = {}
        for e in ENGS:
            c = 0
            for o in ops[e]:
                if o.dma:
                    continue
                if o.sig:
                    c += 1
                    o.sigord = c
            nsig[e] = c
        st = self.stack
        csem = {}
        for e in ENGS:
            n = (nsig[e] + EPOCH - 1) // EPOCH
            csem[e] = [st.enter_context(nc.semaphore(f"c_{e}_{i}")) for i in range(max(n, 1))]
        dsem = {}
        for e in ("sp", "pool", "act"):
            dsem[e] = [st.enter_context(nc.semaphore(f"d_{e}_{i}")) for i in range(NDSEM[e])]
        for e in ("sp", "pool", "act"):
            cnt = [0] * NDSEM[e]
            k = 0
            for o in ops[e]:
                if o.dma:
                    s = k % NDSEM[e]
                    k += 1
                    cnt[s] += 1
                    o.dsem = dsem[e][s]
                    o.dval = 16 * cnt[s]
        for e in ("pe", "dve"):
            for o in ops[e]:
                assert not o.dma

        def target(de, di):
            d = ops[de][di]
            if d.dma:
                return d.dsem, d.dval
            so = d.sigord
            return csem[de][(so - 1) // EPOCH], (so - 1) % EPOCH + 1

        engobj = {"pe": "tensor", "act": "scalar", "dve": "vector", "pool": "gpsimd", "sp": "sync"}
        stats = {}
        with nc.Block() as block:
            def body(ename):
                def run(eng):
                    seen = {}
                    nw = 0
                    for idx, o in enumerate(ops[ename]):
                        waits = []
                        for (de, di) in o.deps:
                            if de == "pe" and ename == "pe":
                                continue
                            waits.append(target(de, di))
                        if o.dma and o.dval > 16:
                            waits.append((o.dsem, o.dval - 16))
                        for (s, v) in waits:
                            key = id(s)
                            if seen.get(key, 0) >= v:
                                continue
                            seen[key] = v
                            eng.wait_ge(s, v)
                            nw += 1
                        ins = o.fn(eng)
                        if o.dma:
                            ins.then_inc(o.dsem, 16)
                        elif o.sig:
                            so = o.sigord
                            ins.then_inc(csem[ename][(so - 1) // EPOCH], 1)
                    if ename == "sp":
                        for (de, di) in final_waits:
                            s, v = target(de, di)
                            eng.wait_ge(s, v)
                    stats[ename] = (len(ops[ename]), nw)
                return run
            block.tensor(body("pe"))
            block.scalar(body("act"))
            block.vector(body("dve"))
            block.gpsimd(body("pool"))
            block.sync(body("sp"))
        return stats


import math

D = 2048
T = 4096
NCH = 16
DIN = 12832
NL = 2
BIG = 30000.0
EPS = 1e-6
TS = 512
NT = T // TS
NB = T // 128

SEG = {}
_o = 0
for _n, _w in [("fq", 1024), ("fk", 1024), ("fv", 1024), ("ff", 8), ("nq", 1024), ("ck", 256), ("cv", 256),
               ("sk", 256), ("sv", 256), ("wk", 256), ("wv", 256), ("ng", 24), ("mq", 1024), ("bg", 6144)]:
    SEG[_n] = (_o, _w)
    _o += _w
assert _o == DIN

VG_PRE_MIX, VG_POST_MIX, VG_PRE_MLP, VG_POST_MLP, VG_MEM, V_NEGBF = 0, 16, 32, 48, 64, 80
VPL = 81


def host_consts():
    c = {}
    half = 16
    inv = (500000.0 ** (-np.arange(half, dtype=np.float32) / half)).astype(np.float32)
    ang = np.arange(T, dtype=np.float32)[:, None] * inv[None, :]
    cos, sin = np.cos(ang).astype(np.float32), np.sin(ang).astype(np.float32)
    cs = np.zeros((2, 32, T), np.float32)
    cs[0, :16] = cos.T; cs[0, 16:] = cos.T
    cs[1, :16] = sin.T; cs[1, 16:] = sin.T
    c["ropecs"] = cs
    pm = np.zeros((32, 32), np.float32)
    for m in range(16):
        pm[m + 16, m] = -1.0
        pm[m, m + 16] = 1.0
    c["pm"] = pm.astype(NPBF)
    c["ident"] = np.eye(128, dtype=np.float32).astype(NPBF)
    c["ones"] = np.ones((128, 128), np.float32).astype(NPBF)
    k = np.arange(128)[:, None]
    q = np.arange(512)[None, :]
    cz = np.zeros((128, 4, 512), np.float32)
    for r in range(4):
        cz[:, r, :] = np.where(r * 128 + k <= q, 0.0, -BIG)
    c["causal"] = cz.astype(NPBF)
    wz = np.zeros((128, 8, 512), np.float32)
    for i in range(8):
        rel = q - ((i - 4) * 128 + k)
        wz[:, i, :] = np.where((rel >= 0) & (rel < 512), 0.0, -BIG)
    c["winmask"] = wz.astype(NPBF)
    n = np.arange(256)
    tt = np.arange(T)
    valid = (n[:, None] * 16 + 31 <= tt[None, :]) & (n[:, None] < 255)
    cm = np.where(valid, 0.0, -BIG).astype(np.float32)
    cm = cm.reshape(2, 128, NT, 512).transpose(1, 2, 0, 3)
    c["cmpmask"] = np.ascontiguousarray(cm).astype(NPBF)
    ci = n[:, None] * 16
    sj = np.arange(64)[None, :] * 64
    ov = ((ci <= sj + 63) & (ci + 31 >= sj) & (n[:, None] < 255)).astype(np.float32)
    ovo = np.concatenate([ov, np.ones((256, 1), np.float32)], axis=1)
    ovo[255, :] = 0.0
    c["ov"] = np.ascontiguousarray(ovo.reshape(2, 128, 65).transpose(1, 0, 2)).astype(NPBF)
    a = np.zeros((64, 32, 128), np.float32)
    for kb in range(32):
        a[2 * kb, kb, 0:64] = 1.0
        a[2 * kb + 1, kb, 64:128] = 1.0
    c["asel"] = a.astype(NPBF)
    blk = np.arange(64)[None, :]
    cur = (tt // 64)[:, None]
    sel_valid = blk <= cur
    forced = (blk == 0) | (blk == cur) | (blk == cur - 1)
    vt = (sel_valid & ~forced).astype(np.float32)
    ct = np.where(forced, 1e6, np.where(sel_valid, 0.0, -1.0)).astype(np.float32)
    def tm(x):
        return np.ascontiguousarray(x.reshape(32, 128, 64).transpose(1, 0, 2)).astype(np.float32)
    c["seltab"] = np.stack([tm(vt), tm(ct), tm(sel_valid.astype(np.float32))], axis=1)
    sg = np.zeros((24, 24, 128), np.float32)
    for r in range(24):
        sg[r, r, :] = 1.0
    c["selg"] = sg.astype(NPBF)
    return c


def host_vecs(inp):
    v = np.zeros((128, NL * VPL), np.float32)
    for l in range(NL):
        b = l * VPL
        for nm, off in [("g_pre_mix", VG_PRE_MIX), ("g_post_mix", VG_POST_MIX), ("g_pre_mlp", VG_PRE_MLP),
                        ("g_post_mlp", VG_POST_MLP), ("g_mem", VG_MEM)]:
            v[:, b + off:b + off + 16] = np.asarray(inp[nm][l], np.float32).reshape(16, 128).T
        v[:8, b + V_NEGBF] = np.asarray(inp["b_f"][l], np.float32)
    return v


CONST_SPECS = {
    "ropecs": ([2, 32, T], F32), "pm": ([32, 32], BF16), "ident": ([128, 128], BF16), "ones": ([128, 128], BF16),
    "causal": ([128, 4, 512], BF16), "winmask": ([128, 8, 512], BF16), "cmpmask": ([128, NT, 2, 512], BF16),
    "ov": ([128, 2, 65], BF16), "asel": ([64, 32, 128], BF16), "seltab": ([128, 3, 32, 64], F32),
    "selg": ([24, 24, 128], BF16), "vecs": ([128, NL * VPL], F32),
}
WEIGHT_SPECS = {
    "w_in": [NL, D, DIN], "w_cmp1_k": [NL, 4096, 128], "w_cmp2_k": [NL, 128, 128], "pe_cmp_k": [NL, 32, 128],
    "w_cmp1_v": [NL, 4096, 128], "w_cmp2_v": [NL, 128, 128], "pe_cmp_v": [NL, 32, 128],
    "w_mem_kv": [NL, D, 2048], "w_up_fox": [NL, 1024, D], "w_up_nsa": [NL, 1024, D], "w_up_mem": [NL, 1024, D],
    "w_o": [NL, D, D], "w_mlp1": [NL, D, 8192], "w_mlp2": [NL, 8192, D],
}
SCRATCH = {
    "hT": ([NCH, 128, T], F32),
    "qfT": ([8, 128, T], BF16), "kfT": ([8, 128, T], BF16), "vf": ([T, 1024], BF16), "logf": ([8, T], F32),
    "qnT": ([8, 128, T], BF16), "ckT": ([2, 128, T], BF16), "cvT": ([2, 128, T], BF16),
    "skT": ([2, 128, T], BF16), "sv": ([T, 256], BF16), "wkT": ([2, 128, T], BF16), "wv": ([T, 256], BF16),
    "gnT": ([24, T], BF16), "mqT": ([8, 128, T], BF16), "bgT": ([48, 128, T], BF16),
    "oT": ([24, 128, T], BF16), "cs3": ([8, 6, T], BF16),
    "wcu": ([12, 128, 8, 512], BF16), "wcb": ([36, 128, 16, 512], BF16),
}


DBG = {"d_acc": ([3, 128, 512], F32), "d_pslc": ([3, 128, 4, 64], F32), "d_gn": ([3, 24, 512], BF16),
       "d_cmk": ([3, 128, 2, 512], BF16), "d_q4": ([3, 128, 4, 512], BF16), "d_stb": ([3, 128, 3, 4, 64], F32),
       "d_selT": ([64, T], BF16)}


class Builder:
    def __init__(self, ext_in=(), ext_out=(), dbg=False):
        self.nc = nc = bass.Bass("TRN2", target_bir_lowering=False)
        self.P = Prog(nc)
        self.dr = {}
        self.dr["xT"] = nc.dram_tensor("xT", [NCH, 128, T], F32, kind="ExternalInput").ap()
        self.dr["memT"] = nc.dram_tensor("memT", [NCH, 128, 256], F32, kind="ExternalInput").ap()
        for k, (shp, dt) in CONST_SPECS.items():
            self.dr[k] = nc.dram_tensor(k, shp, dt, kind="ExternalInput").ap()
        for k, shp in WEIGHT_SPECS.items():
            self.dr[k] = nc.dram_tensor(k, shp, F32, kind="ExternalInput").ap()
        for k, (shp, dt) in SCRATCH.items():
            kind = "Internal"
            if k in ext_in:
                kind = "ExternalInput"
            elif k in ext_out:
                kind = "ExternalOutput"
            self.dr[k] = nc.dram_tensor(k, shp, dt, kind=kind).ap()
        self.dr["outT"] = nc.dram_tensor("outT", [NCH, 128, T], F32, kind="ExternalOutput").ap()
        self.dbg = dbg
        if dbg:
            for k, (shp, dt) in DBG.items():
                self.dr[k] = nc.dram_tensor(k, shp, dt, kind="ExternalOutput").ap()
        self.st = self.P.stack
        self.final = []
        self.sb = {}
        for k in ("pm", "ident", "ones", "vecs", "selg"):
            shp, dt = CONST_SPECS[k]
            self.sb[k] = self.st.enter_context(nc.sbuf_tensor("c_" + k, shp, dt))
            src = self.dr[k]
            self.P.dma("sp", self.sb[k][:], src[:], writes=[("c", k)])
        self.ps = [self.st.enter_context(nc.psum_tensor(f"ps{i}", [128, 512], F32)) for i in range(7)]
        self.psb = self.st.enter_context(nc.psum_tensor("psb", [128, 1024], BF16))
        self.psi = 0
        self._uid = 0

    def uid(self):
        self._uid += 1
        return self._uid

    def sbuf(self, stack, name, shape, dt):
        return stack.enter_context(self.nc.sbuf_tensor(f"{name}_{self.uid()}", shape, dt))

    def cast_begin(self, l, stage):
        dr = self.dr
        jobs = []
        for br, wn in enumerate(("w_up_fox", "w_up_nsa", "w_up_mem")):
            for cg in range(4):
                jobs.append((dr[wn][l, :, cg * 512:(cg + 1) * 512].rearrange("(c p) n -> p c n", p=128), "wcu", br * 4 + cg, 8))
        for cg in range(4):
            jobs.append((dr["w_o"][l, :, cg * 512:(cg + 1) * 512].rearrange("(c p) n -> p c n", p=128), "wcb", cg, NCH))
        for i in range(16):
            jobs.append((dr["w_mlp1"][l, :, i * 512:(i + 1) * 512].rearrange("(c p) n -> p c n", p=128), "wcb", 4 + i, NCH))
        for g in range(4):
            for cg in range(4):
                jobs.append((dr["w_mlp2"][l, g * 2048:(g + 1) * 2048, cg * 512:(cg + 1) * 512].rearrange("(c p) n -> p c n", p=128),
                             "wcb", 20 + g * 4 + cg, NCH))
        self.cjobs = jobs
        self.cstage = stage
        self.cpos = 0

    def cast_step(self):
        P = self.P
        i = self.cpos
        n = len(self.cjobs)
        if i > n:
            return
        if i < n:
            src, dn, idx, nk = self.cjobs[i]
            sb_ = self.cstage[i % 2]
            P.dma("pool", sb_[:, 0:nk, :], src, writes=[("cstage", i % 2)])
        if i >= 1:
            src, dn, idx, nk = self.cjobs[i - 1]
            sb_ = self.cstage[(i - 1) % 2]
            P.dma("pool", self.dr[dn][idx], sb_[:, 0:nk, :], reads=[("cstage", (i - 1) % 2)], writes=[("wc", dn, idx)])
        self.cpos += 1

    def nextps(self, lo=0, hi=7):
        i = lo + (self.psi % (hi - lo))
        self.psi += 1
        return i

    def rmsnorm_stats(self, src, srckey, sq, rstd, tmp, nfree=512):
        P = self.P
        pi = self.nextps()
        ps = self.ps[pi]
        P.op("act", lambda e: e.activation(out=sq[:, :, 0:nfree], in_=src[:, :, 0:nfree], func=AF.Square),
             reads=[srckey], writes=[("sq",)])
        for c in range(NCH):
            P.op("pe", lambda e, c=c: e.matmul(ps[:, 0:nfree], lhsT=self.sb["ones"][:], rhs=sq[:, c, 0:nfree],
                                               start=(c == 0), stop=(c == NCH - 1)),
                 reads=[("sq",), ("c", "ones")], writes=[("ps", pi)])
        P.op("dve", lambda e: e.tensor_scalar(out=tmp[:, 0:nfree], in0=ps[:, 0:nfree], scalar1=1.0 / D, scalar2=EPS,
                                              op0=ALU.mult, op1=ALU.add),
             reads=[("ps", pi)], writes=[("nt",)])
        P.op("act", lambda e: e.activation(out=tmp[:, 0:nfree], in_=tmp[:, 0:nfree], func=AF.Sqrt),
             reads=[("nt",)], writes=[("nt",)])
        P.op("dve", lambda e: e.reciprocal(out=rstd[:, 0:nfree], in_=tmp[:, 0:nfree]),
             reads=[("nt",)], writes=[("rstd",)])

    def phase_A(self, l, src_name):
        nc, P, dr, sb = self.nc, self.P, self.dr, self.sb
        RC = 1024
        NTC = RC // TS
        vb = l * VPL
        with contextlib.ExitStack() as st:
            uT = self.sbuf(st, "A_uT", [128, NCH, RC], BF16)
            hT = self.sbuf(st, "A_hT", [128, NCH, TS], F32)
            sq = self.sbuf(st, "A_sq", [128, NCH, TS], BF16)
            rstd = self.sbuf(st, "A_rstd", [128, TS], F32)
            ntmp = self.sbuf(st, "A_ntmp", [128, TS], F32)
            wb = [self.sbuf(st, f"A_w{i}", [128, NCH, 512], BF16) for i in range(2)]
            NSTG = 6
            stg = [self.sbuf(st, f"A_stg{i}", [128, 512], BF16) for i in range(NSTG)]
            rcs = [self.sbuf(st, f"A_rcs{i}", [32, 2, TS], F32) for i in range(NTC)]
            rt1 = self.sbuf(st, "A_rt1", [32, TS], F32)
            rt2 = self.sbuf(st, "A_rt2", [32, TS], F32)
            ft = self.sbuf(st, "A_ft", [8, TS], F32)
            tiles = []
            for nm in ["fk", "fv", "ff", "ck", "cv", "sk", "sv", "wk", "wv", "fq", "nq", "ng", "mq", "bg"]:
                c0, w = SEG[nm]
                for o in range(0, w, 512):
                    tiles.append((nm, c0 + o, min(512, w - o), o))
            sgi = [0]

            def load_w(i):
                nm, c0, w, o = tiles[i]
                b = i % 2
                P.dma("pool", wb[b][:, :, 0:w],
                      dr["w_in"][l, :, c0:c0 + w].rearrange("(c p) n -> p c n", p=128),
                      writes=[("A_w", b)])

            for ch in range(T // RC):
                t0 = ch * RC
                for tl in range(NTC):
                    ts0 = t0 + tl * TS
                    P.dma("sp", hT[:], dr[src_name][:, :, ts0:ts0 + TS].rearrange("c p t -> p c t"),
                          reads=[(src_name, ts0 // TS)], writes=[("A_hT",)])
                    P.dma("sp", rcs[tl][:], dr["ropecs"][:, :, ts0:ts0 + TS].rearrange("a p t -> p a t"),
                          writes=[("A_rcs", tl)])
                    self.rmsnorm_stats(hT, ("A_hT",), sq, rstd, ntmp)
                    for c in range(NCH):
                        P.op("dve", lambda e, c=c, tl=tl: e.scalar_tensor_tensor(
                            out=uT[:, c, tl * TS:(tl + 1) * TS], in0=hT[:, c, :],
                            scalar=sb["vecs"][:, vb + VG_PRE_MIX + c:vb + VG_PRE_MIX + c + 1], in1=rstd[:],
                            op0=ALU.mult, op1=ALU.mult),
                            reads=[("A_hT",), ("rstd",), ("c", "vecs")], writes=[("A_uT", tl)])
                load_w(0)
                for i, (nm, c0, w, o) in enumerate(tiles):
                    if i + 1 < len(tiles):
                        load_w(i + 1)
                    b = i % 2
                    W = wb[b]
                    tokmajor = nm in ("fv", "sv", "wv")
                    if tokmajor:
                        dst = {"fv": "vf", "sv": "sv", "wv": "wv"}[nm]
                        for tb in range(RC // 128):
                            pi = self.nextps(); ps = self.ps[pi]
                            for c in range(NCH):
                                P.op("pe", lambda e, c=c, tb=tb, ps=ps, W=W, w=w: e.matmul(
                                    ps[:, 0:w], lhsT=uT[:, c, tb * 128:(tb + 1) * 128], rhs=W[:, c, 0:w],
                                    start=(c == 0), stop=(c == NCH - 1)),
                                    reads=[("A_uT", tb // 4), ("A_w", b)], writes=[("ps", pi)])
                            si = sgi[0] % NSTG; sgi[0] += 1
                            S = stg[si]
                            P.op("act", lambda e, S=S, ps=ps, w=w: e.activation(out=S[:, 0:w], in_=ps[:, 0:w], func=AF.Copy),
                                 reads=[("ps", pi)], writes=[("A_stg", si)])
                            r0 = t0 + tb * 128
                            P.dma("sp", dr[dst][r0:r0 + 128, o:o + w], S[:, 0:w],
                                  reads=[("A_stg", si)], writes=[(dst, r0 // 128, o // 512)])
                        continue
                    nblk = (w + 127) // 128
                    for cb in range(nblk):
                        m = min(128, w - cb * 128)
                        for tl in range(NTC):
                            ts0 = t0 + tl * TS
                            tj = ts0 // TS
                            pi = self.nextps(); ps = self.ps[pi]
                            for c in range(NCH):
                                P.op("pe", lambda e, c=c, tl=tl, ps=ps, W=W, cb=cb, m=m: e.matmul(
                                    ps[0:m, :], lhsT=W[:, c, cb * 128:cb * 128 + m], rhs=uT[:, c, tl * TS:(tl + 1) * TS],
                                    start=(c == 0), stop=(c == NCH - 1)),
                                    reads=[("A_uT", tl), ("A_w", b)], writes=[("ps", pi)])
                            gcb = (o // 128) + cb
                            if nm == "ff":
                                P.op("dve", lambda e, ps=ps: e.tensor_scalar(out=ft[:], in0=ps[0:8, :], scalar1=sb["vecs"][0:8, vb + V_NEGBF:vb + V_NEGBF + 1],
                                                                             scalar2=None, op0=ALU.add),
                                     reads=[("ps", pi), ("c", "vecs")], writes=[("A_ft",)])
                                P.op("act", lambda e: e.activation(out=ft[:], in_=ft[:], func=AF.Exp, scale=-1.0),
                                     reads=[("A_ft",)], writes=[("A_ft",)])
                                P.op("act", lambda e: e.activation(out=ft[:], in_=ft[:], func=AF.Ln, bias=1.0),
                                     reads=[("A_ft",)], writes=[("A_ft",)])
                                P.op("dve", lambda e: e.tensor_scalar(out=ft[:], in0=ft[:], scalar1=-1.0, scalar2=None, op0=ALU.mult),
                                     reads=[("A_ft",)], writes=[("A_ft",)])
                                P.dma("sp", dr["logf"][:, ts0:ts0 + TS], ft[:], reads=[("A_ft",)], writes=[("logf", tj)])
                                continue
                            si = sgi[0] % NSTG; sgi[0] += 1
                            S = stg[si]
                            if nm in ("bg", "ng"):
                                P.op("act", lambda e, S=S, ps=ps, m=m: e.activation(out=S[0:m, :], in_=ps[0:m, :], func=AF.Sigmoid),
                                     reads=[("ps", pi)], writes=[("A_stg", si)])
                            else:
                                sc = 1.0
                                if nm in ("fq", "nq"):
                                    sc = 128.0 ** -0.5
                                elif nm == "mq":
                                    sc = 256.0 ** -0.5
                                P.op("act", lambda e, S=S, ps=ps, m=m, sc=sc: e.activation(out=S[0:m, :], in_=ps[0:m, :], func=AF.Copy, scale=sc),
                                     reads=[("ps", pi)], writes=[("A_stg", si)])
                            if nm in ("nq", "ck", "sk", "wk"):
                                pj = self.nextps(); ps2 = self.ps[pj]
                                P.op("pe", lambda e, S=S, ps2=ps2: e.matmul(ps2[0:32, :], lhsT=sb["pm"][:], rhs=S[0:32, :], start=True, stop=True),
                                     reads=[("A_stg", si), ("c", "pm")], writes=[("ps", pj)])
                                P.op("dve", lambda e, S=S, tl=tl: e.tensor_tensor(out=rt1[:], in0=S[0:32, :], in1=rcs[tl][:, 0, :], op=ALU.mult),
                                     reads=[("A_stg", si), ("A_rcs", tl)], writes=[("A_rt1",)])
                                P.op("dve", lambda e, ps2=ps2, tl=tl: e.tensor_tensor(out=rt2[:], in0=ps2[0:32, :], in1=rcs[tl][:, 1, :], op=ALU.mult),
                                     reads=[("ps", pj), ("A_rcs", tl)], writes=[("A_rt2",)])
                                P.op("dve", lambda e, S=S: e.tensor_tensor(out=S[0:32, :], in0=rt1[:], in1=rt2[:], op=ALU.add),
                                     reads=[("A_rt1",), ("A_rt2",)], writes=[("A_stg", si)])
                            dstn = {"fq": "qfT", "fk": "kfT", "nq": "qnT", "ck": "ckT", "cv": "cvT", "sk": "skT", "wk": "wkT",
                                    "ng": "gnT", "mq": "mqT", "bg": "bgT"}[nm]
                            if nm == "ng":
                                P.dma("sp", dr["gnT"][:, ts0:ts0 + TS], S[0:24, :], reads=[("A_stg", si)], writes=[("gnT", tj)])
                            else:
                                P.dma("sp", dr[dstn][gcb, :, ts0:ts0 + TS], S[:, :], reads=[("A_stg", si)], writes=[(dstn, gcb, tj)])
        P.barrier()


def _phase_B(self, l):
    nc, P, dr, sb = self.nc, self.P, self.dr, self.sb
    vb = l * VPL
    ps = self.ps
    ident, ones = sb["ident"], sb["ones"]
    KI, KO = ("c", "ident"), ("c", "ones")

    with contextlib.ExitStack() as stB:
        kcT = [self.sbuf(stB, f"B_kcT{g}", [128, 256], BF16) for g in range(2)]
        vc = [self.sbuf(stB, f"B_vc{g}", [128, 2, 128], BF16) for g in range(2)]
        mkT = self.sbuf(stB, "B_mkT", [128, 8, 256], BF16)
        mv = self.sbuf(stB, "B_mv", [128, 2, 1024], BF16)
        Pt = [self.sbuf(stB, f"B_P{i}", [128, 512], BF16) for i in range(4)]
        zt = self.sbuf(stB, "B_zt", [128, 512], F32)
        rz = self.sbuf(stB, "B_rz", [128, 512], F32)
        wt = self.sbuf(stB, "B_wt", [128, 512], F32)
        ctr = self.sbuf(stB, "B_ctr", [128, 512], F32)
        ostg = [self.sbuf(stB, f"B_ostg{i}", [128, 512], BF16) for i in range(4)]
        cnt = {"p": 0, "o": 0, "s": 0, "oz": 0}
        cstage = [self.sbuf(stB, f"B_cst{i}", [128, NCH, 512], BF16) for i in range(2)]
        self.cast_begin(l, cstage)

        with contextlib.ExitStack() as st:
            lf = self.sbuf(st, "B0_lf", [8, T], F32)
            on8 = self.sbuf(st, "B0_on8", [8, T], F32)
            cc = self.sbuf(st, "B0_c", [8, T], F32)
            cb16 = self.sbuf(st, "B0_cb", [8, 6, T], BF16)
            cf = on8
            P.dma("sp", lf[:], dr["logf"][:, :], reads=[("logf", j) for j in range(NT)], writes=[("B0_lf",)])
            P.op("pool", lambda e: e.memset(on8[:], 1.0), writes=[("B0_on8",)])
            P.op("dve", lambda e: e.tensor_tensor_scan(out=cc[:], data0=on8[:], data1=lf[:], initial=0.0,
                                                       op0=ALU.mult, op1=ALU.add),
                 reads=[("B0_lf",), ("B0_on8",)], writes=[("B0_c",)])
            for i in range(3):
                P.op("dve", lambda e, i=i: e.tensor_copy(out=cb16[:, i, :], in_=cc[:]), reads=[("B0_c",)], writes=[("B0_cb", i)])
                P.op("dve", lambda e, i=i: e.tensor_scalar(out=cb16[:, 3 + i, :], in0=cb16[:, i, :], scalar1=-1.0, scalar2=None, op0=ALU.mult),
                     reads=[("B0_cb", i)], writes=[("B0_cb", 3 + i)])
                if i < 2:
                    P.op("dve", lambda e, i=i: e.tensor_copy(out=cf[:], in_=cb16[:, i, :]), reads=[("B0_cb", i)], writes=[("B0_on8",)])
                    P.op("dve", lambda e: e.tensor_tensor(out=cc[:], in0=cc[:], in1=cf[:], op=ALU.subtract),
                         reads=[("B0_c",), ("B0_on8",)], writes=[("B0_c",)])
            P.dma("sp", dr["cs3"][:, :, :], cb16[:], reads=[("B0_cb", i) for i in range(6)], writes=[("cs3",)])
        P.barrier()
        with contextlib.ExitStack() as st:
            mT = self.sbuf(st, "B0_mT", [128, NCH, 256], F32)
            msq = self.sbuf(st, "B0_msq", [128, NCH, 256], BF16)
            umT = self.sbuf(st, "B0_umT", [128, NCH, 256], BF16)
            wbm = [self.sbuf(st, f"B0_wm{i}", [128, NCH, 512], BF16) for i in range(2)]
            P.dma("sp", mT[:], dr["memT"].rearrange("c p t -> p c t"), writes=[("B0_mT",)])
            self.rmsnorm_stats(mT, ("B0_mT",), msq, rz, zt, nfree=256)
            for c in range(NCH):
                P.op("dve", lambda e, c=c: e.scalar_tensor_tensor(
                    out=umT[:, c, :], in0=mT[:, c, :], scalar=sb["vecs"][:, vb + VG_MEM + c:vb + VG_MEM + c + 1],
                    in1=rz[:, 0:256], op0=ALU.mult, op1=ALU.mult),
                    reads=[("B0_mT",), ("rstd",), ("c", "vecs")], writes=[("B0_umT",)])
            for ti in range(4):
                b = ti % 2
                P.dma("pool", wbm[b][:], dr["w_mem_kv"][l, :, ti * 512:(ti + 1) * 512].rearrange("(c p) n -> p c n", p=128),
                      writes=[("B0_w", b)])
                if ti < 2:
                    for cbk in range(4):
                        pi = self.nextps(); pp = ps[pi]
                        for c in range(NCH):
                            P.op("pe", lambda e, c=c, pp=pp, b=b, cbk=cbk: e.matmul(
                                pp[:, 0:256], lhsT=wbm[b][:, c, cbk * 128:(cbk + 1) * 128], rhs=umT[:, c, :],
                                start=(c == 0), stop=(c == NCH - 1)),
                                reads=[("B0_umT",), ("B0_w", b)], writes=[("ps", pi)])
                        P.op("act", lambda e, pp=pp, ti=ti, cbk=cbk: e.activation(out=mkT[:, ti * 4 + cbk, :], in_=pp[:, 0:256], func=AF.Copy),
                             reads=[("ps", pi)], writes=[("B_mkT",)])
                else:
                    for mt in range(2):
                        pi = self.nextps(); pp = ps[pi]
                        for c in range(NCH):
                            P.op("pe", lambda e, c=c, pp=pp, b=b, mt=mt: e.matmul(
                                pp[:, :], lhsT=umT[:, c, mt * 128:(mt + 1) * 128], rhs=wbm[b][:, c, :],
                                start=(c == 0), stop=(c == NCH - 1)),
                                reads=[("B0_umT",), ("B0_w", b)], writes=[("ps", pi)])
                        P.op("act", lambda e, pp=pp, ti=ti, mt=mt: e.activation(out=mv[:, mt, (ti - 2) * 512:(ti - 1) * 512], in_=pp[:, :], func=AF.Copy),
                             reads=[("ps", pi)], writes=[("B_mv",)])

            xc = self.sbuf(st, "B0_xc", [128, T], BF16)
            w1 = self.sbuf(st, "B0_w1", [128, 32, 128], BF16)
            w2 = self.sbuf(st, "B0_w2", [128, 128], BF16)
            peT = self.sbuf(st, "B0_peT", [128, 32], BF16)
            bsb = self.sbuf(st, "B0_bsb", [128, 1], F32)
            xg = self.sbuf(st, "B0_xg", [128, 256], F32)
            x2 = self.sbuf(st, "B0_x2", [128, 256], F32)
            hd = self.sbuf(st, "B0_hd", [128, 256], BF16)
            P.op("pool", lambda e: e.memset(xg[:], 0.0), writes=[("B0_xg",)])
            for kv in range(2):
                sfx = "k" if kv == 0 else "v"
                P.dma("pool", w1[:], dr["w_cmp1_" + sfx][l].rearrange("(j d) o -> d j o", d=128), writes=[("B0_w1",)])
                P.dma("pool", w2[:], dr["w_cmp2_" + sfx][l], writes=[("B0_w2",)])
                P.op("pool", lambda e, sfx=sfx: e.dma_start(out=peT[:], in_=dr["pe_cmp_" + sfx][l].rearrange("j d -> d j"),
                                                            allow_slow_non_contiguous=True), writes=[("B0_peT",)], dma=True)
                for g in range(2):
                    srcn = "ckT" if kv == 0 else "cvT"
                    P.dma("sp", xc[:], dr[srcn][g, :, :], reads=[(srcn, g, j) for j in range(NT)], writes=[("B0_xc",)])
                    pi = self.nextps(); pp = ps[pi]
                    for j in range(32):
                        P.op("pe", lambda e, j=j, pp=pp: e.matmul(pp[:, 0:255], lhsT=w1[:, j, :], rhs=xc[:, j:j + 16 * 254 + 1:16],
                                                                  start=(j == 0), stop=(j == 31)),
                             reads=[("B0_xc",), ("B0_w1",)], writes=[("ps", pi)])
                    pj = self.nextps(); pp2 = ps[pj]
                    for j in range(32):
                        P.op("pe", lambda e, j=j, pp2=pp2: e.matmul(pp2[:, 0:1], lhsT=w1[:, j, :], rhs=peT[:, j:j + 1],
                                                                    start=(j == 0), stop=(j == 31)),
                             reads=[("B0_peT",), ("B0_w1",)], writes=[("ps", pj)])
                    P.op("dve", lambda e, pp2=pp2: e.tensor_copy(out=bsb[:], in_=pp2[:, 0:1]), reads=[("ps", pj)], writes=[("B0_bsb",)])
                    P.op("dve", lambda e, pp=pp: e.tensor_scalar(out=xg[:, 0:255], in0=pp[:, 0:255], scalar1=bsb[:, 0:1], scalar2=None, op0=ALU.add),
                         reads=[("ps", pi), ("B0_bsb",)], writes=[("B0_xg",)])
                    P.op("dve", lambda e: e.tensor_tensor(out=x2[:], in0=xg[:], in1=xg[:], op=ALU.mult), reads=[("B0_xg",)], writes=[("B0_x2",)])
                    P.op("dve", lambda e: e.tensor_scalar(out=x2[:], in0=x2[:], scalar1=0.044715, scalar2=1.0, op0=ALU.mult, op1=ALU.add),
                         reads=[("B0_x2",)], writes=[("B0_x2",)])
                    P.op("dve", lambda e: e.tensor_tensor(out=x2[:], in0=x2[:], in1=xg[:], op=ALU.mult), reads=[("B0_x2",), ("B0_xg",)], writes=[("B0_x2",)])
                    P.op("act", lambda e: e.activation(out=x2[:], in_=x2[:], func=AF.Tanh, scale=0.7978845608028654),
                         reads=[("B0_x2",)], writes=[("B0_x2",)])
                    P.op("dve", lambda e: e.tensor_scalar(out=x2[:], in0=x2[:], scalar1=1.0, scalar2=0.5, op0=ALU.add, op1=ALU.mult),
                         reads=[("B0_x2",)], writes=[("B0_x2",)])
                    P.op("dve", lambda e: e.tensor_tensor(out=hd[:], in0=x2[:], in1=xg[:], op=ALU.mult), reads=[("B0_x2",), ("B0_xg",)], writes=[("B0_hd",)])
                    if kv == 0:
                        pk = self.nextps(); pp3 = ps[pk]
                        P.op("pe", lambda e, pp3=pp3: e.matmul(pp3[:, 0:256], lhsT=w2[:], rhs=hd[:], start=True, stop=True),
                             reads=[("B0_hd",), ("B0_w2",)], writes=[("ps", pk)])
                        P.op("act", lambda e, pp3=pp3, g=g: e.activation(out=kcT[g][:], in_=pp3[:, 0:256], func=AF.Copy),
                             reads=[("ps", pk)], writes=[("B_kcT", g)])
                    else:
                        for nt in range(2):
                            pk = self.nextps(); pp3 = ps[pk]
                            P.op("pe", lambda e, pp3=pp3, nt=nt: e.matmul(pp3[:, 0:128], lhsT=hd[:, nt * 128:(nt + 1) * 128], rhs=w2[:], start=True, stop=True),
                                 reads=[("B0_hd",), ("B0_w2",)], writes=[("ps", pk)])
                            P.op("act", lambda e, pp3=pp3, g=g, nt=nt: e.activation(out=vc[g][:, nt, :], in_=pp3[:, 0:128], func=AF.Copy),
                                 reads=[("ps", pk)], writes=[("B_vc", g)])
        P.barrier()

        def attn(blocks, pv_sets, obanks, zbank):
            nb = len(blocks)
            pend = []
            for bi, blk in enumerate(blocks):
                si = cnt["s"] % 2; cnt["s"] += 1
                S = ps[si]
                ns = len(blk["s"])
                c0, c1 = blk.get("cols", (0, 512))
                assert bi > 0 or (c0, c1) == (0, 512)
                for mi, ent in enumerate(blk["s"]):
                    lt, rh, keys = ent[0], ent[1], ent[2]
                    a0, a1 = ent[3] if len(ent) > 3 else (c0, c1)
                    P.op("pe", lambda e, S=S, lt=lt, rh=rh, mi=mi, ns=ns, a0=a0, a1=a1: e.matmul(
                        S[:, a0:a1], lhsT=lt, rhs=rh[:, a0:a1], start=(mi == 0), stop=(mi == ns - 1)),
                         reads=keys, writes=[("ps", si)])
                pidx = cnt["p"] % 4; cnt["p"] += 1
                Pb = Pt[pidx]
                P.op("act", lambda e, S=S, Pb=Pb, c0=c0, c1=c1: e.activation(out=Pb[:, c0:c1], in_=S[:, c0:c1], func=AF.Exp),
                     reads=[("ps", si)], writes=[("B_P", pidx)])
                pend.append((bi, blk, Pb, pidx))
                if len(pend) == 2:
                    emit_pv(pend.pop(0), nb, obanks, zbank)
            while pend:
                emit_pv(pend.pop(0), nb, obanks, zbank)

        def emit_pv(item, nb, obanks, zbank):
            bi, blk, Pb, pidx = item
            c0, c1 = blk.get("cols", (0, 512))
            for oi, ob in enumerate(obanks):
                lt = blk["v"][oi]
                P.op("pe", lambda e, ob=ob, lt=lt, Pb=Pb, bi=bi, nb=nb, c0=c0, c1=c1: e.matmul(
                    ps[ob][:, c0:c1], lhsT=lt, rhs=Pb[:, c0:c1], start=(bi == 0), stop=(bi == nb - 1)),
                     reads=[("B_P", pidx)] + blk["vkeys"], writes=[("ps", ob)])
            P.op("pe", lambda e, Pb=Pb, bi=bi, nb=nb, c0=c0, c1=c1: e.matmul(
                ps[zbank][:, c0:c1], lhsT=ones[:], rhs=Pb[:, c0:c1], start=(bi == 0), stop=(bi == nb - 1)),
                 reads=[("B_P", pidx), KO], writes=[("ps", zbank)])

        def next_oz():
            k = cnt["oz"] % 2; cnt["oz"] += 1
            return 2 + k, 4 + k

        def next_ostg():
            k = cnt["o"] % 4; cnt["o"] += 1
            return k

        with contextlib.ExitStack() as st:
            causal = self.sbuf(st, "N_causal", [128, 4, 512], BF16)
            winm = self.sbuf(st, "N_winm", [128, 8, 512], BF16)
            ov = self.sbuf(st, "N_ov", [128, 2, 65], BF16)
            asel = self.sbuf(st, "N_asel", [64, 32, 128], BF16)
            P.dma("sp", causal[:], dr["causal"][:], writes=[("N_causal",)])
            P.dma("sp", winm[:], dr["winmask"][:], writes=[("N_winm",)])
            P.dma("sp", ov[:], dr["ov"][:], writes=[("N_ov",)])
            P.dma("sp", asel[:], dr["asel"][:], writes=[("N_asel",)])
            skT = self.sbuf(st, "N_skT", [128, T], BF16)
            svs = self.sbuf(st, "N_sv", [128, NB, 128], BF16)
            wkT = self.sbuf(st, "N_wkT", [128, T], BF16)
            wvs = self.sbuf(st, "N_wv", [128, NB, 128], BF16)
            selT = self.sbuf(st, "N_selT", [64, T], BF16)
            q4 = [self.sbuf(st, f"N_q4_{i}", [128, 4, 512], BF16) for i in range(2)]
            cmk = [self.sbuf(st, f"N_cmk{i}", [128, 2, 512], BF16) for i in range(2)]
            stb = [self.sbuf(st, f"N_stb{i}", [128, 3, 4, 64], F32) for i in range(2)]
            gnt = [self.sbuf(st, f"N_gn{i}", [24, 512], BF16) for i in range(2)]
            acc = [self.sbuf(st, f"N_acc{i}", [128, 512], F32) for i in range(4)]
            pslc = self.sbuf(st, "N_pslc", [128, 4, 64], F32)
            z4 = self.sbuf(st, "N_z4", [128, 4], F32)
            sc = self.sbuf(st, "N_sc", [128, 64], F32)
            sc2 = self.sbuf(st, "N_sc2", [128, 64], F32)
            m8 = self.sbuf(st, "N_m8", [128, 8], F32)
            m8b = self.sbuf(st, "N_m8b", [128, 8], F32)
            sbb = self.sbuf(st, "N_sbb", [128, 64], BF16)

            def gate_w(row, jb, zb):
                P.op("pe", lambda e, row=row, jb=jb: e.matmul(ps[6][:, :], lhsT=sb["selg"][:, row, :], rhs=gnt[jb][:], start=True, stop=True),
                     reads=[("N_gn", jb), ("c", "selg")], writes=[("ps", 6)])
                P.op("dve", lambda e, zb=zb: e.tensor_scalar(out=zt[:], in0=ps[zb][:, :], scalar1=1e-30, scalar2=None, op0=ALU.add),
                     reads=[("ps", zb)], writes=[("B_zt",)])
                P.op("dve", lambda e: e.reciprocal(out=rz[:], in_=zt[:]), reads=[("B_zt",)], writes=[("B_rz",)])
                P.op("dve", lambda e: e.tensor_tensor(out=wt[:], in0=ps[6][:, :], in1=rz[:], op=ALU.mult),
                     reads=[("ps", 6), ("B_rz",)], writes=[("B_wt",)])

            for g in range(2):
                P.dma("sp", skT[:], dr["skT"][g, :, :], reads=[("skT", g, j) for j in range(NT)], writes=[("N_skT",)])
                P.dma("sp", wkT[:], dr["wkT"][g, :, :], reads=[("wkT", g, j) for j in range(NT)], writes=[("N_wkT",)])
                P.dma("sp", svs[:], dr["sv"][:, g * 128:(g + 1) * 128].rearrange("(b p) d -> p b d", p=128),
                      reads=[("sv", r, 0) for r in range(NB)], writes=[("N_sv",)])
                P.dma("sp", wvs[:], dr["wv"][:, g * 128:(g + 1) * 128].rearrange("(b p) d -> p b d", p=128),
                      reads=[("wv", r, 0) for r in range(NB)], writes=[("N_wv",)])
                for j in range(NT):
                    jb = j % 2
                    t0 = j * TS
                    Q = q4[jb]
                    for hh in range(4):
                        h = 4 * g + hh
                        P.dma("sp", Q[:, hh, :], dr["qnT"][h, :, t0:t0 + TS], reads=[("qnT", h, j)], writes=[("N_q4", jb, hh)])
                    P.dma("sp", cmk[jb][:], dr["cmpmask"][:, j, :, :], writes=[("N_cmk", jb)])
                    P.dma("sp", stb[jb][:], dr["seltab"][:, :, 4 * j:4 * j + 4, :], writes=[("N_stb", jb)])
                    P.dma("sp", gnt[jb][:], dr["gnT"][:, t0:t0 + TS], reads=[("gnT", j)], writes=[("N_gn", jb)])
                    nts = [0] if j < 4 else [0, 1]
                    for hh in range(4):
                        h = 4 * g + hh
                        qa = Q[:, hh, :]
                        qk = ("N_q4", jb, hh)
                        ob, zb = next_oz()
                        blocks = []
                        for nt in nts:
                            blocks.append(dict(
                                s=[(kcT[g][:, nt * 128:(nt + 1) * 128], qa, [("B_kcT", g), qk]),
                                   (ident[:], cmk[jb][:, nt, :], [KI, ("N_cmk", jb)])],
                                v=[vc[g][:, nt, :]], vkeys=[("B_vc", g)]))
                        p_start = cnt["p"]
                        attn(blocks, 1, [ob], zb)
                        Es = [((p_start + i) % 4) for i in range(len(nts))]
                        for r in range(4):
                            for i, nt in enumerate(nts):
                                Eb = Pt[Es[i]]
                                P.op("pe", lambda e, r=r, Eb=Eb, nt=nt, i=i, n=len(nts): e.matmul(
                                    ps[6][:, r * 128:r * 128 + 65], lhsT=Eb[:, r * 128:(r + 1) * 128], rhs=ov[:, nt, :],
                                    start=(i == 0), stop=(i == n - 1)),
                                    reads=[("B_P", Es[i]), ("N_ov",)], writes=[("ps", 6)])
                        p6 = ps[6][:, :].rearrange("p (r k) -> p r k", k=128)
                        P.op("dve", lambda e, p6=p6: e.tensor_scalar(out=z4[:], in0=p6[:, :, 64], scalar1=1e-30, scalar2=None, op0=ALU.add),
                             reads=[("ps", 6)], writes=[("N_z4",)])
                        P.op("dve", lambda e: e.reciprocal(out=z4[:], in_=z4[:]), reads=[("N_z4",)], writes=[("N_z4",)])
                        for r in range(4):
                            if hh == 0:
                                P.op("dve", lambda e, r=r, p6=p6: e.tensor_scalar(out=pslc[:, r, :], in0=p6[:, r, 0:64], scalar1=z4[:, r:r + 1], scalar2=None, op0=ALU.mult),
                                     reads=[("ps", 6), ("N_z4",)], writes=[("N_pslc",)])
                            else:
                                P.op("dve", lambda e, r=r, p6=p6: e.scalar_tensor_tensor(out=pslc[:, r, :], in0=p6[:, r, 0:64], scalar=z4[:, r:r + 1], in1=pslc[:, r, :],
                                                                                         op0=ALU.mult, op1=ALU.add),
                                     reads=[("ps", 6), ("N_z4",), ("N_pslc",)], writes=[("N_pslc",)])
                        gate_w(0 * 8 + h, jb, zb)
                        P.op("dve", lambda e, ob=ob, hh=hh: e.tensor_tensor(out=acc[hh][:], in0=ps[ob][:, :], in1=wt[:], op=ALU.mult),
                             reads=[("ps", ob), ("B_wt",)], writes=[("N_acc", hh)])
                    self.cast_step()
                    if getattr(self, "dbg", False) and g == 0 and j in (0, 1, 2):
                        dj = j
                        P.dma("sp", dr["d_acc"][dj], acc[0][:], reads=[("N_acc", 0)])
                        P.dma("sp", dr["d_pslc"][dj], pslc[:], reads=[("N_pslc",)])
                        P.dma("sp", dr["d_gn"][dj], gnt[jb][:], reads=[("N_gn", jb)])
                        P.dma("sp", dr["d_cmk"][dj], cmk[jb][:], reads=[("N_cmk", jb)])
                        P.dma("sp", dr["d_q4"][dj], Q[:], reads=[("N_q4", jb, hh) for hh in range(4)])
                        P.dma("sp", dr["d_stb"][dj], stb[jb][:], reads=[("N_stb", jb)])
                    for r in range(4):
                        qb = 4 * j + r
                        P.op("dve", lambda e, r=r, jb=jb: e.tensor_tensor(out=sc[:], in0=pslc[:, r, :], in1=stb[jb][:, 0, r, :], op=ALU.mult),
                             reads=[("N_pslc",), ("N_stb", jb)], writes=[("N_sc",)])
                        P.op("dve", lambda e, r=r, jb=jb: e.tensor_tensor(out=sc[:], in0=sc[:], in1=stb[jb][:, 1, r, :], op=ALU.add),
                             reads=[("N_sc",), ("N_stb", jb)], writes=[("N_sc",)])
                        P.op("dve", lambda e: e.max(out=m8[:], in_=sc[:]), reads=[("N_sc",)], writes=[("N_m8",)])
                        P.op("dve", lambda e: e.match_replace(out=sc2[:], in_to_replace=m8[:], in_values=sc[:], imm_value=-1e9),
                             reads=[("N_sc",), ("N_m8",)], writes=[("N_sc2",)])
                        P.op("dve", lambda e: e.max(out=m8b[:], in_=sc2[:]), reads=[("N_sc2",)], writes=[("N_m8b",)])
                        P.op("dve", lambda e: e.tensor_scalar(out=sc2[:], in0=sc[:], scalar1=m8b[:, 7:8], scalar2=None, op0=ALU.is_ge),
                             reads=[("N_sc",), ("N_m8b",)], writes=[("N_sc2",)])
                        P.op("dve", lambda e, r=r, jb=jb: e.tensor_tensor(out=sc2[:], in0=sc2[:], in1=stb[jb][:, 2, r, :], op=ALU.mult),
                             reads=[("N_sc2",), ("N_stb", jb)], writes=[("N_sc2",)])
                        P.op("dve", lambda e: e.tensor_scalar(out=sbb[:], in0=sc2[:], scalar1=BIG, scalar2=-BIG, op0=ALU.mult, op1=ALU.add),
                             reads=[("N_sc2",)], writes=[("N_sbb",)])
                        P.op("pe", lambda e: e.transpose(out=self.psb[0:64, 0:128], in_=sbb[:], identity=ident[:]),
                             reads=[("N_sbb",), KI], writes=[("psb",)])
                        P.op("act", lambda e, qb=qb: e.activation(out=selT[:, qb * 128:(qb + 1) * 128], in_=self.psb[0:64, 0:128], func=AF.Copy),
                             reads=[("psb",)], writes=[("N_selT", qb)])
                    for hh in range(4):
                        h = 4 * g + hh
                        qa = Q[:, hh, :]
                        qk = ("N_q4", jb, hh)
                        ob, zb = next_oz()
                        blocks = []
                        for kb in range(4 * j + 4):
                            s = [(skT[:, kb * 128:(kb + 1) * 128], qa, [("N_skT",), qk]),
                                 (asel[:, kb, :], selT[:, t0:t0 + TS], [("N_asel",)] + [("N_selT", 4 * j + r) for r in range(4)])]
                            cols = (0, 512)
                            if kb >= 4 * j:
                                r_ = kb - 4 * j
                                cols = (128 * r_, 512)
                                s.append((ident[:], causal[:, r_, :], [KI, ("N_causal",)], (128 * r_, 128 * r_ + 128)))
                            blocks.append(dict(s=s, v=[svs[:, kb, :]], vkeys=[("N_sv",)], cols=cols))
                        attn(blocks, 1, [ob], zb)
                        gate_w(1 * 8 + h, jb, zb)
                        P.op("dve", lambda e, ob=ob: e.tensor_tensor(out=ctr[:], in0=ps[ob][:, :], in1=wt[:], op=ALU.mult),
                             reads=[("ps", ob), ("B_wt",)], writes=[("B_ctr",)])
                        P.op("pool", lambda e, hh=hh: e.tensor_tensor(out=acc[hh][:], in0=acc[hh][:], in1=ctr[:], op=ALU.add),
                             reads=[("N_acc", hh), ("B_ctr",)], writes=[("N_acc", hh)])
                    self.cast_step()
                    for hh in range(4):
                        h = 4 * g + hh
                        qa = Q[:, hh, :]
                        qk = ("N_q4", jb, hh)
                        ob, zb = next_oz()
                        blocks = []
                        kbs = [4 * j] + [kb for kb in range(max(0, 4 * j - 4), 4 * j + 4) if kb != 4 * j]
                        for kb in kbs:
                            i_ = kb - 4 * j + 4
                            cols = (128 * max(0, i_ - 4), 128 * (min(3, i_) + 1))
                            s = [(wkT[:, kb * 128:(kb + 1) * 128], qa, [("N_wkT",), qk]),
                                 (ident[:], winm[:, i_, :], [KI, ("N_winm",)])]
                            blocks.append(dict(s=s, v=[wvs[:, kb, :]], vkeys=[("N_wv",)], cols=cols))
                        attn(blocks, 1, [ob], zb)
                        gate_w(2 * 8 + h, jb, zb)
                        P.op("dve", lambda e, ob=ob: e.tensor_tensor(out=ctr[:], in0=ps[ob][:, :], in1=wt[:], op=ALU.mult),
                             reads=[("ps", ob), ("B_wt",)], writes=[("B_ctr",)])
                        oi = next_ostg()
                        P.op("pool", lambda e, hh=hh, oi=oi: e.tensor_tensor(out=ostg[oi][:], in0=acc[hh][:], in1=ctr[:], op=ALU.add),
                             reads=[("N_acc", hh), ("B_ctr",)], writes=[("B_ostg", oi)])
                        P.dma("pool", dr["oT"][8 + h, :, t0:t0 + TS], ostg[oi][:], reads=[("B_ostg", oi)], writes=[("oT", 8 + h, j)])
                    self.cast_step()
                if getattr(self, "dbg", False) and g == 0:
                    P.dma("sp", dr["d_selT"][:, :], selT[:], reads=[("N_selT", q) for q in range(NB)])
        P.barrier()

        with contextlib.ExitStack() as st:
            causal = self.sbuf(st, "F_causal", [128, 4, 512], BF16)
            P.dma("sp", causal[:], dr["causal"][:], writes=[("F_causal",)])
            kT = [self.sbuf(st, f"F_kT{i}", [128, T], BF16) for i in range(2)]
            vs = [self.sbuf(st, f"F_v{i}", [128, NB, 128], BF16) for i in range(2)]
            q6 = [self.sbuf(st, f"F_q6{i}", [6, T], BF16) for i in range(2)]
            k6 = [self.sbuf(st, f"F_k6{i}", [6, T], BF16) for i in range(2)]
            qt = [self.sbuf(st, f"F_q{i}", [128, 512], BF16) for i in range(2)]
            qc = 0
            for h in range(8):
                hb = h % 2
                P.dma("sp", kT[hb][:], dr["kfT"][h, :, :], reads=[("kfT", h, j) for j in range(NT)], writes=[("F_kT", hb)])
                P.dma("sp", vs[hb][:], dr["vf"][:, h * 128:(h + 1) * 128].rearrange("(b p) d -> p b d", p=128),
                      reads=[("vf", r, h // 4) for r in range(NB)], writes=[("F_v", hb)])
                P.op("pool", lambda e, hb=hb: e.memset(q6[hb][:], 1.0), writes=[("F_q6", hb)])
                P.op("pool", lambda e, hb=hb: e.memset(k6[hb][:], 1.0), writes=[("F_k6", hb)])
                P.dma("sp", q6[hb][0:3, :], dr["cs3"][h, 0:3, :], reads=[("cs3",)], writes=[("F_q6", hb)])
                P.dma("sp", k6[hb][3:6, :], dr["cs3"][h, 3:6, :], reads=[("cs3",)], writes=[("F_k6", hb)])
                for j in range(NT):
                    t0 = j * TS
                    qb_ = qc % 2; qc += 1
                    P.dma("sp", qt[qb_][:], dr["qfT"][h, :, t0:t0 + TS], reads=[("qfT", h, j)], writes=[("F_q", qb_)])
                    ob, zb = next_oz()
                    blocks = []
                    for kb in range(4 * j + 4):
                        s = [(kT[hb][:, kb * 128:(kb + 1) * 128], qt[qb_][:], [("F_kT", hb), ("F_q", qb_)]),
                             (k6[hb][:, kb * 128:(kb + 1) * 128], q6[hb][:, t0:t0 + TS], [("F_k6", hb), ("F_q6", hb)])]
                        cols = (0, 512)
                        if kb >= 4 * j:
                            r_ = kb - 4 * j
                            cols = (128 * r_, 512)
                            s.append((ident[:], causal[:, r_, :], [KI, ("F_causal",)], (128 * r_, 128 * r_ + 128)))
                        blocks.append(dict(s=s, v=[vs[hb][:, kb, :]], vkeys=[("F_v", hb)], cols=cols))
                    attn(blocks, 1, [ob], zb)
                    P.op("dve", lambda e, zb=zb: e.reciprocal(out=rz[:], in_=ps[zb][:, :]), reads=[("ps", zb)], writes=[("B_rz",)])
                    oi = next_ostg()
                    P.op("dve", lambda e, ob=ob, oi=oi: e.tensor_tensor(out=ostg[oi][:], in0=ps[ob][:, :], in1=rz[:], op=ALU.mult),
                         reads=[("ps", ob), ("B_rz",)], writes=[("B_ostg", oi)])
                    P.dma("pool", dr["oT"][h, :, t0:t0 + TS], ostg[oi][:], reads=[("B_ostg", oi)], writes=[("oT", h, j)])
                    self.cast_step()
        P.barrier()

        with contextlib.ExitStack() as st:
            mq = [self.sbuf(st, f"M_q{i}", [128, 2, 512], BF16) for i in range(2)]
            qc = 0
            for h in range(4):
                for j in range(NT):
                    t0 = j * TS
                    qb_ = qc % 2; qc += 1
                    for c in range(2):
                        P.dma("sp", mq[qb_][:, c, :], dr["mqT"][h * 2 + c, :, t0:t0 + TS], reads=[("mqT", h * 2 + c, j)], writes=[("M_q", qb_, c)])
                    blocks = []
                    for mt in range(2):
                        s = [(mkT[:, h * 2 + c, mt * 128:(mt + 1) * 128], mq[qb_][:, c, :], [("B_mkT",), ("M_q", qb_, c)]) for c in range(2)]
                        blocks.append(dict(s=s, v=[mv[:, mt, h * 256 + c * 128:h * 256 + (c + 1) * 128] for c in range(2)], vkeys=[("B_mv",)]))
                    attn(blocks, 2, [2, 3], 4)
                    P.op("dve", lambda e: e.reciprocal(out=rz[:], in_=ps[4][:, :]), reads=[("ps", 4)], writes=[("B_rz",)])
                    for c in range(2):
                        oi = next_ostg()
                        P.op("dve", lambda e, c=c, oi=oi: e.tensor_tensor(out=ostg[oi][:], in0=ps[2 + c][:, :], in1=rz[:], op=ALU.mult),
                             reads=[("ps", 2 + c), ("B_rz",)], writes=[("B_ostg", oi)])
                        P.dma("pool", dr["oT"][16 + h * 2 + c, :, t0:t0 + TS], ostg[oi][:], reads=[("B_ostg", oi)], writes=[("oT", 16 + h * 2 + c, j)])
                    self.cast_step()
    P.barrier()


Builder.phase_B = _phase_B


def _phase_C(self, l, src_name, dst_name):
    nc, P, dr, sb = self.nc, self.P, self.dr, self.sb
    vb = l * VPL
    ps = self.ps
    vec = sb["vecs"]
    with contextlib.ExitStack() as st:
        oTt = self.sbuf(st, "C_oT", [128, 24, TS], BF16)
        NG = 4
        gts = [self.sbuf(st, f"C_g{i}", [128, TS], BF16) for i in range(NG)]
        NWB = 4
        Wb = [self.sbuf(st, f"C_W{i}", [128, NCH, 512], BF16) for i in range(NWB)]
        mrg = self.sbuf(st, "C_mrg", [128, NCH, TS], BF16)
        y = self.sbuf(st, "C_y", [128, NCH, TS], F32)
        h = self.sbuf(st, "C_h", [128, NCH, TS], F32)
        ag = self.sbuf(st, "C_ag", [128, NCH, TS], BF16)
        tmp = [self.sbuf(st, f"C_t{i}", [128, TS], F32) for i in range(3)]
        rl = [self.sbuf(st, f"C_rl{i}", [128, TS], BF16) for i in range(2)]
        rstd = self.sbuf(st, "C_rstd", [128, TS], F32)
        ntmp = self.sbuf(st, "C_ntmp", [128, TS], F32)
        wi = [0]
        gi = [0]

        def loadW(dn, idx, nk):
            b = wi[0] % NWB; wi[0] += 1
            P.dma("sp", Wb[b][:, 0:nk, :], dr[dn][idx], reads=[("wc", dn, idx)], writes=[("C_W", b)])
            return b

        def norm_apply(srcbuf, srckey, gcol, dstfn):
            self.rmsnorm_stats(srcbuf, srckey, ag, rstd, ntmp)

        for j in range(NT):
            t0 = j * TS
            P.dma("sp", oTt[:], dr["oT"][:, :, t0:t0 + TS].rearrange("c p t -> p c t"),
                  reads=[("oT", c, j) for c in range(24)], writes=[("C_oT",)])
            for cg in range(4):
                bs = []
                for br, wn in enumerate(("w_up_fox", "w_up_nsa", "w_up_mem")):
                    bs.append(loadW("wcu", br * 4 + cg, 8))
                for cbl in range(4):
                    cb = cg * 4 + cbl
                    gb = []
                    for br in range(3):
                        k = gi[0] % NG; gi[0] += 1
                        P.dma("sp", gts[k][:], dr["bgT"][br * 16 + cb, :, t0:t0 + TS], reads=[("bgT", br * 16 + cb, j)], writes=[("C_g", k)])
                        gb.append(k)
                    pis = []
                    for br in range(3):
                        pi = self.nextps(); pis.append(pi)
                        for k in range(8):
                            P.op("pe", lambda e, pi=pi, br=br, k=k, cbl=cbl, b=bs[br]: e.matmul(
                                ps[pi][:, :], lhsT=Wb[b][:, k, cbl * 128:(cbl + 1) * 128], rhs=oTt[:, br * 8 + k, :],
                                start=(k == 0), stop=(k == 7)),
                                reads=[("C_W", bs[br]), ("C_oT",)], writes=[("ps", pi)])
                    for br in range(3):
                        P.op("dve", lambda e, br=br, pi=pis[br], k=gb[br]: e.tensor_tensor(out=tmp[br][:], in0=ps[pi][:, :], in1=gts[k][:], op=ALU.mult),
                             reads=[("ps", pis[br]), ("C_g", gb[br])], writes=[("C_t", br)])
                    P.op("pool", lambda e: e.tensor_tensor(out=tmp[0][:], in0=tmp[0][:], in1=tmp[1][:], op=ALU.add),
                         reads=[("C_t", 0), ("C_t", 1)], writes=[("C_t", 0)])
                    P.op("pool", lambda e, cb=cb: e.tensor_tensor(out=mrg[:, cb, :], in0=tmp[0][:], in1=tmp[2][:], op=ALU.add),
                         reads=[("C_t", 0), ("C_t", 2)], writes=[("C_mrg", cb)])
            MK = [("C_mrg", c) for c in range(NCH)]
            for cg in range(4):
                b = loadW("wcb", cg, NCH)
                for cbl in range(4):
                    cb = cg * 4 + cbl
                    pi = self.nextps()
                    for k in range(NCH):
                        P.op("pe", lambda e, pi=pi, k=k, cbl=cbl, b=b: e.matmul(
                            ps[pi][:, :], lhsT=Wb[b][:, k, cbl * 128:(cbl + 1) * 128], rhs=mrg[:, k, :],
                            start=(k == 0), stop=(k == NCH - 1)),
                            reads=[("C_W", b)] + MK, writes=[("ps", pi)])
                    P.op("act", lambda e, pi=pi, cb=cb: e.activation(out=y[:, cb, :], in_=ps[pi][:, :], func=AF.Copy),
                         reads=[("ps", pi)], writes=[("C_y", cb)])
            YK = [("C_y", c) for c in range(NCH)]
            P.dma("sp", h[:], dr[src_name][:, :, t0:t0 + TS].rearrange("c p t -> p c t"),
                  reads=[(src_name, j)], writes=[("C_h",)])
            P.op("act", lambda e: e.activation(out=ag[:], in_=y[:], func=AF.Square), reads=YK, writes=[("sq",)] + [("C_ag", c) for c in range(NCH)])
            self._stats_from_sq(ag, rstd, ntmp)
            for cb in range(NCH):
                tb = cb % 2
                P.op("dve", lambda e, cb=cb, tb=tb: e.scalar_tensor_tensor(
                    out=tmp[tb][:], in0=y[:, cb, :], scalar=vec[:, vb + VG_POST_MIX + cb:vb + VG_POST_MIX + cb + 1], in1=rstd[:],
                    op0=ALU.mult, op1=ALU.mult),
                    reads=[("C_y", cb), ("rstd",), ("c", "vecs")], writes=[("C_t", tb)])
                P.op("pool", lambda e, cb=cb, tb=tb: e.tensor_tensor(out=h[:, cb, :], in0=h[:, cb, :], in1=tmp[tb][:], op=ALU.add),
                     reads=[("C_h",), ("C_t", tb)], writes=[("C_h",)])
            P.op("act", lambda e: e.activation(out=ag[:], in_=h[:], func=AF.Square), reads=[("C_h",)], writes=[("sq",)] + [("C_ag", c) for c in range(NCH)])
            self._stats_from_sq(ag, rstd, ntmp)
            for cb in range(NCH):
                P.op("dve", lambda e, cb=cb: e.scalar_tensor_tensor(
                    out=mrg[:, cb, :], in0=h[:, cb, :], scalar=vec[:, vb + VG_PRE_MLP + cb:vb + VG_PRE_MLP + cb + 1], in1=rstd[:],
                    op0=ALU.mult, op1=ALU.mult),
                    reads=[("C_h",), ("rstd",), ("c", "vecs")], writes=[("C_mrg", cb)])
            for g in range(4):
                for wt_ in range(4):
                    b = loadW("wcb", 4 + g * 4 + wt_, NCH)
                    for cbl in range(4):
                        pi = self.nextps()
                        for k in range(NCH):
                            P.op("pe", lambda e, pi=pi, k=k, cbl=cbl, b=b: e.matmul(
                                ps[pi][:, :], lhsT=Wb[b][:, k, cbl * 128:(cbl + 1) * 128], rhs=mrg[:, k, :],
                                start=(k == 0), stop=(k == NCH - 1)),
                                reads=[("C_W", b)] + MK, writes=[("ps", pi)])
                        ri = (wt_ * 4 + cbl) % 2
                        P.op("act", lambda e, pi=pi, ri=ri: e.activation(out=rl[ri][:], in_=ps[pi][:, :], func=AF.Relu),
                             reads=[("ps", pi)], writes=[("C_rl", ri)])
                        ac = wt_ * 4 + cbl
                        P.op("pool", lambda e, ri=ri, ac=ac: e.tensor_tensor(out=ag[:, ac, :], in0=rl[ri][:], in1=rl[ri][:], op=ALU.mult),
                             reads=[("C_rl", ri)], writes=[("C_ag", ac)])
                AK = [("C_ag", c) for c in range(NCH)]
                for cg in range(4):
                    b = loadW("wcb", 20 + g * 4 + cg, NCH)
                    for cbl in range(4):
                        cb = cg * 4 + cbl
                        pi = self.nextps()
                        for k in range(NCH):
                            P.op("pe", lambda e, pi=pi, k=k, cbl=cbl, b=b: e.matmul(
                                ps[pi][:, :], lhsT=Wb[b][:, k, cbl * 128:(cbl + 1) * 128], rhs=ag[:, k, :],
                                start=(k == 0), stop=(k == NCH - 1)),
                                reads=[("C_W", b)] + AK, writes=[("ps", pi)])
                        if g == 0:
                            P.op("act", lambda e, pi=pi, cb=cb: e.activation(out=y[:, cb, :], in_=ps[pi][:, :], func=AF.Copy),
                                 reads=[("ps", pi)], writes=[("C_y", cb)])
                        else:
                            P.op("dve", lambda e, pi=pi, cb=cb: e.tensor_tensor(out=y[:, cb, :], in0=ps[pi][:, :], in1=y[:, cb, :], op=ALU.add),
                                 reads=[("ps", pi), ("C_y", cb)], writes=[("C_y", cb)])
            P.op("act", lambda e: e.activation(out=ag[:], in_=y[:], func=AF.Square), reads=YK, writes=[("sq",)] + AK)
            self._stats_from_sq(ag, rstd, ntmp)
            for cb in range(NCH):
                tb = cb % 2
                P.op("dve", lambda e, cb=cb, tb=tb: e.scalar_tensor_tensor(
                    out=tmp[tb][:], in0=y[:, cb, :], scalar=vec[:, vb + VG_POST_MLP + cb:vb + VG_POST_MLP + cb + 1], in1=rstd[:],
                    op0=ALU.mult, op1=ALU.mult),
                    reads=[("C_y", cb), ("rstd",), ("c", "vecs")], writes=[("C_t", tb)])
                P.op("pool", lambda e, cb=cb, tb=tb: e.tensor_tensor(out=h[:, cb, :], in0=h[:, cb, :], in1=tmp[tb][:], op=ALU.add),
                     reads=[("C_h",), ("C_t", tb)], writes=[("C_h",)])
            d = P.dma("act", dr[dst_name][:, :, t0:t0 + TS].rearrange("c p t -> p c t"), h[:],
                      reads=[("C_h",)], writes=[(dst_name, j)])
            if dst_name == "outT":
                self.final.append(d)
    P.barrier()


def _stats_from_sq(self, sq, rstd, tmp, nfree=512):
    P = self.P
    pi = self.nextps()
    ps = self.ps[pi]
    for c in range(NCH):
        P.op("pe", lambda e, c=c: e.matmul(ps[:, 0:nfree], lhsT=self.sb["ones"][:], rhs=sq[:, c, 0:nfree],
                                           start=(c == 0), stop=(c == NCH - 1)),
             reads=[("sq",), ("c", "ones")], writes=[("ps", pi)])
    P.op("dve", lambda e: e.tensor_scalar(out=tmp[:, 0:nfree], in0=ps[:, 0:nfree], scalar1=1.0 / D, scalar2=EPS,
                                          op0=ALU.mult, op1=ALU.add),
         reads=[("ps", pi)], writes=[("nt",)])
    P.op("act", lambda e: e.activation(out=tmp[:, 0:nfree], in_=tmp[:, 0:nfree], func=AF.Sqrt),
         reads=[("nt",)], writes=[("nt",)])
    P.op("dve", lambda e: e.reciprocal(out=rstd[:, 0:nfree], in_=tmp[:, 0:nfree]),
         reads=[("nt",)], writes=[("rstd",)])


Builder.phase_C = _phase_C
Builder._stats_from_sq = _stats_from_sq


from concourse.bass_utils import run_bass_kernel_spmd

N_ACTIVE = 4


def build_fused():
    B = Builder()
    src = "xT"
    for l in range(NL):
        B.phase_A(l, src)
        B.phase_B(l)
        dst = "outT" if l == NL - 1 else "hT"
        B.phase_C(l, src, dst)
        src = dst
    B.P.emit(final_waits=B.final)
    return B


def kernel(**inputs):
    inp = {k: np.asarray(v) for k, v in inputs.items()}
    consts = host_consts()
    consts["vecs"] = host_vecs(inp)
    B = build_fused()
    in_maps = []
    for b in range(N_ACTIVE):
        m = {"xT": np.ascontiguousarray(inp["x"][b].T.reshape(NCH, 128, T)).astype(np.float32),
             "memT": np.ascontiguousarray(inp["mem"][b].T.reshape(NCH, 128, 256)).astype(np.float32)}
        m.update(consts)
        for k in WEIGHT_SPECS:
            m[k] = np.ascontiguousarray(inp[k], dtype=np.float32)
        in_maps.append(m)
    res = run_bass_kernel_spmd(B.nc, in_maps, core_ids=list(range(N_ACTIVE)))
    out = np.empty((N_ACTIVE, T, D), np.float32)
    for b in range(N_ACTIVE):
        oT = np.asarray(res.results[b]["outT"], dtype=np.float32)
        out[b] = oT.reshape(D, T).T
    return out
```

```python
import contextlib
import numpy as np
import ml_dtypes
import concourse.bass as bass
import concourse.mybir as mybir

F32 = mybir.dt.float32
BF16 = mybir.dt.bfloat16
AF = mybir.ActivationFunctionType
ALU = mybir.AluOpType
NPBF = ml_dtypes.bfloat16

ENGS = ["pe", "act", "dve", "pool", "sp"]
EPOCH = 20000
NDSEM = {"sp": 28, "pool": 20, "act": 8}


class Op:
    __slots__ = ("eng", "fn", "deps", "dma", "sig", "sigord", "dsem", "dval", "prev")

    def __init__(self, eng, fn, deps, dma):
        self.eng = eng
        self.fn = fn
        self.deps = deps
        self.dma = dma
        self.sig = False
        self.sigord = 0
        self.dsem = None
        self.dval = 0
        self.prev = None


class Prog:
    def __init__(self, nc):
        self.nc = nc
        self.ops = {e: [] for e in ENGS}
        self.last_w = {}
        self.rd_c = {}
        self.rd_d = {}
        self.stack = contextlib.ExitStack()
        self.dma_since = []

    def op(self, eng, fn, reads=(), writes=(), dma=False):
        ops = self.ops[eng]
        idx = len(ops)
        deps = set()
        for k in reads:
            w = self.last_w.get(k)
            if w is not None:
                deps.add(w)
        for k in writes:
            w = self.last_w.get(k)
            if w is not None:
                deps.add(w)
            rc = self.rd_c.get(k)
            if rc:
                for e, i in rc.items():
                    deps.add((e, i))
            rd = self.rd_d.get(k)
            if rd:
                deps.update(rd)
        ops.append(Op(eng, fn, deps, dma))
        me = (eng, idx)
        if dma:
            self.dma_since.append(me)
        for k in reads:
            if dma:
                self.rd_d.setdefault(k, []).append(me)
            else:
                self.rd_c.setdefault(k, {})[eng] = idx
        for k in writes:
            self.last_w[k] = me
            self.rd_c[k] = {}
            self.rd_d[k] = []
        return me

    def dma(self, eng, out, in_, reads=(), writes=()):
        return self.op(eng, lambda e: e.dma_start(out=out, in_=in_), reads, writes, dma=True)

    def barrier(self):
        deps = set(self.dma_since)
        for e in ENGS:
            for i in range(len(self.ops[e]) - 1, -1, -1):
                if not self.ops[e][i].dma:
                    deps.add((e, i))
                    break
        for e in ENGS:
            self.ops[e].append(Op(e, lambda eng: eng.nop(), set(deps), False))
        self.dma_since = []

    def emit(self, final_waits=()):
        nc = self.nc
        ops = self.ops
        for e in ENGS:
            for o in ops[e]:
                for (de, di) in o.deps:
                    d = ops[de][di]
                    if not d.dma:
                        if de == "pe" and e == "pe":
                            continue
                        d.sig = True
        for (de, di) in final_waits:
            if not ops[de][di].dma:
                ops[de][di].sig = True
        nsig = {}
        for e in ENGS:
            c = 0
            for o in ops[e]:
                if o.dma:
                    continue
                if o.sig:
                    c += 1
                    o.sigord = c
            nsig[e] = c
        st = self.stack
        csem = {}
        for e in ENGS:
            n = (nsig[e] + EPOCH - 1) // EPOCH
            csem[e] = [st.enter_context(nc.semaphore(f"c_{e}_{i}")) for i in range(max(n, 1))]
        dsem = {}
        for e in ("sp", "pool", "act"):
            dsem[e] = [st.enter_context(nc.semaphore(f"d_{e}_{i}")) for i in range(NDSEM[e])]
        for e in ("sp", "pool", "act"):
            cnt = [0] * NDSEM[e]
            k = 0
            for o in ops[e]:
                if o.dma:
                    s = k % NDSEM[e]
                    k += 1
                    cnt[s] += 1
                    o.dsem = dsem[e][s]
                    o.dval = 16 * cnt[s]
        for e in ("pe", "dve"):
            for o in ops[e]:
                assert not o.dma

        def target(de, di):
            d = ops[de][di]
            if d.dma:
                return d.dsem, d.dval
            so = d.sigord
            return csem[de][(so - 1) // EPOCH], (so - 1) % EPOCH + 1

        engobj = {"pe": "tensor", "act": "scalar", "dve": "vector", "pool": "gpsimd", "sp": "sync"}
        stats = {}
        with nc.Block() as block:
            def body(ename):
                def run(eng):
                    seen = {}
                    nw = 0
                    for idx, o in enumerate(ops[ename]):
                        waits = []
                        for (de, di) in o.deps:
                            if de == "pe" and ename == "pe":
                                continue
                            waits.append(target(de, di))
                        if o.dma and o.dval > 16:
                            waits.append((o.dsem, o.dval - 16))
                        for (s, v) in waits:
                            key = id(s)
                            if seen.get(key, 0) >= v:
                                continue
                            seen[key] = v
                            eng.wait_ge(s, v)
                            nw += 1
                        ins = o.fn(eng)
                        if o.dma:
                            ins.then_inc(o.dsem, 16)
                        elif o.sig:
                            so = o.sigord
                            ins.then_inc(csem[ename][(so - 1) // EPOCH], 1)
                    if ename == "sp":
                        for (de, di) in final_waits:
                            s, v = target(de, di)
                            eng.wait_ge(s, v)
                    stats[ename] = (len(ops[ename]), nw)
                return run
            block.tensor(body("pe"))
            block.scalar(body("act"))
            block.vector(body("dve"))
            block.gpsimd(body("pool"))
            block.sync(body("sp"))
        return stats


import math

D = 2048
T = 4096
NCH = 16
DIN = 12832
NL = 2
BIG = 30000.0
EPS = 1e-6
TS = 512
NT = T // TS
NB = T // 128

SEG = {}
_o = 0
for _n, _w in [("fq", 1024), ("fk", 1024), ("fv", 1024), ("ff", 8), ("nq", 1024), ("ck", 256), ("cv", 256),
               ("sk", 256), ("sv", 256), ("wk", 256), ("wv", 256), ("ng", 24), ("mq", 1024), ("bg", 6144)]:
    SEG[_n] = (_o, _w)
    _o += _w
assert _o == DIN

VG_PRE_MIX, VG_POST_MIX, VG_PRE_MLP, VG_POST_MLP, VG_MEM, V_NEGBF = 0, 16, 32, 48, 64, 80
VPL = 81


def host_consts():
    c = {}
    half = 16
    inv = (500000.0 ** (-np.arange(half, dtype=np.float32) / half)).astype(np.float32)
    ang = np.arange(T, dtype=np.float32)[:, None] * inv[None, :]
    cos, sin = np.cos(ang).astype(np.float32), np.sin(ang).astype(np.float32)
    cs = np.zeros((2, 32, T), np.float32)
    cs[0, :16] = cos.T; cs[0, 16:] = cos.T
    cs[1, :16] = sin.T; cs[1, 16:] = sin.T
    c["ropecs"] = cs
    pm = np.zeros((32, 32), np.float32)
    for m in range(16):
        pm[m + 16, m] = -1.0
        pm[m, m + 16] = 1.0
    c["pm"] = pm.astype(NPBF)
    c["ident"] = np.eye(128, dtype=np.float32).astype(NPBF)
    c["ones"] = np.ones((128, 128), np.float32).astype(NPBF)
    k = np.arange(128)[:, None]
    q = np.arange(512)[None, :]
    cz = np.zeros((128, 4, 512), np.float32)
    for r in range(4):
        cz[:, r, :] = np.where(r * 128 + k <= q, 0.0, -BIG)
    c["causal"] = cz.astype(NPBF)
    wz = np.zeros((128, 8, 512), np.float32)
    for i in range(8):
        rel = q - ((i - 4) * 128 + k)
        wz[:, i, :] = np.where((rel >= 0) & (rel < 512), 0.0, -BIG)
    c["winmask"] = wz.astype(NPBF)
    n = np.arange(256)
    tt = np.arange(T)
    valid = (n[:, None] * 16 + 31 <= tt[None, :]) & (n[:, None] < 255)
    cm = np.where(valid, 0.0, -BIG).astype(np.float32)
    cm = cm.reshape(2, 128, NT, 512).transpose(1, 2, 0, 3)
    c["cmpmask"] = np.ascontiguousarray(cm).astype(NPBF)
    ci = n[:, None] * 16
    sj = np.arange(64)[None, :] * 64
    ov = ((ci <= sj + 63) & (ci + 31 >= sj) & (n[:, None] < 255)).astype(np.float32)
    ovo = np.concatenate([ov, np.ones((256, 1), np.float32)], axis=1)
    ovo[255, :] = 0.0
    c["ov"] = np.ascontiguousarray(ovo.reshape(2, 128, 65).transpose(1, 0, 2)).astype(NPBF)
    a = np.zeros((64, 32, 128), np.float32)
    for kb in range(32):
        a[2 * kb, kb, 0:64] = 1.0
        a[2 * kb + 1, kb, 64:128] = 1.0
    c["asel"] = a.astype(NPBF)
    blk = np.arange(64)[None, :]
    cur = (tt // 64)[:, None]
    sel_valid = blk <= cur
    forced = (blk == 0) | (blk == cur) | (blk == cur - 1)
    vt = (sel_valid & ~forced).astype(np.float32)
    ct = np.where(forced, 1e6, np.where(sel_valid, 0.0, -1.0)).astype(np.float32)
    def tm(x):
        return np.ascontiguousarray(x.reshape(32, 128, 64).transpose(1, 0, 2)).astype(np.float32)
    c["seltab"] = np.stack([tm(vt), tm(ct), tm(sel_valid.astype(np.float32))], axis=1)
    sg = np.zeros((24, 24, 128), np.float32)
    for r in range(24):
        sg[r, r, :] = 1.0
    c["selg"] = sg.astype(NPBF)
    return c


def host_vecs(inp):
    v = np.zeros((128, NL * VPL), np.float32)
    for l in range(NL):
        b = l * VPL
        for nm, off in [("g_pre_mix", VG_PRE_MIX), ("g_post_mix", VG_POST_MIX), ("g_pre_mlp", VG_PRE_MLP),
                        ("g_post_mlp", VG_POST_MLP), ("g_mem", VG_MEM)]:
            v[:, b + off:b + off + 16] = np.asarray(inp[nm][l], np.float32).reshape(16, 128).T
        v[:8, b + V_NEGBF] = np.asarray(inp["b_f"][l], np.float32)
    return v


CONST_SPECS = {
    "ropecs": ([2, 32, T], F32), "pm": ([32, 32], BF16), "ident": ([128, 128], BF16), "ones": ([128, 128], BF16),
    "causal": ([128, 4, 512], BF16), "winmask": ([128, 8, 512], BF16), "cmpmask": ([128, NT, 2, 512], BF16),
    "ov": ([128, 2, 65], BF16), "asel": ([64, 32, 128], BF16), "seltab": ([128, 3, 32, 64], F32),
    "selg": ([24, 24, 128], BF16), "vecs": ([128, NL * VPL], F32),
}
WEIGHT_SPECS = {
    "w_in": [NL, D, DIN], "w_cmp1_k": [NL, 4096, 128], "w_cmp2_k": [NL, 128, 128], "pe_cmp_k": [NL, 32, 128],
    "w_cmp1_v": [NL, 4096, 128], "w_cmp2_v": [NL, 128, 128], "pe_cmp_v": [NL, 32, 128],
    "w_mem_kv": [NL, D, 2048], "w_up_fox": [NL, 1024, D], "w_up_nsa": [NL, 1024, D], "w_up_mem": [NL, 1024, D],
    "w_o": [NL, D, D], "w_mlp1": [NL, D, 8192], "w_mlp2": [NL, 8192, D],
}
SCRATCH = {
    "hT": ([NCH, 128, T], F32),
    "qfT": ([8, 128, T], BF16), "kfT": ([8, 128, T], BF16), "vf": ([T, 1024], BF16), "logf": ([8, T], F32),
    "qnT": ([8, 128, T], BF16), "ckT": ([2, 128, T], BF16), "cvT": ([2, 128, T], BF16),
    "skT": ([2, 128, T], BF16), "sv": ([T, 256], BF16), "wkT": ([2, 128, T], BF16), "wv": ([T, 256], BF16),
    "gnT": ([24, T], BF16), "mqT": ([8, 128, T], BF16), "bgT": ([48, 128, T], BF16),
    "oT": ([24, 128, T], BF16), "cs3": ([8, 6, T], BF16),
    "wcu": ([12, 128, 8, 512], BF16), "wcb": ([36, 128, 16, 512], BF16),
}


DBG = {"d_acc": ([3, 128, 512], F32), "d_pslc": ([3, 128, 4, 64], F32), "d_gn": ([3, 24, 512], BF16),
       "d_cmk": ([3, 128, 2, 512], BF16), "d_q4": ([3, 128, 4, 512], BF16), "d_stb": ([3, 128, 3, 4, 64], F32),
       "d_selT": ([64, T], BF16)}


class Builder:
    def __init__(self, ext_in=(), ext_out=(), dbg=False):
        self.nc = nc = bass.Bass("TRN2", target_bir_lowering=False)
        self.P = Prog(nc)
        self.dr = {}
        self.dr["xT"] = nc.dram_tensor("xT", [NCH, 128, T], F32, kind="ExternalInput").ap()
        self.dr["memT"] = nc.dram_tensor("memT", [NCH, 128, 256], F32, kind="ExternalInput").ap()
        for k, (shp, dt) in CONST_SPECS.items():
            self.dr[k] = nc.dram_tensor(k, shp, dt, kind="ExternalInput").ap()
        for k, shp in WEIGHT_SPECS.items():
            self.dr[k] = nc.dram_tensor(k, shp, F32, kind="ExternalInput").ap()
        for k, (shp, dt) in SCRATCH.items():
            kind = "Internal"
            if k in ext_in:
                kind = "ExternalInput"
            elif k in ext_out:
                kind = "ExternalOutput"
            self.dr[k] = nc.dram_tensor(k, shp, dt, kind=kind).ap()
        self.dr["outT"] = nc.dram_tensor("outT", [NCH, 128, T], F32, kind="ExternalOutput").ap()
        self.dbg = dbg
        if dbg:
            for k, (shp, dt) in DBG.items():
                self.dr[k] = nc.dram_tensor(k, shp, dt, kind="ExternalOutput").ap()
        self.st = self.P.stack
        self.final = []
        self.sb = {}
        for k in ("pm", "ident", "ones", "vecs", "selg"):
            shp, dt = CONST_SPECS[k]
            self.sb[k] = self.st.enter_context(nc.sbuf_tensor("c_" + k, shp, dt))
            src = self.dr[k]
            self.P.dma("sp", self.sb[k][:], src[:], writes=[("c", k)])
        self.ps = [self.st.enter_context(nc.psum_tensor(f"ps{i}", [128, 512], F32)) for i in range(7)]
        self.psb = self.st.enter_context(nc.psum_tensor("psb", [128, 1024], BF16))
        self.psi = 0
        self._uid = 0

    def uid(self):
        self._uid += 1
        return self._uid

    def sbuf(self, stack, name, shape, dt):
        return stack.enter_context(self.nc.sbuf_tensor(f"{name}_{self.uid()}", shape, dt))

    def cast_begin(self, l, stage):
        dr = self.dr
        jobs = []
        for br, wn in enumerate(("w_up_fox", "w_up_nsa", "w_up_mem")):
            for cg in range(4):
                jobs.append((dr[wn][l, :, cg * 512:(cg + 1) * 512].rearrange("(c p) n -> p c n", p=128), "wcu", br * 4 + cg, 8))
        for cg in range(4):
            jobs.append((dr["w_o"][l, :, cg * 512:(cg + 1) * 512].rearrange("(c p) n -> p c n", p=128), "wcb", cg, NCH))
        for i in range(16):
            jobs.append((dr["w_mlp1"][l, :, i * 512:(i + 1) * 512].rearrange("(c p) n -> p c n", p=128), "wcb", 4 + i, NCH))
        for g in range(4):
            for cg in range(4):
                jobs.append((dr["w_mlp2"][l, g * 2048:(g + 1) * 2048, cg * 512:(cg + 1) * 512].rearrange("(c p) n -> p c n", p=128),
                             "wcb", 20 + g * 4 + cg, NCH))
        self.cjobs = jobs
        self.cstage = stage
        self.cpos = 0

    def cast_step(self):
        P = self.P
        i = self.cpos
        n = len(self.cjobs)
        if i > n:
            return
        if i < n:
            src, dn, idx, nk = self.cjobs[i]
            sb_ = self.cstage[i % 2]
            P.dma("pool", sb_[:, 0:nk, :], src, writes=[("cstage", i % 2)])
        if i >= 1:
            src, dn, idx, nk = self.cjobs[i - 1]
            sb_ = self.cstage[(i - 1) % 2]
            P.dma("pool", self.dr[dn][idx], sb_[:, 0:nk, :], reads=[("cstage", (i - 1) % 2)], writes=[("wc", dn, idx)])
        self.cpos += 1

    def nextps(self, lo=0, hi=7):
        i = lo + (self.psi % (hi - lo))
        self.psi += 1
        return i

    def rmsnorm_stats(self, src, srckey, sq, rstd, tmp, nfree=512):
        P = self.P
        pi = self.nextps()
        ps = self.ps[pi]
        P.op("act", lambda e: e.activation(out=sq[:, :, 0:nfree], in_=src[:, :, 0:nfree], func=AF.Square),
             reads=[srckey], writes=[("sq",)])
        for c in range(NCH):
            P.op("pe", lambda e, c=c: e.matmul(ps[:, 0:nfree], lhsT=self.sb["ones"][:], rhs=sq[:, c, 0:nfree],
                                               start=(c == 0), stop=(c == NCH - 1)),
                 reads=[("sq",), ("c", "ones")], writes=[("ps", pi)])
        P.op("dve", lambda e: e.tensor_scalar(out=tmp[:, 0:nfree], in0=ps[:, 0:nfree], scalar1=1.0 / D, scalar2=EPS,
                                              op0=ALU.mult, op1=ALU.add),
             reads=[("ps", pi)], writes=[("nt",)])
        P.op("act", lambda e: e.activation(out=tmp[:, 0:nfree], in_=tmp[:, 0:nfree], func=AF.Sqrt),
             reads=[("nt",)], writes=[("nt",)])
        P.op("dve", lambda e: e.reciprocal(out=rstd[:, 0:nfree], in_=tmp[:, 0:nfree]),
             reads=[("nt",)], writes=[("rstd",)])

    def phase_A(self, l, src_name):
        nc, P, dr, sb = self.nc, self.P, self.dr, self.sb
        RC = 1024
        NTC = RC // TS
        vb = l * VPL
        with contextlib.ExitStack() as st:
            uT = self.sbuf(st, "A_uT", [128, NCH, RC], BF16)
            hT = self.sbuf(st, "A_hT", [128, NCH, TS], F32)
            sq = self.sbuf(st, "A_sq", [128, NCH, TS], BF16)
            rstd = self.sbuf(st, "A_rstd", [128, TS], F32)
            ntmp = self.sbuf(st, "A_ntmp", [128, TS], F32)
            wb = [self.sbuf(st, f"A_w{i}", [128, NCH, 512], BF16) for i in range(2)]
            NSTG = 6
            stg = [self.sbuf(st, f"A_stg{i}", [128, 512], BF16) for i in range(NSTG)]
            rcs = [self.sbuf(st, f"A_rcs{i}", [32, 2, TS], F32) for i in range(NTC)]
            rt1 = self.sbuf(st, "A_rt1", [32, TS], F32)
            rt2 = self.sbuf(st, "A_rt2", [32, TS], F32)
            ft = self.sbuf(st, "A_ft", [8, TS], F32)
            tiles = []
            for nm in ["fk", "fv", "ff", "ck", "cv", "sk", "sv", "wk", "wv", "fq", "nq", "ng", "mq", "bg"]:
                c0, w = SEG[nm]
                for o in range(0, w, 512):
                    tiles.append((nm, c0 + o, min(512, w - o), o))
            sgi = [0]

            def load_w(i):
                nm, c0, w, o = tiles[i]
                b = i % 2
                P.dma("pool", wb[b][:, :, 0:w],
                      dr["w_in"][l, :, c0:c0 + w].rearrange("(c p) n -> p c n", p=128),
                      writes=[("A_w", b)])

            for ch in range(T // RC):
                t0 = ch * RC
                for tl in range(NTC):
                    ts0 = t0 + tl * TS
                    P.dma("sp", hT[:], dr[src_name][:, :, ts0:ts0 + TS].rearrange("c p t -> p c t"),
                          reads=[(src_name, ts0 // TS)], writes=[("A_hT",)])
                    P.dma("sp", rcs[tl][:], dr["ropecs"][:, :, ts0:ts0 + TS].rearrange("a p t -> p a t"),
                          writes=[("A_rcs", tl)])
                    self.rmsnorm_stats(hT, ("A_hT",), sq, rstd, ntmp)
                    for c in range(NCH):
                        P.op("dve", lambda e, c=c, tl=tl: e.scalar_tensor_tensor(
                            out=uT[:, c, tl * TS:(tl + 1) * TS], in0=hT[:, c, :],
                            scalar=sb["vecs"][:, vb + VG_PRE_MIX + c:vb + VG_PRE_MIX + c + 1], in1=rstd[:],
                            op0=ALU.mult, op1=ALU.mult),
                            reads=[("A_hT",), ("rstd",), ("c", "vecs")], writes=[("A_uT", tl)])
                load_w(0)
                for i, (nm, c0, w, o) in enumerate(tiles):
                    if i + 1 < len(tiles):
                        load_w(i + 1)
                    b = i % 2
                    W = wb[b]
                    tokmajor = nm in ("fv", "sv", "wv")
                    if tokmajor:
                        dst = {"fv": "vf", "sv": "sv", "wv": "wv"}[nm]
                        for tb in range(RC // 128):
                            pi = self.nextps(); ps = self.ps[pi]
                            for c in range(NCH):
                                P.op("pe", lambda e, c=c, tb=tb, ps=ps, W=W, w=w: e.matmul(
                                    ps[:, 0:w], lhsT=uT[:, c, tb * 128:(tb + 1) * 128], rhs=W[:, c, 0:w],
                                    start=(c == 0), stop=(c == NCH - 1)),
                                    reads=[("A_uT", tb // 4), ("A_w", b)], writes=[("ps", pi)])
                            si = sgi[0] % NSTG; sgi[0] += 1
                            S = stg[si]
                            P.op("act", lambda e, S=S, ps=ps, w=w: e.activation(out=S[:, 0:w], in_=ps[:, 0:w], func=AF.Copy),
                                 reads=[("ps", pi)], writes=[("A_stg", si)])
                            r0 = t0 + tb * 128
                            P.dma("sp", dr[dst][r0:r0 + 128, o:o + w], S[:, 0:w],
                                  reads=[("A_stg", si)], writes=[(dst, r0 // 128, o // 512)])
                        continue
                    nblk = (w + 127) // 128
                    for cb in range(nblk):
                        m = min(128, w - cb * 128)
                        for tl in range(NTC):
                            ts0 = t0 + tl * TS
                            tj = ts0 // TS
                            pi = self.nextps(); ps = self.ps[pi]
                            for c in range(NCH):
                                P.op("pe", lambda e, c=c, tl=tl, ps=ps, W=W, cb=cb, m=m: e.matmul(
                                    ps[0:m, :], lhsT=W[:, c, cb * 128:cb * 128 + m], rhs=uT[:, c, tl * TS:(tl + 1) * TS],
                                    start=(c == 0), stop=(c == NCH - 1)),
                                    reads=[("A_uT", tl), ("A_w", b)], writes=[("ps", pi)])
                            gcb = (o // 128) + cb
                            if nm == "ff":
                                P.op("dve", lambda e, ps=ps: e.tensor_scalar(out=ft[:], in0=ps[0:8, :], scalar1=sb["vecs"][0:8, vb + V_NEGBF:vb + V_NEGBF + 1],
                                                                             scalar2=None, op0=ALU.add),
                                     reads=[("ps", pi), ("c", "vecs")], writes=[("A_ft",)])
                                P.op("act", lambda e: e.activation(out=ft[:], in_=ft[:], func=AF.Exp, scale=-1.0),
                                     reads=[("A_ft",)], writes=[("A_ft",)])
                                P.op("act", lambda e: e.activation(out=ft[:], in_=ft[:], func=AF.Ln, bias=1.0),
                                     reads=[("A_ft",)], writes=[("A_ft",)])
                                P.op("dve", lambda e: e.tensor_scalar(out=ft[:], in0=ft[:], scalar1=-1.0, scalar2=None, op0=ALU.mult),
                                     reads=[("A_ft",)], writes=[("A_ft",)])
                                P.dma("sp", dr["logf"][:, ts0:ts0 + TS], ft[:], reads=[("A_ft",)], writes=[("logf", tj)])
                                continue
                            si = sgi[0] % NSTG; sgi[0] += 1
                            S = stg[si]
                            if nm in ("bg", "ng"):
                                P.op("act", lambda e, S=S, ps=ps, m=m: e.activation(out=S[0:m, :], in_=ps[0:m, :], func=AF.Sigmoid),
                                     reads=[("ps", pi)], writes=[("A_stg", si)])
                            else:
                                sc = 1.0
                                if nm in ("fq", "nq"):
                                    sc = 128.0 ** -0.5
                                elif nm == "mq":
                                    sc = 256.0 ** -0.5
                                P.op("act", lambda e, S=S, ps=ps, m=m, sc=sc: e.activation(out=S[0:m, :], in_=ps[0:m, :], func=AF.Copy, scale=sc),
                                     reads=[("ps", pi)], writes=[("A_stg", si)])
                            if nm in ("nq", "ck", "sk", "wk"):
                                pj = self.nextps(); ps2 = self.ps[pj]
                                P.op("pe", lambda e, S=S, ps2=ps2: e.matmul(ps2[0:32, :], lhsT=sb["pm"][:], rhs=S[0:32, :], start=True, stop=True),
                                     reads=[("A_stg", si), ("c", "pm")], writes=[("ps", pj)])
                                P.op("dve", lambda e, S=S, tl=tl: e.tensor_tensor(out=rt1[:], in0=S[0:32, :], in1=rcs[tl][:, 0, :], op=ALU.mult),
                                     reads=[("A_stg", si), ("A_rcs", tl)], writes=[("A_rt1",)])
                                P.op("dve", lambda e, ps2=ps2, tl=tl: e.tensor_tensor(out=rt2[:], in0=ps2[0:32, :], in1=rcs[tl][:, 1, :], op=ALU.mult),
                                     reads=[("ps", pj), ("A_rcs", tl)], writes=[("A_rt2",)])
                                P.op("dve", lambda e, S=S: e.tensor_tensor(out=S[0:32, :], in0=rt1[:], in1=rt2[:], op=ALU.add),
                                     reads=[("A_rt1",), ("A_rt2",)], writes=[("A_stg", si)])
                            dstn = {"fq": "qfT", "fk": "kfT", "nq": "qnT", "ck": "ckT", "cv": "cvT", "sk": "skT", "wk": "wkT",
                                    "ng": "gnT", "mq": "mqT", "bg": "bgT"}[nm]
                            if nm == "ng":
                                P.dma("sp", dr["gnT"][:, ts0:ts0 + TS], S[0:24, :], reads=[("A_stg", si)], writes=[("gnT", tj)])
                            else:
                                P.dma("sp", dr[dstn][gcb, :, ts0:ts0 + TS], S[:, :], reads=[("A_stg", si)], writes=[(dstn, gcb, tj)])
        P.barrier()


def _phase_B(self, l):
    nc, P, dr, sb = self.nc, self.P, self.dr, self.sb
    vb = l * VPL
    ps = self.ps
    ident, ones = sb["ident"], sb["ones"]
    KI, KO = ("c", "ident"), ("c", "ones")

    with contextlib.ExitStack() as stB:
        kcT = [self.sbuf(stB, f"B_kcT{g}", [128, 256], BF16) for g in range(2)]
        vc = [self.sbuf(stB, f"B_vc{g}", [128, 2, 128], BF16) for g in range(2)]
        mkT = self.sbuf(stB, "B_mkT", [128, 8, 256], BF16)
        mv = self.sbuf(stB, "B_mv", [128, 2, 1024], BF16)
        Pt = [self.sbuf(stB, f"B_P{i}", [128, 512], BF16) for i in range(4)]
        zt = self.sbuf(stB, "B_zt", [128, 512], F32)
        rz = self.sbuf(stB, "B_rz", [128, 512], F32)
        wt = self.sbuf(stB, "B_wt", [128, 512], F32)
        ctr = self.sbuf(stB, "B_ctr", [128, 512], F32)
        ostg = [self.sbuf(stB, f"B_ostg{i}", [128, 512], BF16) for i in range(4)]
        cnt = {"p": 0, "o": 0, "s": 0, "oz": 0}
        cstage = [self.sbuf(stB, f"B_cst{i}", [128, NCH, 512], BF16) for i in range(2)]
        self.cast_begin(l, cstage)

        with contextlib.ExitStack() as st:
            lf = self.sbuf(st, "B0_lf", [8, T], F32)
            on8 = self.sbuf(st, "B0_on8", [8, T], F32)
            cc = self.sbuf(st, "B0_c", [8, T], F32)
            cb16 = self.sbuf(st, "B0_cb", [8, 6, T], BF16)
            cf = on8
            P.dma("sp", lf[:], dr["logf"][:, :], reads=[("logf", j) for j in range(NT)], writes=[("B0_lf",)])
            P.op("pool", lambda e: e.memset(on8[:], 1.0), writes=[("B0_on8",)])
            P.op("dve", lambda e: e.tensor_tensor_scan(out=cc[:], data0=on8[:], data1=lf[:], initial=0.0,
                                                       op0=ALU.mult, op1=ALU.add),
                 reads=[("B0_lf",), ("B0_on8",)], writes=[("B0_c",)])
            for i in range(3):
                P.op("dve", lambda e, i=i: e.tensor_copy(out=cb16[:, i, :], in_=cc[:]), reads=[("B0_c",)], writes=[("B0_cb", i)])
                P.op("dve", lambda e, i=i: e.tensor_scalar(out=cb16[:, 3 + i, :], in0=cb16[:, i, :], scalar1=-1.0, scalar2=None, op0=ALU.mult),
                     reads=[("B0_cb", i)], writes=[("B0_cb", 3 + i)])
                if i < 2:
                    P.op("dve", lambda e, i=i: e.tensor_copy(out=cf[:], in_=cb16[:, i, :]), reads=[("B0_cb", i)], writes=[("B0_on8",)])
                    P.op("dve", lambda e: e.tensor_tensor(out=cc[:], in0=cc[:], in1=cf[:], op=ALU.subtract),
                         reads=[("B0_c",), ("B0_on8",)], writes=[("B0_c",)])
            P.dma("sp", dr["cs3"][:, :, :], cb16[:], reads=[("B0_cb", i) for i in range(6)], writes=[("cs3",)])
        P.barrier()
        with contextlib.ExitStack() as st:
            mT = self.sbuf(st, "B0_mT", [128, NCH, 256], F32)
            msq = self.sbuf(st, "B0_msq", [128, NCH, 256], BF16)
            umT = self.sbuf(st, "B0_umT", [128, NCH, 256], BF16)
            wbm = [self.sbuf(st, f"B0_wm{i}", [128, NCH, 512], BF16) for i in range(2)]
            P.dma("sp", mT[:], dr["memT"].rearrange("c p t -> p c t"), writes=[("B0_mT",)])
            self.rmsnorm_stats(mT, ("B0_mT",), msq, rz, zt, nfree=256)
            for c in range(NCH):
                P.op("dve", lambda e, c=c: e.scalar_tensor_tensor(
                    out=umT[:, c, :], in0=mT[:, c, :], scalar=sb["vecs"][:, vb + VG_MEM + c:vb + VG_MEM + c + 1],
                    in1=rz[:, 0:256], op0=ALU.mult, op1=ALU.mult),
                    reads=[("B0_mT",), ("rstd",), ("c", "vecs")], writes=[("B0_umT",)])
            for ti in range(4):
                b = ti % 2
                P.dma("pool", wbm[b][:], dr["w_mem_kv"][l, :, ti * 512:(ti + 1) * 512].rearrange("(c p) n -> p c n", p=128),
                      writes=[("B0_w", b)])
                if ti < 2:
                    for cbk in range(4):
                        pi = self.nextps(); pp = ps[pi]
                        for c in range(NCH):
                            P.op("pe", lambda e, c=c, pp=pp, b=b, cbk=cbk: e.matmul(
                                pp[:, 0:256], lhsT=wbm[b][:, c, cbk * 128:(cbk + 1) * 128], rhs=umT[:, c, :],
                                start=(c == 0), stop=(c == NCH - 1)),
                                reads=[("B0_umT",), ("B0_w", b)], writes=[("ps", pi)])
                        P.op("act", lambda e, pp=pp, ti=ti, cbk=cbk: e.activation(out=mkT[:, ti * 4 + cbk, :], in_=pp[:, 0:256], func=AF.Copy),
                             reads=[("ps", pi)], writes=[("B_mkT",)])
                else:
                    for mt in range(2):
                        pi = self.nextps(); pp = ps[pi]
                        for c in range(NCH):
                            P.op("pe", lambda e, c=c, pp=pp, b=b, mt=mt: e.matmul(
                                pp[:, :], lhsT=umT[:, c, mt * 128:(mt + 1) * 128], rhs=wbm[b][:, c, :],
                                start=(c == 0), stop=(c == NCH - 1)),
                                reads=[("B0_umT",), ("B0_w", b)], writes=[("ps", pi)])
                        P.op("act", lambda e, pp=pp, ti=ti, mt=mt: e.activation(out=mv[:, mt, (ti - 2) * 512:(ti - 1) * 512], in_=pp[:, :], func=AF.Copy),
                             reads=[("ps", pi)], writes=[("B_mv",)])

            xc = self.sbuf(st, "B0_xc", [128, T], BF16)
            w1 = self.sbuf(st, "B0_w1", [128, 32, 128], BF16)
            w2 = self.sbuf(st, "B0_w2", [128, 128], BF16)
            peT = self.sbuf(st, "B0_peT", [128, 32], BF16)
            bsb = self.sbuf(st, "B0_bsb", [128, 1], F32)
            xg = self.sbuf(st, "B0_xg", [128, 256], F32)
            x2 = self.sbuf(st, "B0_x2", [128, 256], F32)
            hd = self.sbuf(st, "B0_hd", [128, 256], BF16)
            P.op("pool", lambda e: e.memset(xg[:], 0.0), writes=[("B0_xg",)])
            for kv in range(2):
                sfx = "k" if kv == 0 else "v"
                P.dma("pool", w1[:], dr["w_cmp1_" + sfx][l].rearrange("(j d) o -> d j o", d=128), writes=[("B0_w1",)])
                P.dma("pool", w2[:], dr["w_cmp2_" + sfx][l], writes=[("B0_w2",)])
                P.op("pool", lambda e, sfx=sfx: e.dma_start(out=peT[:], in_=dr["pe_cmp_" + sfx][l].rearrange("j d -> d j"),
                                                            allow_slow_non_contiguous=True), writes=[("B0_peT",)], dma=True)
                for g in range(2):
                    srcn = "ckT" if kv == 0 else "cvT"
                    P.dma("sp", xc[:], dr[srcn][g, :, :], reads=[(srcn, g, j) for j in range(NT)], writes=[("B0_xc",)])
                    pi = self.nextps(); pp = ps[pi]
                    for j in range(32):
                        P.op("pe", lambda e, j=j, pp=pp: e.matmul(pp[:, 0:255], lhsT=w1[:, j, :], rhs=xc[:, j:j + 16 * 254 + 1:16],
                                                                  start=(j == 0), stop=(j == 31)),
                             reads=[("B0_xc",), ("B0_w1",)], writes=[("ps", pi)])
                    pj = self.nextps(); pp2 = ps[pj]
                    for j in range(32):
                        P.op("pe", lambda e, j=j, pp2=pp2: e.matmul(pp2[:, 0:1], lhsT=w1[:, j, :], rhs=peT[:, j:j + 1],
                                                                    start=(j == 0), stop=(j == 31)),
                             reads=[("B0_peT",), ("B0_w1",)], writes=[("ps", pj)])
                    P.op("dve", lambda e, pp2=pp2: e.tensor_copy(out=bsb[:], in_=pp2[:, 0:1]), reads=[("ps", pj)], writes=[("B0_bsb",)])
                    P.op("dve", lambda e, pp=pp: e.tensor_scalar(out=xg[:, 0:255], in0=pp[:, 0:255], scalar1=bsb[:, 0:1], scalar2=None, op0=ALU.add),
                         reads=[("ps", pi), ("B0_bsb",)], writes=[("B0_xg",)])
                    P.op("dve", lambda e: e.tensor_tensor(out=x2[:], in0=xg[:], in1=xg[:], op=ALU.mult), reads=[("B0_xg",)], writes=[("B0_x2",)])
                    P.op("dve", lambda e: e.tensor_scalar(out=x2[:], in0=x2[:], scalar1=0.044715, scalar2=1.0, op0=ALU.mult, op1=ALU.add),
                         reads=[("B0_x2",)], writes=[("B0_x2",)])
                    P.op("dve", lambda e: e.tensor_tensor(out=x2[:], in0=x2[:], in1=xg[:], op=ALU.mult), reads=[("B0_x2",), ("B0_xg",)], writes=[("B0_x2",)])
                    P.op("act", lambda e: e.activation(out=x2[:], in_=x2[:], func=AF.Tanh, scale=0.7978845608028654),
                         reads=[("B0_x2",)], writes=[("B0_x2",)])
                    P.op("dve", lambda e: e.tensor_scalar(out=x2[:], in0=x2[:], scalar1=1.0, scalar2=0.5, op0=ALU.add, op1=ALU.mult),
                         reads=[("B0_x2",)], writes=[("B0_x2",)])
                    P.op("dve", lambda e: e.tensor_tensor(out=hd[:], in0=x2[:], in1=xg[:], op=ALU.mult), reads=[("B0_x2",), ("B0_xg",)], writes=[("B0_hd",)])
                    if kv == 0:
                        pk = self.nextps(); pp3 = ps[pk]
                        P.op("pe", lambda e, pp3=pp3: e.matmul(pp3[:, 0:256], lhsT=w2[:], rhs=hd[:], start=True, stop=True),
                             reads=[("B0_hd",), ("B0_w2",)], writes=[("ps", pk)])
                        P.op("act", lambda e, pp3=pp3, g=g: e.activation(out=kcT[g][:], in_=pp3[:, 0:256], func=AF.Copy),
                             reads=[("ps", pk)], writes=[("B_kcT", g)])
                    else:
                        for nt in range(2):
                            pk = self.nextps(); pp3 = ps[pk]
                            P.op("pe", lambda e, pp3=pp3, nt=nt: e.matmul(pp3[:, 0:128], lhsT=hd[:, nt * 128:(nt + 1) * 128], rhs=w2[:], start=True, stop=True),
                                 reads=[("B0_hd",), ("B0_w2",)], writes=[("ps", pk)])
                            P.op("act", lambda e, pp3=pp3, g=g, nt=nt: e.activation(out=vc[g][:, nt, :], in_=pp3[:, 0:128], func=AF.Copy),
                                 reads=[("ps", pk)], writes=[("B_vc", g)])
        P.barrier()

        def attn(blocks, pv_sets, obanks, zbank):
            nb = len(blocks)
            pend = []
            for bi, blk in enumerate(blocks):
                si = cnt["s"] % 2; cnt["s"] += 1
                S = ps[si]
                ns = len(blk["s"])
                for mi, (lt, rh, keys) in enumerate(blk["s"]):
                    P.op("pe", lambda e, S=S, lt=lt, rh=rh, mi=mi, ns=ns: e.matmul(S[:, :], lhsT=lt, rhs=rh, start=(mi == 0), stop=(mi == ns - 1)),
                         reads=keys, writes=[("ps", si)])
                pidx = cnt["p"] % 4; cnt["p"] += 1
                Pb = Pt[pidx]
                P.op("act", lambda e, S=S, Pb=Pb: e.activation(out=Pb[:], in_=S[:, :], func=AF.Exp),
                     reads=[("ps", si)], writes=[("B_P", pidx)])
                pend.append((bi, blk, Pb, pidx))
                if len(pend) == 2:
                    emit_pv(pend.pop(0), nb, obanks, zbank)
            while pend:
                emit_pv(pend.pop(0), nb, obanks, zbank)

        def emit_pv(item, nb, obanks, zbank):
            bi, blk, Pb, pidx = item
            for oi, ob in enumerate(obanks):
                lt = blk["v"][oi]
                P.op("pe", lambda e, ob=ob, lt=lt, Pb=Pb, bi=bi, nb=nb: e.matmul(ps[ob][:, :], lhsT=lt, rhs=Pb[:], start=(bi == 0), stop=(bi == nb - 1)),
                     reads=[("B_P", pidx)] + blk["vkeys"], writes=[("ps", ob)])
            P.op("pe", lambda e, Pb=Pb, bi=bi, nb=nb: e.matmul(ps[zbank][:, :], lhsT=ones[:], rhs=Pb[:], start=(bi == 0), stop=(bi == nb - 1)),
                 reads=[("B_P", pidx), KO], writes=[("ps", zbank)])

        def next_oz():
            k = cnt["oz"] % 2; cnt["oz"] += 1
            return 2 + k, 4 + k

        def next_ostg():
            k = cnt["o"] % 4; cnt["o"] += 1
            return k

        with contextlib.ExitStack() as st:
            causal = self.sbuf(st, "N_causal", [128, 4, 512], BF16)
            winm = self.sbuf(st, "N_winm", [128, 8, 512], BF16)
            ov = self.sbuf(st, "N_ov", [128, 2, 65], BF16)
            asel = self.sbuf(st, "N_asel", [64, 32, 128], BF16)
            P.dma("sp", causal[:], dr["causal"][:], writes=[("N_causal",)])
            P.dma("sp", winm[:], dr["winmask"][:], writes=[("N_winm",)])
            P.dma("sp", ov[:], dr["ov"][:], writes=[("N_ov",)])
            P.dma("sp", asel[:], dr["asel"][:], writes=[("N_asel",)])
            skT = self.sbuf(st, "N_skT", [128, T], BF16)
            svs = self.sbuf(st, "N_sv", [128, NB, 128], BF16)
            wkT = self.sbuf(st, "N_wkT", [128, T], BF16)
            wvs = self.sbuf(st, "N_wv", [128, NB, 128], BF16)
            selT = self.sbuf(st, "N_selT", [64, T], BF16)
            q4 = [self.sbuf(st, f"N_q4_{i}", [128, 4, 512], BF16) for i in range(2)]
            cmk = [self.sbuf(st, f"N_cmk{i}", [128, 2, 512], BF16) for i in range(2)]
            stb = [self.sbuf(st, f"N_stb{i}", [128, 3, 4, 64], F32) for i in range(2)]
            gnt = [self.sbuf(st, f"N_gn{i}", [24, 512], BF16) for i in range(2)]
            acc = [self.sbuf(st, f"N_acc{i}", [128, 512], F32) for i in range(4)]
            pslc = self.sbuf(st, "N_pslc", [128, 4, 64], F32)
            z4 = self.sbuf(st, "N_z4", [128, 4], F32)
            sc = self.sbuf(st, "N_sc", [128, 64], F32)
            sc2 = self.sbuf(st, "N_sc2", [128, 64], F32)
            m8 = self.sbuf(st, "N_m8", [128, 8], F32)
            m8b = self.sbuf(st, "N_m8b", [128, 8], F32)
            sbbs = [self.sbuf(st, f"N_sbb{i}", [128, 64], BF16) for i in range(4)]

            def gate_w(row, jb, zb):
                P.op("pe", lambda e, row=row, jb=jb: e.matmul(ps[6][:, :], lhsT=sb["selg"][:, row, :], rhs=gnt[jb][:], start=True, stop=True),
                     reads=[("N_gn", jb), ("c", "selg")], writes=[("ps", 6)])
                P.op("dve", lambda e, zb=zb: e.tensor_scalar(out=zt[:], in0=ps[zb][:, :], scalar1=1e-30, scalar2=None, op0=ALU.add),
                     reads=[("ps", zb)], writes=[("B_zt",)])
                P.op("dve", lambda e: e.reciprocal(out=rz[:], in_=zt[:]), reads=[("B_zt",)], writes=[("B_rz",)])
                P.op("dve", lambda e: e.tensor_tensor(out=wt[:], in0=ps[6][:, :], in1=rz[:], op=ALU.mult),
                     reads=[("ps", 6), ("B_rz",)], writes=[("B_wt",)])

            for g in range(2):
                P.dma("sp", skT[:], dr["skT"][g, :, :], reads=[("skT", g, j) for j in range(NT)], writes=[("N_skT",)])
                P.dma("sp", wkT[:], dr["wkT"][g, :, :], reads=[("wkT", g, j) for j in range(NT)], writes=[("N_wkT",)])
                P.dma("sp", svs[:], dr["sv"][:, g * 128:(g + 1) * 128].rearrange("(b p) d -> p b d", p=128),
                      reads=[("sv", r, 0) for r in range(NB)], writes=[("N_sv",)])
                P.dma("sp", wvs[:], dr["wv"][:, g * 128:(g + 1) * 128].rearrange("(b p) d -> p b d", p=128),
                      reads=[("wv", r, 0) for r in range(NB)], writes=[("N_wv",)])
                for j in range(NT):
                    jb = j % 2
                    t0 = j * TS
                    Q = q4[jb]
                    for hh in range(4):
                        h = 4 * g + hh
                        P.dma("sp", Q[:, hh, :], dr["qnT"][h, :, t0:t0 + TS], reads=[("qnT", h, j)], writes=[("N_q4", jb, hh)])
                    P.dma("sp", cmk[jb][:], dr["cmpmask"][:, j, :, :], writes=[("N_cmk", jb)])
                    P.dma("sp", stb[jb][:], dr["seltab"][:, :, 4 * j:4 * j + 4, :], writes=[("N_stb", jb)])
                    P.dma("sp", gnt[jb][:], dr["gnT"][:, t0:t0 + TS], reads=[("gnT", j)], writes=[("N_gn", jb)])
                    nts = [0] if j < 4 else [0, 1]
                    for hh in range(4):
                        h = 4 * g + hh
                        qa = Q[:, hh, :]
                        qk = ("N_q4", jb, hh)
                        ob, zb = next_oz()
                        blocks = []
                        for nt in nts:
                            blocks.append(dict(
                                s=[(kcT[g][:, nt * 128:(nt + 1) * 128], qa, [("B_kcT", g), qk]),
                                   (ident[:], cmk[jb][:, nt, :], [KI, ("N_cmk", jb)])],
                                v=[vc[g][:, nt, :]], vkeys=[("B_vc", g)]))
                        p_start = cnt["p"]
                        attn(blocks, 1, [ob], zb)
                        Es = [((p_start + i) % 4) for i in range(len(nts))]
                        for r in range(4):
                            for i, nt in enumerate(nts):
                                Eb = Pt[Es[i]]
                                P.op("pe", lambda e, r=r, Eb=Eb, nt=nt, i=i, n=len(nts): e.matmul(
                                    ps[6][:, r * 128:r * 128 + 65], lhsT=Eb[:, r * 128:(r + 1) * 128], rhs=ov[:, nt, :],
                                    start=(i == 0), stop=(i == n - 1)),
                                    reads=[("B_P", Es[i]), ("N_ov",)], writes=[("ps", 6)])
                        p6 = ps[6][:, :].rearrange("p (r k) -> p r k", k=128)
                        P.op("dve", lambda e, p6=p6: e.tensor_scalar(out=z4[:], in0=p6[:, :, 64], scalar1=1e-30, scalar2=None, op0=ALU.add),
                             reads=[("ps", 6)], writes=[("N_z4",)])
                        P.op("dve", lambda e: e.reciprocal(out=z4[:], in_=z4[:]), reads=[("N_z4",)], writes=[("N_z4",)])
                        for r in range(4):
                            if hh == 0:
                                P.op("dve", lambda e, r=r, p6=p6: e.tensor_scalar(out=pslc[:, r, :], in0=p6[:, r, 0:64], scalar1=z4[:, r:r + 1], scalar2=None, op0=ALU.mult),
                                     reads=[("ps", 6), ("N_z4",)], writes=[("N_pslc",)])
                            else:
                                P.op("dve", lambda e, r=r, p6=p6: e.scalar_tensor_tensor(out=pslc[:, r, :], in0=p6[:, r, 0:64], scalar=z4[:, r:r + 1], in1=pslc[:, r, :],
                                                                                         op0=ALU.mult, op1=ALU.add),
                                     reads=[("ps", 6), ("N_z4",), ("N_pslc",)], writes=[("N_pslc",)])
                        gate_w(0 * 8 + h, jb, zb)
                        P.op("dve", lambda e, ob=ob, hh=hh: e.tensor_tensor(out=acc[hh][:], in0=ps[ob][:, :], in1=wt[:], op=ALU.mult),
                             reads=[("ps", ob), ("B_wt",)], writes=[("N_acc", hh)])
                    self.cast_step()
                    if getattr(self, "dbg", False) and g == 0 and j in (0, 1, 2):
                        dj = j
                        P.dma("sp", dr["d_acc"][dj], acc[0][:], reads=[("N_acc", 0)])
                        P.dma("sp", dr["d_pslc"][dj], pslc[:], reads=[("N_pslc",)])
                        P.dma("sp", dr["d_gn"][dj], gnt[jb][:], reads=[("N_gn", jb)])
                        P.dma("sp", dr["d_cmk"][dj], cmk[jb][:], reads=[("N_cmk", jb)])
                        P.dma("sp", dr["d_q4"][dj], Q[:], reads=[("N_q4", jb, hh) for hh in range(4)])
                        P.dma("sp", dr["d_stb"][dj], stb[jb][:], reads=[("N_stb", jb)])
                    for r in range(4):
                        qb = 4 * j + r
                        P.op("dve", lambda e, r=r, jb=jb: e.tensor_tensor(out=sc[:], in0=pslc[:, r, :], in1=stb[jb][:, 0, r, :], op=ALU.mult),
                             reads=[("N_pslc",), ("N_stb", jb)], writes=[("N_sc",)])
                        P.op("dve", lambda e, r=r, jb=jb: e.tensor_tensor(out=sc[:], in0=sc[:], in1=stb[jb][:, 1, r, :], op=ALU.add),
                             reads=[("N_sc",), ("N_stb", jb)], writes=[("N_sc",)])
                        P.op("dve", lambda e: e.max(out=m8[:], in_=sc[:]), reads=[("N_sc",)], writes=[("N_m8",)])
                        P.op("dve", lambda e: e.match_replace(out=sc2[:], in_to_replace=m8[:], in_values=sc[:], imm_value=-1e9),
                             reads=[("N_sc",), ("N_m8",)], writes=[("N_sc2",)])
                        P.op("dve", lambda e: e.max(out=m8b[:], in_=sc2[:]), reads=[("N_sc2",)], writes=[("N_m8b",)])
                        P.op("dve", lambda e: e.tensor_scalar(out=sc2[:], in0=sc[:], scalar1=m8b[:, 7:8], scalar2=None, op0=ALU.is_ge),
                             reads=[("N_sc",), ("N_m8b",)], writes=[("N_sc2",)])
                        P.op("dve", lambda e, r=r, jb=jb: e.tensor_tensor(out=sc2[:], in0=sc2[:], in1=stb[jb][:, 2, r, :], op=ALU.mult),
                             reads=[("N_sc2",), ("N_stb", jb)], writes=[("N_sc2",)])
                        P.op("dve", lambda e, r=r: e.tensor_scalar(out=sbbs[r][:], in0=sc2[:], scalar1=BIG, scalar2=-BIG, op0=ALU.mult, op1=ALU.add),
                             reads=[("N_sc2",)], writes=[("N_sbb", r)])

                    def sel_transposes(j=j):
                        for r in range(4):
                            qb = 4 * j + r
                            P.op("pe", lambda e, r=r: e.transpose(out=self.psb[0:64, r * 128:(r + 1) * 128], in_=sbbs[r][:], identity=ident[:]),
                                 reads=[("N_sbb", r), KI], writes=[("psb", r)])
                            P.op("act", lambda e, qb=qb, r=r: e.activation(out=selT[:, qb * 128:(qb + 1) * 128], in_=self.psb[0:64, r * 128:(r + 1) * 128], func=AF.Copy),
                                 reads=[("psb", r)], writes=[("N_selT", qb)])
                    for hh in range(4):
                        h = 4 * g + hh
                        qa = Q[:, hh, :]
                        qk = ("N_q4", jb, hh)
                        ob, zb = next_oz()
                        blocks = []
                        for kb in range(max(0, 4 * j - 4), 4 * j + 4):
                            s = [(wkT[:, kb * 128:(kb + 1) * 128], qa, [("N_wkT",), qk]),
                                 (ident[:], winm[:, kb - 4 * j + 4, :], [KI, ("N_winm",)])]
                            blocks.append(dict(s=s, v=[wvs[:, kb, :]], vkeys=[("N_wv",)]))
                        attn(blocks, 1, [ob], zb)
                        gate_w(2 * 8 + h, jb, zb)
                        P.op("dve", lambda e, ob=ob: e.tensor_tensor(out=ctr[:], in0=ps[ob][:, :], in1=wt[:], op=ALU.mult),
                             reads=[("ps", ob), ("B_wt",)], writes=[("B_ctr",)])
                        P.op("pool", lambda e, hh=hh: e.tensor_tensor(out=acc[hh][:], in0=acc[hh][:], in1=ctr[:], op=ALU.add),
                             reads=[("N_acc", hh), ("B_ctr",)], writes=[("N_acc", hh)])
                    sel_transposes()
                    self.cast_step()
                    for hh in range(4):
                        h = 4 * g + hh
                        qa = Q[:, hh, :]
                        qk = ("N_q4", jb, hh)
                        ob, zb = next_oz()
                        blocks = []
                        for kb in range(4 * j + 4):
                            s = [(skT[:, kb * 128:(kb + 1) * 128], qa, [("N_skT",), qk]),
                                 (asel[:, kb, :], selT[:, t0:t0 + TS], [("N_asel",)] + [("N_selT", 4 * j + r) for r in range(4)])]
                            if kb >= 4 * j:
                                s.append((ident[:], causal[:, kb - 4 * j, :], [KI, ("N_causal",)]))
                            blocks.append(dict(s=s, v=[svs[:, kb, :]], vkeys=[("N_sv",)]))
                        attn(blocks, 1, [ob], zb)
                        gate_w(1 * 8 + h, jb, zb)
                        P.op("dve", lambda e, ob=ob: e.tensor_tensor(out=ctr[:], in0=ps[ob][:, :], in1=wt[:], op=ALU.mult),
                             reads=[("ps", ob), ("B_wt",)], writes=[("B_ctr",)])
                        oi = next_ostg()
                        P.op("pool", lambda e, hh=hh, oi=oi: e.tensor_tensor(out=ostg[oi][:], in0=acc[hh][:], in1=ctr[:], op=ALU.add),
                             reads=[("N_acc", hh), ("B_ctr",)], writes=[("B_ostg", oi)])
                        P.dma("pool", dr["oT"][8 + h, :, t0:t0 + TS], ostg[oi][:], reads=[("B_ostg", oi)], writes=[("oT", 8 + h, j)])
                    self.cast_step()
                if getattr(self, "dbg", False) and g == 0:
                    P.dma("sp", dr["d_selT"][:, :], selT[:], reads=[("N_selT", q) for q in range(NB)])
        P.barrier()

        with contextlib.ExitStack() as st:
            causal = self.sbuf(st, "F_causal", [128, 4, 512], BF16)
            P.dma("sp", causal[:], dr["causal"][:], writes=[("F_causal",)])
            kT = [self.sbuf(st, f"F_kT{i}", [128, T], BF16) for i in range(2)]
            vs = [self.sbuf(st, f"F_v{i}", [128, NB, 128], BF16) for i in range(2)]
            q6 = [self.sbuf(st, f"F_q6{i}", [6, T], BF16) for i in range(2)]
            k6 = [self.sbuf(st, f"F_k6{i}", [6, T], BF16) for i in range(2)]
            qt = [self.sbuf(st, f"F_q{i}", [128, 512], BF16) for i in range(2)]
            qc = 0
            for h in range(8):
                hb = h % 2
                P.dma("sp", kT[hb][:], dr["kfT"][h, :, :], reads=[("kfT", h, j) for j in range(NT)], writes=[("F_kT", hb)])
                P.dma("sp", vs[hb][:], dr["vf"][:, h * 128:(h + 1) * 128].rearrange("(b p) d -> p b d", p=128),
                      reads=[("vf", r, h // 4) for r in range(NB)], writes=[("F_v", hb)])
                P.op("pool", lambda e, hb=hb: e.memset(q6[hb][:], 1.0), writes=[("F_q6", hb)])
                P.op("pool", lambda e, hb=hb: e.memset(k6[hb][:], 1.0), writes=[("F_k6", hb)])
                P.dma("sp", q6[hb][0:3, :], dr["cs3"][h, 0:3, :], reads=[("cs3",)], writes=[("F_q6", hb)])
                P.dma("sp", k6[hb][3:6, :], dr["cs3"][h, 3:6, :], reads=[("cs3",)], writes=[("F_k6", hb)])
                for j in range(NT):
                    t0 = j * TS
                    qb_ = qc % 2; qc += 1
                    P.dma("sp", qt[qb_][:], dr["qfT"][h, :, t0:t0 + TS], reads=[("qfT", h, j)], writes=[("F_q", qb_)])
                    ob, zb = next_oz()
                    blocks = []
                    for kb in range(4 * j + 4):
                        s = [(kT[hb][:, kb * 128:(kb + 1) * 128], qt[qb_][:], [("F_kT", hb), ("F_q", qb_)]),
                             (k6[hb][:, kb * 128:(kb + 1) * 128], q6[hb][:, t0:t0 + TS], [("F_k6", hb), ("F_q6", hb)])]
                        if kb >= 4 * j:
                            s.append((ident[:], causal[:, kb - 4 * j, :], [KI, ("F_causal",)]))
                        blocks.append(dict(s=s, v=[vs[hb][:, kb, :]], vkeys=[("F_v", hb)]))
                    attn(blocks, 1, [ob], zb)
                    P.op("dve", lambda e, zb=zb: e.reciprocal(out=rz[:], in_=ps[zb][:, :]), reads=[("ps", zb)], writes=[("B_rz",)])
                    oi = next_ostg()
                    P.op("dve", lambda e, ob=ob, oi=oi: e.tensor_tensor(out=ostg[oi][:], in0=ps[ob][:, :], in1=rz[:], op=ALU.mult),
                         reads=[("ps", ob), ("B_rz",)], writes=[("B_ostg", oi)])
                    P.dma("pool", dr["oT"][h, :, t0:t0 + TS], ostg[oi][:], reads=[("B_ostg", oi)], writes=[("oT", h, j)])
                    self.cast_step()
        P.barrier()

        with contextlib.ExitStack() as st:
            mq = [self.sbuf(st, f"M_q{i}", [128, 2, 512], BF16) for i in range(2)]
            qc = 0
            for h in range(4):
                for j in range(NT):
                    t0 = j * TS
                    qb_ = qc % 2; qc += 1
                    for c in range(2):
                        P.dma("sp", mq[qb_][:, c, :], dr["mqT"][h * 2 + c, :, t0:t0 + TS], reads=[("mqT", h * 2 + c, j)], writes=[("M_q", qb_, c)])
                    blocks = []
                    for mt in range(2):
                        s = [(mkT[:, h * 2 + c, mt * 128:(mt + 1) * 128], mq[qb_][:, c, :], [("B_mkT",), ("M_q", qb_, c)]) for c in range(2)]
                        blocks.append(dict(s=s, v=[mv[:, mt, h * 256 + c * 128:h * 256 + (c + 1) * 128] for c in range(2)], vkeys=[("B_mv",)]))
                    attn(blocks, 2, [2, 3], 4)
                    P.op("dve", lambda e: e.reciprocal(out=rz[:], in_=ps[4][:, :]), reads=[("ps", 4)], writes=[("B_rz",)])
                    for c in range(2):
                        oi = next_ostg()
                        P.op("dve", lambda e, c=c, oi=oi: e.tensor_tensor(out=ostg[oi][:], in0=ps[2 + c][:, :], in1=rz[:], op=ALU.mult),
                             reads=[("ps", 2 + c), ("B_rz",)], writes=[("B_ostg", oi)])
                        P.dma("pool", dr["oT"][16 + h * 2 + c, :, t0:t0 + TS], ostg[oi][:], reads=[("B_ostg", oi)], writes=[("oT", 16 + h * 2 + c, j)])
                    self.cast_step()
    P.barrier()


Builder.phase_B = _phase_B


def _phase_C(self, l, src_name, dst_name):
    nc, P, dr, sb = self.nc, self.P, self.dr, self.sb
    vb = l * VPL
    ps = self.ps
    vec = sb["vecs"]
    with contextlib.ExitStack() as st:
        oTt = self.sbuf(st, "C_oT", [128, 24, TS], BF16)
        NG = 4
        gts = [self.sbuf(st, f"C_g{i}", [128, TS], BF16) for i in range(NG)]
        NWB = 4
        Wb = [self.sbuf(st, f"C_W{i}", [128, NCH, 512], BF16) for i in range(NWB)]
        mrg = self.sbuf(st, "C_mrg", [128, NCH, TS], BF16)
        y = self.sbuf(st, "C_y", [128, NCH, TS], F32)
        h = self.sbuf(st, "C_h", [128, NCH, TS], F32)
        ag = self.sbuf(st, "C_ag", [128, NCH, TS], BF16)
        tmp = [self.sbuf(st, f"C_t{i}", [128, TS], F32) for i in range(3)]
        rl = [self.sbuf(st, f"C_rl{i}", [128, TS], BF16) for i in range(2)]
        rstd = self.sbuf(st, "C_rstd", [128, TS], F32)
        ntmp = self.sbuf(st, "C_ntmp", [128, TS], F32)
        wi = [0]
        gi = [0]

        def loadW(dn, idx, nk):
            b = wi[0] % NWB; wi[0] += 1
            P.dma("sp", Wb[b][:, 0:nk, :], dr[dn][idx], reads=[("wc", dn, idx)], writes=[("C_W", b)])
            return b

        def norm_apply(srcbuf, srckey, gcol, dstfn):
            self.rmsnorm_stats(srcbuf, srckey, ag, rstd, ntmp)

        for j in range(NT):
            t0 = j * TS
            P.dma("sp", oTt[:], dr["oT"][:, :, t0:t0 + TS].rearrange("c p t -> p c t"),
                  reads=[("oT", c, j) for c in range(24)], writes=[("C_oT",)])
            for cg in range(4):
                bs = []
                for br, wn in enumerate(("w_up_fox", "w_up_nsa", "w_up_mem")):
                    bs.append(loadW("wcu", br * 4 + cg, 8))
                for cbl in range(4):
                    cb = cg * 4 + cbl
                    gb = []
                    for br in range(3):
                        k = gi[0] % NG; gi[0] += 1
                        P.dma("sp", gts[k][:], dr["bgT"][br * 16 + cb, :, t0:t0 + TS], reads=[("bgT", br * 16 + cb, j)], writes=[("C_g", k)])
                        gb.append(k)
                    pis = []
                    for br in range(3):
                        pi = self.nextps(); pis.append(pi)
                        for k in range(8):
                            P.op("pe", lambda e, pi=pi, br=br, k=k, cbl=cbl, b=bs[br]: e.matmul(
                                ps[pi][:, :], lhsT=Wb[b][:, k, cbl * 128:(cbl + 1) * 128], rhs=oTt[:, br * 8 + k, :],
                                start=(k == 0), stop=(k == 7)),
                                reads=[("C_W", bs[br]), ("C_oT",)], writes=[("ps", pi)])
                    for br in range(3):
                        P.op("dve", lambda e, br=br, pi=pis[br], k=gb[br]: e.tensor_tensor(out=tmp[br][:], in0=ps[pi][:, :], in1=gts[k][:], op=ALU.mult),
                             reads=[("ps", pis[br]), ("C_g", gb[br])], writes=[("C_t", br)])
                    P.op("pool", lambda e: e.tensor_tensor(out=tmp[0][:], in0=tmp[0][:], in1=tmp[1][:], op=ALU.add),
                         reads=[("C_t", 0), ("C_t", 1)], writes=[("C_t", 0)])
                    P.op("pool", lambda e, cb=cb: e.tensor_tensor(out=mrg[:, cb, :], in0=tmp[0][:], in1=tmp[2][:], op=ALU.add),
                         reads=[("C_t", 0), ("C_t", 2)], writes=[("C_mrg", cb)])
            MK = [("C_mrg", c) for c in range(NCH)]
            for cg in range(4):
                b = loadW("wcb", cg, NCH)
                for cbl in range(4):
                    cb = cg * 4 + cbl
                    pi = self.nextps()
                    for k in range(NCH):
                        P.op("pe", lambda e, pi=pi, k=k, cbl=cbl, b=b: e.matmul(
                            ps[pi][:, :], lhsT=Wb[b][:, k, cbl * 128:(cbl + 1) * 128], rhs=mrg[:, k, :],
                            start=(k == 0), stop=(k == NCH - 1)),
                            reads=[("C_W", b)] + MK, writes=[("ps", pi)])
                    P.op("act", lambda e, pi=pi, cb=cb: e.activation(out=y[:, cb, :], in_=ps[pi][:, :], func=AF.Copy),
                         reads=[("ps", pi)], writes=[("C_y", cb)])
            YK = [("C_y", c) for c in range(NCH)]
            P.dma("sp", h[:], dr[src_name][:, :, t0:t0 + TS].rearrange("c p t -> p c t"),
                  reads=[(src_name, j)], writes=[("C_h",)])
            P.op("act", lambda e: e.activation(out=ag[:], in_=y[:], func=AF.Square), reads=YK, writes=[("sq",)] + [("C_ag", c) for c in range(NCH)])
            self._stats_from_sq(ag, rstd, ntmp)
            for cb in range(NCH):
                tb = cb % 2
                P.op("dve", lambda e, cb=cb, tb=tb: e.scalar_tensor_tensor(
                    out=tmp[tb][:], in0=y[:, cb, :], scalar=vec[:, vb + VG_POST_MIX + cb:vb + VG_POST_MIX + cb + 1], in1=rstd[:],
                    op0=ALU.mult, op1=ALU.mult),
                    reads=[("C_y", cb), ("rstd",), ("c", "vecs")], writes=[("C_t", tb)])
                P.op("pool", lambda e, cb=cb, tb=tb: e.tensor_tensor(out=h[:, cb, :], in0=h[:, cb, :], in1=tmp[tb][:], op=ALU.add),
                     reads=[("C_h",), ("C_t", tb)], writes=[("C_h",)])
            P.op("act", lambda e: e.activation(out=ag[:], in_=h[:], func=AF.Square), reads=[("C_h",)], writes=[("sq",)] + [("C_ag", c) for c in range(NCH)])
            self._stats_from_sq(ag, rstd, ntmp)
            for cb in range(NCH):
                P.op("dve", lambda e, cb=cb: e.scalar_tensor_tensor(
                    out=mrg[:, cb, :], in0=h[:, cb, :], scalar=vec[:, vb + VG_PRE_MLP + cb:vb + VG_PRE_MLP + cb + 1], in1=rstd[:],
                    op0=ALU.mult, op1=ALU.mult),
                    reads=[("C_h",), ("rstd",), ("c", "vecs")], writes=[("C_mrg", cb)])
            for g in range(4):
                for wt_ in range(4):
                    b = loadW("wcb", 4 + g * 4 + wt_, NCH)
                    for cbl in range(4):
                        pi = self.nextps()
                        for k in range(NCH):
                            P.op("pe", lambda e, pi=pi, k=k, cbl=cbl, b=b: e.matmul(
                                ps[pi][:, :], lhsT=Wb[b][:, k, cbl * 128:(cbl + 1) * 128], rhs=mrg[:, k, :],
                                start=(k == 0), stop=(k == NCH - 1)),
                                reads=[("C_W", b)] + MK, writes=[("ps", pi)])
                        ri = (wt_ * 4 + cbl) % 2
                        P.op("act", lambda e, pi=pi, ri=ri: e.activation(out=rl[ri][:], in_=ps[pi][:, :], func=AF.Relu),
                             reads=[("ps", pi)], writes=[("C_rl", ri)])
                        ac = wt_ * 4 + cbl
                        P.op("pool", lambda e, ri=ri, ac=ac: e.tensor_tensor(out=ag[:, ac, :], in0=rl[ri][:], in1=rl[ri][:], op=ALU.mult),
                             reads=[("C_rl", ri)], writes=[("C_ag", ac)])
                AK = [("C_ag", c) for c in range(NCH)]
                for cg in range(4):
                    b = loadW("wcb", 20 + g * 4 + cg, NCH)
                    for cbl in range(4):
                        cb = cg * 4 + cbl
                        pi = self.nextps()
                        for k in range(NCH):
                            P.op("pe", lambda e, pi=pi, k=k, cbl=cbl, b=b: e.matmul(
                                ps[pi][:, :], lhsT=Wb[b][:, k, cbl * 128:(cbl + 1) * 128], rhs=ag[:, k, :],
                                start=(k == 0), stop=(k == NCH - 1)),
                                reads=[("C_W", b)] + AK, writes=[("ps", pi)])
                        if g == 0:
                            P.op("act", lambda e, pi=pi, cb=cb: e.activation(out=y[:, cb, :], in_=ps[pi][:, :], func=AF.Copy),
                                 reads=[("ps", pi)], writes=[("C_y", cb)])
                        else:
                            P.op("dve", lambda e, pi=pi, cb=cb: e.tensor_tensor(out=y[:, cb, :], in0=ps[pi][:, :], in1=y[:, cb, :], op=ALU.add),
                                 reads=[("ps", pi), ("C_y", cb)], writes=[("C_y", cb)])
            P.op("act", lambda e: e.activation(out=ag[:], in_=y[:], func=AF.Square), reads=YK, writes=[("sq",)] + AK)
            self._stats_from_sq(ag, rstd, ntmp)
            for cb in range(NCH):
                tb = cb % 2
                P.op("dve", lambda e, cb=cb, tb=tb: e.scalar_tensor_tensor(
                    out=tmp[tb][:], in0=y[:, cb, :], scalar=vec[:, vb + VG_POST_MLP + cb:vb + VG_POST_MLP + cb + 1], in1=rstd[:],
                    op0=ALU.mult, op1=ALU.mult),
                    reads=[("C_y", cb), ("rstd",), ("c", "vecs")], writes=[("C_t", tb)])
                P.op("pool", lambda e, cb=cb, tb=tb: e.tensor_tensor(out=h[:, cb, :], in0=h[:, cb, :], in1=tmp[tb][:], op=ALU.add),
                     reads=[("C_h",), ("C_t", tb)], writes=[("C_h",)])
            d = P.dma("act", dr[dst_name][:, :, t0:t0 + TS].rearrange("c p t -> p c t"), h[:],
                      reads=[("C_h",)], writes=[(dst_name, j)])
            if dst_name == "outT":
                self.final.append(d)
    P.barrier()


def _stats_from_sq(self, sq, rstd, tmp, nfree=512):
    P = self.P
    pi = self.nextps()
    ps = self.ps[pi]
    for c in range(NCH):
        P.op("pe", lambda e, c=c: e.matmul(ps[:, 0:nfree], lhsT=self.sb["ones"][:], rhs=sq[:, c, 0:nfree],
                                           start=(c == 0), stop=(c == NCH - 1)),
             reads=[("sq",), ("c", "ones")], writes=[("ps", pi)])
    P.op("dve", lambda e: e.tensor_scalar(out=tmp[:, 0:nfree], in0=ps[:, 0:nfree], scalar1=1.0 / D, scalar2=EPS,
                                          op0=ALU.mult, op1=ALU.add),
         reads=[("ps", pi)], writes=[("nt",)])
    P.op("act", lambda e: e.activation(out=tmp[:, 0:nfree], in_=tmp[:, 0:nfree], func=AF.Sqrt),
         reads=[("nt",)], writes=[("nt",)])
    P.op("dve", lambda e: e.reciprocal(out=rstd[:, 0:nfree], in_=tmp[:, 0:nfree]),
         reads=[("nt",)], writes=[("rstd",)])


Builder.phase_C = _phase_C
Builder._stats_from_sq = _stats_from_sq


from concourse.bass_utils import run_bass_kernel_spmd

N_ACTIVE = 4


def build_fused():
    B = Builder()
    src = "xT"
    for l in range(NL):
        B.phase_A(l, src)
        B.phase_B(l)
        dst = "outT" if l == NL - 1 else "hT"
        B.phase_C(l, src, dst)
        src = dst
    B.P.emit(final_waits=B.final)
    return B


def kernel(**inputs):
    inp = {k: np.asarray(v) for k, v in inputs.items()}
    consts = host_consts()
    consts["vecs"] = host_vecs(inp)
    B = build_fused()
    in_maps = []
    for b in range(N_ACTIVE):
        m = {"xT": np.ascontiguousarray(inp["x"][b].T.reshape(NCH, 128, T)).astype(np.float32),
             "memT": np.ascontiguousarray(inp["mem"][b].T.reshape(NCH, 128, 256)).astype(np.float32)}
        m.update(consts)
        for k in WEIGHT_SPECS:
            m[k] = np.ascontiguousarray(inp[k], dtype=np.float32)
        in_maps.append(m)
    res = run_bass_kernel_spmd(B.nc, in_maps, core_ids=list(range(N_ACTIVE)))
    out = np.empty((N_ACTIVE, T, D), np.float32)
    for b in range(N_ACTIVE):
        oT = np.asarray(res.results[b]["outT"], dtype=np.float32)
        out[b] = oT.reshape(D, T).T
    return out
```

```python
import contextlib
import numpy as np
import ml_dtypes
import concourse.bass as bass
import concourse.mybir as mybir

F32 = mybir.dt.float32
BF16 = mybir.dt.bfloat16
AF = mybir.ActivationFunctionType
ALU = mybir.AluOpType
NPBF = ml_dtypes.bfloat16

ENGS = ["pe", "act", "dve", "pool", "sp"]
EPOCH = 20000
NDSEM = {"sp": 28, "pool": 20, "act": 8}


class Op:
    __slots__ = ("eng", "fn", "deps", "dma", "sig", "sigord", "dsem", "dval", "prev")

    def __init__(self, eng, fn, deps, dma):
        self.eng = eng
        self.fn = fn
        self.deps = deps
        self.dma = dma
        self.sig = False
        self.sigord = 0
        self.dsem = None
        self.dval = 0
        self.prev = None


class Prog:
    def __init__(self, nc):
        self.nc = nc
        self.ops = {e: [] for e in ENGS}
        self.last_w = {}
        self.rd_c = {}
        self.rd_d = {}
        self.stack = contextlib.ExitStack()
        self.dma_since = []

    def op(self, eng, fn, reads=(), writes=(), dma=False):
        ops = self.ops[eng]
        idx = len(ops)
        deps = set()
        for k in reads:
            w = self.last_w.get(k)
            if w is not None:
                deps.add(w)
        for k in writes:
            w = self.last_w.get(k)
            if w is not None:
                deps.add(w)
            rc = self.rd_c.get(k)
            if rc:
                for e, i in rc.items():
                    deps.add((e, i))
            rd = self.rd_d.get(k)
            if rd:
                deps.update(rd)
        ops.append(Op(eng, fn, deps, dma))
        me = (eng, idx)
        if dma:
            self.dma_since.append(me)
        for k in reads:
            if dma:
                self.rd_d.setdefault(k, []).append(me)
            else:
                self.rd_c.setdefault(k, {})[eng] = idx
        for k in writes:
            self.last_w[k] = me
            self.rd_c[k] = {}
            self.rd_d[k] = []
        return me

    def dma(self, eng, out, in_, reads=(), writes=()):
        return self.op(eng, lambda e: e.dma_start(out=out, in_=in_), reads, writes, dma=True)

    def barrier(self):
        deps = set(self.dma_since)
        for e in ENGS:
            for i in range(len(self.ops[e]) - 1, -1, -1):
                if not self.ops[e][i].dma:
                    deps.add((e, i))
                    break
        for e in ENGS:
            self.ops[e].append(Op(e, lambda eng: eng.nop(), set(deps), False))
        self.dma_since = []

    def emit(self, final_waits=()):
        nc = self.nc
        ops = self.ops
        for e in ENGS:
            for o in ops[e]:
                for (de, di) in o.deps:
                    d = ops[de][di]
                    if not d.dma:
                        if de == "pe" and e == "pe":
                            continue
                        d.sig = True
        for (de, di) in final_waits:
            if not ops[de][di].dma:
                ops[de][di].sig = True
        nsig = {}
        for e in ENGS:
            c = 0
            for o in ops[e]:
                if o.dma:
                    continue
                if o.sig:
                    c += 1
                    o.sigord = c
            nsig[e] = c
        st = self.stack
        csem = {}
        for e in ENGS:
            n = (nsig[e] + EPOCH - 1) // EPOCH
            csem[e] = [st.enter_context(nc.semaphore(f"c_{e}_{i}")) for i in range(max(n, 1))]
        dsem = {}
        for e in ("sp", "pool", "act"):
            dsem[e] = [st.enter_context(nc.semaphore(f"d_{e}_{i}")) for i in range(NDSEM[e])]
        for e in ("sp", "pool", "act"):
            cnt = [0] * NDSEM[e]
            k = 0
            for o in ops[e]:
                if o.dma:
                    s = k % NDSEM[e]
                    k += 1
                    cnt[s] += 1
                    o.dsem = dsem[e][s]
                    o.dval = 16 * cnt[s]
        for e in ("pe", "dve"):
            for o in ops[e]:
                assert not o.dma

        def target(de, di):
            d = ops[de][di]
            if d.dma:
                return d.dsem, d.dval
            so = d.sigord
            return csem[de][(so - 1) // EPOCH], (so - 1) % EPOCH + 1

        engobj = {"pe": "tensor", "act": "scalar", "dve": "vector", "pool": "gpsimd", "sp": "sync"}
        stats = {}
        with nc.Block() as block:
            def body(ename):
                def run(eng):
                    seen = {}
                    nw = 0
                    for idx, o in enumerate(ops[ename]):
                        waits = []
                        for (de, di) in o.deps:
                            if de == "pe" and ename == "pe":
                                continue
                            waits.append(target(de, di))
                        if o.dma and o.dval > 16:
                            waits.append((o.dsem, o.dval - 16))
                        for (s, v) in waits:
                            key = id(s)
                            if seen.get(key, 0) >= v:
                                continue
                            seen[key] = v
                            eng.wait_ge(s, v)
                            nw += 1
                        ins = o.fn(eng)
                        if o.dma:
                            ins.then_inc(o.dsem, 16)
                        elif o.sig:
                            so = o.sigord
                            ins.then_inc(csem[ename][(so - 1) // EPOCH], 1)
                    if ename == "sp":
                        for (de, di) in final_waits:
                            s, v = target(de, di)
                            eng.wait_ge(s, v)
                    stats[ename] = (len(ops[ename]), nw)
                return run
            block.tensor(body("pe"))
            block.scalar(body("act"))
            block.vector(body("dve"))
            block.gpsimd(body("pool"))
            block.sync(body("sp"))
        return stats


import math

D = 2048
T = 4096
NCH = 16
DIN = 12832
NL = 2
BIG = 30000.0
EPS = 1e-6
TS = 512
NT = T // TS
NB = T // 128

SEG = {}
_o = 0
for _n, _w in [("fq", 1024), ("fk", 1024), ("fv", 1024), ("ff", 8), ("nq", 1024), ("ck", 256), ("cv", 256),
               ("sk", 256), ("sv", 256), ("wk", 256), ("wv", 256), ("ng", 24), ("mq", 1024), ("bg", 6144)]:
    SEG[_n] = (_o, _w)
    _o += _w
assert _o == DIN

VG_PRE_MIX, VG_POST_MIX, VG_PRE_MLP, VG_POST_MLP, VG_MEM, V_NEGBF = 0, 16, 32, 48, 64, 80
VPL = 81


def host_consts():
    c = {}
    half = 16
    inv = (500000.0 ** (-np.arange(half, dtype=np.float32) / half)).astype(np.float32)
    ang = np.arange(T, dtype=np.float32)[:, None] * inv[None, :]
    cos, sin = np.cos(ang).astype(np.float32), np.sin(ang).astype(np.float32)
    cs = np.zeros((2, 32, T), np.float32)
    cs[0, :16] = cos.T; cs[0, 16:] = cos.T
    cs[1, :16] = sin.T; cs[1, 16:] = sin.T
    c["ropecs"] = cs
    pm = np.zeros((32, 32), np.float32)
    for m in range(16):
        pm[m + 16, m] = -1.0
        pm[m, m + 16] = 1.0
    c["pm"] = pm.astype(NPBF)
    c["ident"] = np.eye(128, dtype=np.float32).astype(NPBF)
    c["ones"] = np.ones((128, 128), np.float32).astype(NPBF)
    k = np.arange(128)[:, None]
    q = np.arange(512)[None, :]
    cz = np.zeros((128, 4, 512), np.float32)
    for r in range(4):
        cz[:, r, :] = np.where(r * 128 + k <= q, 0.0, -BIG)
    c["causal"] = cz.astype(NPBF)
    wz = np.zeros((128, 8, 512), np.float32)
    for i in range(8):
        rel = q - ((i - 4) * 128 + k)
        wz[:, i, :] = np.where((rel >= 0) & (rel < 512), 0.0, -BIG)
    c["winmask"] = wz.astype(NPBF)
    n = np.arange(256)
    tt = np.arange(T)
    valid = (n[:, None] * 16 + 31 <= tt[None, :]) & (n[:, None] < 255)
    cm = np.where(valid, 0.0, -BIG).astype(np.float32)
    cm = cm.reshape(2, 128, NT, 512).transpose(1, 2, 0, 3)
    c["cmpmask"] = np.ascontiguousarray(cm).astype(NPBF)
    ci = n[:, None] * 16
    sj = np.arange(64)[None, :] * 64
    ov = ((ci <= sj + 63) & (ci + 31 >= sj) & (n[:, None] < 255)).astype(np.float32)
    ovo = np.concatenate([ov, np.ones((256, 1), np.float32)], axis=1)
    ovo[255, :] = 0.0
    c["ov"] = np.ascontiguousarray(ovo.reshape(2, 128, 65).transpose(1, 0, 2)).astype(NPBF)
    a = np.zeros((128, 32, 128), np.float32)
    for kb in range(32):
        a[2 * kb, kb, 0:64] = 1.0
        a[2 * kb + 1, kb, 64:128] = 1.0
    c["asel"] = a.astype(NPBF)
    blk = np.arange(64)[None, :]
    cur = (tt // 64)[:, None]
    sel_valid = blk <= cur
    forced = (blk == 0) | (blk == cur) | (blk == cur - 1)
    vt = (sel_valid & ~forced).astype(np.float32)
    ct = np.where(forced, 1e6, np.where(sel_valid, 0.0, -1.0)).astype(np.float32)
    def tm(x):
        return np.ascontiguousarray(x.reshape(32, 128, 64).transpose(1, 0, 2)).astype(np.float32)
    c["seltab"] = np.stack([tm(vt), tm(ct), tm(sel_valid.astype(np.float32))], axis=1)
    sg = np.zeros((128, 24, 128), np.float32)
    for r in range(24):
        sg[r, r, :] = 1.0
    c["selg"] = sg.astype(NPBF)
    return c


def host_vecs(inp):
    v = np.zeros((128, NL * VPL), np.float32)
    for l in range(NL):
        b = l * VPL
        for nm, off in [("g_pre_mix", VG_PRE_MIX), ("g_post_mix", VG_POST_MIX), ("g_pre_mlp", VG_PRE_MLP),
                        ("g_post_mlp", VG_POST_MLP), ("g_mem", VG_MEM)]:
            v[:, b + off:b + off + 16] = np.asarray(inp[nm][l], np.float32).reshape(16, 128).T
        v[:8, b + V_NEGBF] = np.asarray(inp["b_f"][l], np.float32)
    return v


CONST_SPECS = {
    "ropecs": ([2, 32, T], F32), "pm": ([32, 32], BF16), "ident": ([128, 128], BF16), "ones": ([128, 128], BF16),
    "causal": ([128, 4, 512], BF16), "winmask": ([128, 8, 512], BF16), "cmpmask": ([128, NT, 2, 512], BF16),
    "ov": ([128, 2, 65], BF16), "asel": ([128, 32, 128], BF16), "seltab": ([128, 3, 32, 64], F32),
    "selg": ([128, 24, 128], BF16), "vecs": ([128, NL * VPL], F32),
}
WEIGHT_SPECS = {
    "w_in": [NL, D, DIN], "w_cmp1_k": [NL, 4096, 128], "w_cmp2_k": [NL, 128, 128], "pe_cmp_k": [NL, 32, 128],
    "w_cmp1_v": [NL, 4096, 128], "w_cmp2_v": [NL, 128, 128], "pe_cmp_v": [NL, 32, 128],
    "w_mem_kv": [NL, D, 2048], "w_up_fox": [NL, 1024, D], "w_up_nsa": [NL, 1024, D], "w_up_mem": [NL, 1024, D],
    "w_o": [NL, D, D], "w_mlp1": [NL, D, 8192], "w_mlp2": [NL, 8192, D],
}
SCRATCH = {
    "hT": ([NCH, 128, T], F32),
    "qfT": ([8, 128, T], BF16), "kfT": ([8, 128, T], BF16), "vf": ([T, 1024], BF16), "logf": ([8, T], F32),
    "qnT": ([8, 128, T], BF16), "ckT": ([2, 128, T], BF16), "cvT": ([2, 128, T], BF16),
    "skT": ([2, 128, T], BF16), "sv": ([T, 256], BF16), "wkT": ([2, 128, T], BF16), "wv": ([T, 256], BF16),
    "gnT": ([24, T], BF16), "mqT": ([8, 128, T], BF16), "bgT": ([48, 128, T], BF16),
    "oT": ([24, 128, T], BF16), "cs3": ([8, 6, T], BF16),
    "wcu": ([12, 128, 8, 512], BF16), "wcb": ([36, 128, 16, 512], BF16),
}


DBG = {"d_acc": ([3, 128, 512], F32), "d_pslc": ([3, 128, 4, 64], F32), "d_gn": ([3, 24, 512], BF16),
       "d_cmk": ([3, 128, 2, 512], BF16), "d_q4": ([3, 128, 4, 512], BF16), "d_stb": ([3, 128, 3, 4, 64], F32),
       "d_selT": ([64, T], BF16)}


class Builder:
    def __init__(self, ext_in=(), ext_out=(), dbg=False):
        self.nc = nc = bass.Bass("TRN2", target_bir_lowering=False)
        self.P = Prog(nc)
        self.dr = {}
        self.dr["xT"] = nc.dram_tensor("xT", [NCH, 128, T], F32, kind="ExternalInput").ap()
        self.dr["memT"] = nc.dram_tensor("memT", [NCH, 128, 256], F32, kind="ExternalInput").ap()
        for k, (shp, dt) in CONST_SPECS.items():
            self.dr[k] = nc.dram_tensor(k, shp, dt, kind="ExternalInput").ap()
        for k, shp in WEIGHT_SPECS.items():
            self.dr[k] = nc.dram_tensor(k, shp, F32, kind="ExternalInput").ap()
        for k, (shp, dt) in SCRATCH.items():
            kind = "Internal"
            if k in ext_in:
                kind = "ExternalInput"
            elif k in ext_out:
                kind = "ExternalOutput"
            self.dr[k] = nc.dram_tensor(k, shp, dt, kind=kind).ap()
        self.dr["outT"] = nc.dram_tensor("outT", [NCH, 128, T], F32, kind="ExternalOutput").ap()
        self.dbg = dbg
        if dbg:
            for k, (shp, dt) in DBG.items():
                self.dr[k] = nc.dram_tensor(k, shp, dt, kind="ExternalOutput").ap()
        self.st = self.P.stack
        self.final = []
        self.sb = {}
        for k in ("pm", "ident", "ones", "vecs", "selg"):
            shp, dt = CONST_SPECS[k]
            self.sb[k] = self.st.enter_context(nc.sbuf_tensor("c_" + k, shp, dt))
            src = self.dr[k]
            self.P.dma("sp", self.sb[k][:], src[:], writes=[("c", k)])
        self.ps = [self.st.enter_context(nc.psum_tensor(f"ps{i}", [128, 512], F32)) for i in range(7)]
        self.psb = self.st.enter_context(nc.psum_tensor("psb", [128, 1024], BF16))
        self.psi = 0
        self._uid = 0

    def uid(self):
        self._uid += 1
        return self._uid

    def sbuf(self, stack, name, shape, dt):
        return stack.enter_context(self.nc.sbuf_tensor(f"{name}_{self.uid()}", shape, dt))

    def cast_begin(self, l, stage):
        dr = self.dr
        jobs = []
        for br, wn in enumerate(("w_up_fox", "w_up_nsa", "w_up_mem")):
            for cg in range(4):
                jobs.append((dr[wn][l, :, cg * 512:(cg + 1) * 512].rearrange("(c p) n -> p c n", p=128), "wcu", br * 4 + cg, 8))
        for cg in range(4):
            jobs.append((dr["w_o"][l, :, cg * 512:(cg + 1) * 512].rearrange("(c p) n -> p c n", p=128), "wcb", cg, NCH))
        for i in range(16):
            jobs.append((dr["w_mlp1"][l, :, i * 512:(i + 1) * 512].rearrange("(c p) n -> p c n", p=128), "wcb", 4 + i, NCH))
        for g in range(4):
            for cg in range(4):
                jobs.append((dr["w_mlp2"][l, g * 2048:(g + 1) * 2048, cg * 512:(cg + 1) * 512].rearrange("(c p) n -> p c n", p=128),
                             "wcb", 20 + g * 4 + cg, NCH))
        self.cjobs = jobs
        self.cstage = stage
        self.cpos = 0

    def cast_step(self):
        P = self.P
        i = self.cpos
        n = len(self.cjobs)
        if i > n:
            return
        if i < n:
            src, dn, idx, nk = self.cjobs[i]
            sb_ = self.cstage[i % 2]
            P.dma("pool", sb_[:, 0:nk, :], src, writes=[("cstage", i % 2)])
        if i >= 1:
            src, dn, idx, nk = self.cjobs[i - 1]
            sb_ = self.cstage[(i - 1) % 2]
            P.dma("pool", self.dr[dn][idx], sb_[:, 0:nk, :], reads=[("cstage", (i - 1) % 2)], writes=[("wc", dn, idx)])
        self.cpos += 1

    def nextps(self, lo=0, hi=7):
        i = lo + (self.psi % (hi - lo))
        self.psi += 1
        return i

    def rmsnorm_stats(self, src, srckey, sq, rstd, tmp, nfree=512):
        P = self.P
        pi = self.nextps()
        ps = self.ps[pi]
        P.op("act", lambda e: e.activation(out=sq[:, :, 0:nfree], in_=src[:, :, 0:nfree], func=AF.Square),
             reads=[srckey], writes=[("sq",)])
        for c in range(NCH):
            P.op("pe", lambda e, c=c: e.matmul(ps[:, 0:nfree], lhsT=self.sb["ones"][:], rhs=sq[:, c, 0:nfree],
                                               start=(c == 0), stop=(c == NCH - 1)),
                 reads=[("sq",), ("c", "ones")], writes=[("ps", pi)])
        P.op("dve", lambda e: e.tensor_scalar(out=tmp[:, 0:nfree], in0=ps[:, 0:nfree], scalar1=1.0 / D, scalar2=EPS,
                                              op0=ALU.mult, op1=ALU.add),
             reads=[("ps", pi)], writes=[("nt",)])
        P.op("act", lambda e: e.activation(out=tmp[:, 0:nfree], in_=tmp[:, 0:nfree], func=AF.Sqrt),
             reads=[("nt",)], writes=[("nt",)])
        P.op("dve", lambda e: e.reciprocal(out=rstd[:, 0:nfree], in_=tmp[:, 0:nfree]),
             reads=[("nt",)], writes=[("rstd",)])

    def phase_A(self, l, src_name):
        nc, P, dr, sb = self.nc, self.P, self.dr, self.sb
        RC = 1024
        NTC = RC // TS
        vb = l * VPL
        with contextlib.ExitStack() as st:
            uT = self.sbuf(st, "A_uT", [128, NCH, RC], BF16)
            hT = self.sbuf(st, "A_hT", [128, NCH, TS], F32)
            sq = self.sbuf(st, "A_sq", [128, NCH, TS], BF16)
            rstd = self.sbuf(st, "A_rstd", [128, TS], F32)
            ntmp = self.sbuf(st, "A_ntmp", [128, TS], F32)
            wb = [self.sbuf(st, f"A_w{i}", [128, NCH, 512], BF16) for i in range(2)]
            NSTG = 6
            stg = [self.sbuf(st, f"A_stg{i}", [128, 512], BF16) for i in range(NSTG)]
            rcs = [self.sbuf(st, f"A_rcs{i}", [32, 2, TS], F32) for i in range(NTC)]
            rt1 = self.sbuf(st, "A_rt1", [32, TS], F32)
            rt2 = self.sbuf(st, "A_rt2", [32, TS], F32)
            ft = self.sbuf(st, "A_ft", [8, TS], F32)
            tiles = []
            for nm in ["fk", "fv", "ff", "ck", "cv", "sk", "sv", "wk", "wv", "fq", "nq", "ng", "mq", "bg"]:
                c0, w = SEG[nm]
                for o in range(0, w, 512):
                    tiles.append((nm, c0 + o, min(512, w - o), o))
            sgi = [0]

            def load_w(i):
                nm, c0, w, o = tiles[i]
                b = i % 2
                P.dma("pool", wb[b][:, :, 0:w],
                      dr["w_in"][l, :, c0:c0 + w].rearrange("(c p) n -> p c n", p=128),
                      writes=[("A_w", b)])

            for ch in range(T // RC):
                t0 = ch * RC
                for tl in range(NTC):
                    ts0 = t0 + tl * TS
                    P.dma("sp", hT[:], dr[src_name][:, :, ts0:ts0 + TS].rearrange("c p t -> p c t"),
                          reads=[(src_name, ts0 // TS)], writes=[("A_hT",)])
                    P.dma("sp", rcs[tl][:], dr["ropecs"][:, :, ts0:ts0 + TS].rearrange("a p t -> p a t"),
                          writes=[("A_rcs", tl)])
                    self.rmsnorm_stats(hT, ("A_hT",), sq, rstd, ntmp)
                    for c in range(NCH):
                        P.op("dve", lambda e, c=c, tl=tl: e.scalar_tensor_tensor(
                            out=uT[:, c, tl * TS:(tl + 1) * TS], in0=hT[:, c, :],
                            scalar=sb["vecs"][:, vb + VG_PRE_MIX + c:vb + VG_PRE_MIX + c + 1], in1=rstd[:],
                            op0=ALU.mult, op1=ALU.mult),
                            reads=[("A_hT",), ("rstd",), ("c", "vecs")], writes=[("A_uT", tl)])
                load_w(0)
                for i, (nm, c0, w, o) in enumerate(tiles):
                    if i + 1 < len(tiles):
                        load_w(i + 1)
                    b = i % 2
                    W = wb[b]
                    tokmajor = nm in ("fv", "sv", "wv")
                    if tokmajor:
                        dst = {"fv": "vf", "sv": "sv", "wv": "wv"}[nm]
                        for tb in range(RC // 128):
                            pi = self.nextps(); ps = self.ps[pi]
                            for c in range(NCH):
                                P.op("pe", lambda e, c=c, tb=tb, ps=ps, W=W, w=w: e.matmul(
                                    ps[:, 0:w], lhsT=uT[:, c, tb * 128:(tb + 1) * 128], rhs=W[:, c, 0:w],
                                    start=(c == 0), stop=(c == NCH - 1)),
                                    reads=[("A_uT", tb // 4), ("A_w", b)], writes=[("ps", pi)])
                            si = sgi[0] % NSTG; sgi[0] += 1
                            S = stg[si]
                            P.op("act", lambda e, S=S, ps=ps, w=w: e.activation(out=S[:, 0:w], in_=ps[:, 0:w], func=AF.Copy),
                                 reads=[("ps", pi)], writes=[("A_stg", si)])
                            r0 = t0 + tb * 128
                            P.dma("sp", dr[dst][r0:r0 + 128, o:o + w], S[:, 0:w],
                                  reads=[("A_stg", si)], writes=[(dst, r0 // 128, o // 512)])
                        continue
                    nblk = (w + 127) // 128
                    for cb in range(nblk):
                        m = min(128, w - cb * 128)
                        for tl in range(NTC):
                            ts0 = t0 + tl * TS
                            tj = ts0 // TS
                            pi = self.nextps(); ps = self.ps[pi]
                            for c in range(NCH):
                                P.op("pe", lambda e, c=c, tl=tl, ps=ps, W=W, cb=cb, m=m: e.matmul(
                                    ps[0:m, :], lhsT=W[:, c, cb * 128:cb * 128 + m], rhs=uT[:, c, tl * TS:(tl + 1) * TS],
                                    start=(c == 0), stop=(c == NCH - 1)),
                                    reads=[("A_uT", tl), ("A_w", b)], writes=[("ps", pi)])
                            gcb = (o // 128) + cb
                            if nm == "ff":
                                P.op("dve", lambda e, ps=ps: e.tensor_scalar(out=ft[:], in0=ps[0:8, :], scalar1=sb["vecs"][0:8, vb + V_NEGBF:vb + V_NEGBF + 1],
                                                                             scalar2=None, op0=ALU.add),
                                     reads=[("ps", pi), ("c", "vecs")], writes=[("A_ft",)])
                                P.op("act", lambda e: e.activation(out=ft[:], in_=ft[:], func=AF.Exp, scale=-1.0),
                                     reads=[("A_ft",)], writes=[("A_ft",)])
                                P.op("act", lambda e: e.activation(out=ft[:], in_=ft[:], func=AF.Ln, bias=1.0),
                                     reads=[("A_ft",)], writes=[("A_ft",)])
                                P.op("dve", lambda e: e.tensor_scalar(out=ft[:], in0=ft[:], scalar1=-1.0, scalar2=None, op0=ALU.mult),
                                     reads=[("A_ft",)], writes=[("A_ft",)])
                                P.dma("sp", dr["logf"][:, ts0:ts0 + TS], ft[:], reads=[("A_ft",)], writes=[("logf", tj)])
                                continue
                            si = sgi[0] % NSTG; sgi[0] += 1
                            S = stg[si]
                            if nm in ("bg", "ng"):
                                P.op("act", lambda e, S=S, ps=ps, m=m: e.activation(out=S[0:m, :], in_=ps[0:m, :], func=AF.Sigmoid),
                                     reads=[("ps", pi)], writes=[("A_stg", si)])
                            else:
                                sc = 1.0
                                if nm in ("fq", "nq"):
                                    sc = 128.0 ** -0.5
                                elif nm == "mq":
                                    sc = 256.0 ** -0.5
                                P.op("act", lambda e, S=S, ps=ps, m=m, sc=sc: e.activation(out=S[0:m, :], in_=ps[0:m, :], func=AF.Copy, scale=sc),
                                     reads=[("ps", pi)], writes=[("A_stg", si)])
                            if nm in ("nq", "ck", "sk", "wk"):
                                pj = self.nextps(); ps2 = self.ps[pj]
                                P.op("pe", lambda e, S=S, ps2=ps2: e.matmul(ps2[0:32, :], lhsT=sb["pm"][:], rhs=S[0:32, :], start=True, stop=True),
                                     reads=[("A_stg", si), ("c", "pm")], writes=[("ps", pj)])
                                P.op("dve", lambda e, S=S, tl=tl: e.tensor_tensor(out=rt1[:], in0=S[0:32, :], in1=rcs[tl][:, 0, :], op=ALU.mult),
                                     reads=[("A_stg", si), ("A_rcs", tl)], writes=[("A_rt1",)])
                                P.op("dve", lambda e, ps2=ps2, tl=tl: e.tensor_tensor(out=rt2[:], in0=ps2[0:32, :], in1=rcs[tl][:, 1, :], op=ALU.mult),
                                     reads=[("ps", pj), ("A_rcs", tl)], writes=[("A_rt2",)])
                                P.op("dve", lambda e, S=S: e.tensor_tensor(out=S[0:32, :], in0=rt1[:], in1=rt2[:], op=ALU.add),
                                     reads=[("A_rt1",), ("A_rt2",)], writes=[("A_stg", si)])
                            dstn = {"fq": "qfT", "fk": "kfT", "nq": "qnT", "ck": "ckT", "cv": "cvT", "sk": "skT", "wk": "wkT",
                                    "ng": "gnT", "mq": "mqT", "bg": "bgT"}[nm]
                            if nm == "ng":
                                P.dma("sp", dr["gnT"][:, ts0:ts0 + TS], S[0:24, :], reads=[("A_stg", si)], writes=[("gnT", tj)])
                            else:
                                P.dma("sp", dr[dstn][gcb, :, ts0:ts0 + TS], S[:, :], reads=[("A_stg", si)], writes=[(dstn, gcb, tj)])
        P.barrier()


def _phase_B(self, l):
    nc, P, dr, sb = self.nc, self.P, self.dr, self.sb
    vb = l * VPL
    ps = self.ps
    ident, ones = sb["ident"], sb["ones"]
    KI, KO = ("c", "ident"), ("c", "ones")

    with contextlib.ExitStack() as stB:
        kcT = [self.sbuf(stB, f"B_kcT{g}", [128, 256], BF16) for g in range(2)]
        vc = [self.sbuf(stB, f"B_vc{g}", [128, 2, 128], BF16) for g in range(2)]
        mkT = self.sbuf(stB, "B_mkT", [128, 8, 256], BF16)
        mv = self.sbuf(stB, "B_mv", [128, 2, 1024], BF16)
        Pt = [self.sbuf(stB, f"B_P{i}", [128, 512], BF16) for i in range(4)]
        zt = self.sbuf(stB, "B_zt", [128, 512], F32)
        rz = self.sbuf(stB, "B_rz", [128, 512], F32)
        wt = self.sbuf(stB, "B_wt", [128, 512], F32)
        ctr = self.sbuf(stB, "B_ctr", [128, 512], F32)
        ostg = [self.sbuf(stB, f"B_ostg{i}", [128, 512], BF16) for i in range(4)]
        cnt = {"p": 0, "o": 0, "s": 0, "oz": 0}
        cstage = [self.sbuf(stB, f"B_cst{i}", [128, NCH, 512], BF16) for i in range(2)]
        self.cast_begin(l, cstage)

        with contextlib.ExitStack() as st:
            lf = self.sbuf(st, "B0_lf", [8, T], F32)
            on8 = self.sbuf(st, "B0_on8", [8, T], F32)
            cc = self.sbuf(st, "B0_c", [8, T], F32)
            cb16 = self.sbuf(st, "B0_cb", [8, 6, T], BF16)
            cf = on8
            P.dma("sp", lf[:], dr["logf"][:, :], reads=[("logf", j) for j in range(NT)], writes=[("B0_lf",)])
            P.op("pool", lambda e: e.memset(on8[:], 1.0), writes=[("B0_on8",)])
            P.op("dve", lambda e: e.tensor_tensor_scan(out=cc[:], data0=on8[:], data1=lf[:], initial=0.0,
                                                       op0=ALU.mult, op1=ALU.add),
                 reads=[("B0_lf",), ("B0_on8",)], writes=[("B0_c",)])
            for i in range(3):
                P.op("dve", lambda e, i=i: e.tensor_copy(out=cb16[:, i, :], in_=cc[:]), reads=[("B0_c",)], writes=[("B0_cb", i)])
                P.op("dve", lambda e, i=i: e.tensor_scalar(out=cb16[:, 3 + i, :], in0=cb16[:, i, :], scalar1=-1.0, scalar2=None, op0=ALU.mult),
                     reads=[("B0_cb", i)], writes=[("B0_cb", 3 + i)])
                if i < 2:
                    P.op("dve", lambda e, i=i: e.tensor_copy(out=cf[:], in_=cb16[:, i, :]), reads=[("B0_cb", i)], writes=[("B0_on8",)])
                    P.op("dve", lambda e: e.tensor_tensor(out=cc[:], in0=cc[:], in1=cf[:], op=ALU.subtract),
                         reads=[("B0_c",), ("B0_on8",)], writes=[("B0_c",)])
            P.dma("sp", dr["cs3"][:, :, :], cb16[:], reads=[("B0_cb", i) for i in range(6)], writes=[("cs3",)])
        P.barrier()
        with contextlib.ExitStack() as st:
            mT = self.sbuf(st, "B0_mT", [128, NCH, 256], F32)
            msq = self.sbuf(st, "B0_msq", [128, NCH, 256], BF16)
            umT = self.sbuf(st, "B0_umT", [128, NCH, 256], BF16)
            wbm = [self.sbuf(st, f"B0_wm{i}", [128, NCH, 512], BF16) for i in range(2)]
            P.dma("sp", mT[:], dr["memT"].rearrange("c p t -> p c t"), writes=[("B0_mT",)])
            self.rmsnorm_stats(mT, ("B0_mT",), msq, rz, zt, nfree=256)
            for c in range(NCH):
                P.op("dve", lambda e, c=c: e.scalar_tensor_tensor(
                    out=umT[:, c, :], in0=mT[:, c, :], scalar=sb["vecs"][:, vb + VG_MEM + c:vb + VG_MEM + c + 1],
                    in1=rz[:, 0:256], op0=ALU.mult, op1=ALU.mult),
                    reads=[("B0_mT",), ("rstd",), ("c", "vecs")], writes=[("B0_umT",)])
            for ti in range(4):
                b = ti % 2
                P.dma("pool", wbm[b][:], dr["w_mem_kv"][l, :, ti * 512:(ti + 1) * 512].rearrange("(c p) n -> p c n", p=128),
                      writes=[("B0_w", b)])
                if ti < 2:
                    for cbk in range(4):
                        pi = self.nextps(); pp = ps[pi]
                        for c in range(NCH):
                            P.op("pe", lambda e, c=c, pp=pp, b=b, cbk=cbk: e.matmul(
                                pp[:, 0:256], lhsT=wbm[b][:, c, cbk * 128:(cbk + 1) * 128], rhs=umT[:, c, :],
                                start=(c == 0), stop=(c == NCH - 1)),
                                reads=[("B0_umT",), ("B0_w", b)], writes=[("ps", pi)])
                        P.op("act", lambda e, pp=pp, ti=ti, cbk=cbk: e.activation(out=mkT[:, ti * 4 + cbk, :], in_=pp[:, 0:256], func=AF.Copy),
                             reads=[("ps", pi)], writes=[("B_mkT",)])
                else:
                    for mt in range(2):
                        pi = self.nextps(); pp = ps[pi]
                        for c in range(NCH):
                            P.op("pe", lambda e, c=c, pp=pp, b=b, mt=mt: e.matmul(
                                pp[:, :], lhsT=umT[:, c, mt * 128:(mt + 1) * 128], rhs=wbm[b][:, c, :],
                                start=(c == 0), stop=(c == NCH - 1)),
                                reads=[("B0_umT",), ("B0_w", b)], writes=[("ps", pi)])
                        P.op("act", lambda e, pp=pp, ti=ti, mt=mt: e.activation(out=mv[:, mt, (ti - 2) * 512:(ti - 1) * 512], in_=pp[:, :], func=AF.Copy),
                             reads=[("ps", pi)], writes=[("B_mv",)])

            xc = self.sbuf(st, "B0_xc", [128, T], BF16)
            w1 = self.sbuf(st, "B0_w1", [128, 32, 128], BF16)
            w2 = self.sbuf(st, "B0_w2", [128, 128], BF16)
            peT = self.sbuf(st, "B0_peT", [128, 32], BF16)
            bsb = self.sbuf(st, "B0_bsb", [128, 1], F32)
            xg = self.sbuf(st, "B0_xg", [128, 256], F32)
            x2 = self.sbuf(st, "B0_x2", [128, 256], F32)
            hd = self.sbuf(st, "B0_hd", [128, 256], BF16)
            P.op("pool", lambda e: e.memset(xg[:], 0.0), writes=[("B0_xg",)])
            for kv in range(2):
                sfx = "k" if kv == 0 else "v"
                P.dma("pool", w1[:], dr["w_cmp1_" + sfx][l].rearrange("(j d) o -> d j o", d=128), writes=[("B0_w1",)])
                P.dma("pool", w2[:], dr["w_cmp2_" + sfx][l], writes=[("B0_w2",)])
                P.op("pool", lambda e, sfx=sfx: e.dma_start(out=peT[:], in_=dr["pe_cmp_" + sfx][l].rearrange("j d -> d j"),
                                                            allow_slow_non_contiguous=True), writes=[("B0_peT",)], dma=True)
                for g in range(2):
                    srcn = "ckT" if kv == 0 else "cvT"
                    P.dma("sp", xc[:], dr[srcn][g, :, :], reads=[(srcn, g, j) for j in range(NT)], writes=[("B0_xc",)])
                    pi = self.nextps(); pp = ps[pi]
                    for j in range(32):
                        P.op("pe", lambda e, j=j, pp=pp: e.matmul(pp[:, 0:255], lhsT=w1[:, j, :], rhs=xc[:, j:j + 16 * 254 + 1:16],
                                                                  start=(j == 0), stop=(j == 31)),
                             reads=[("B0_xc",), ("B0_w1",)], writes=[("ps", pi)])
                    pj = self.nextps(); pp2 = ps[pj]
                    for j in range(32):
                        P.op("pe", lambda e, j=j, pp2=pp2: e.matmul(pp2[:, 0:1], lhsT=w1[:, j, :], rhs=peT[:, j:j + 1],
                                                                    start=(j == 0), stop=(j == 31)),
                             reads=[("B0_peT",), ("B0_w1",)], writes=[("ps", pj)])
                    P.op("dve", lambda e, pp2=pp2: e.tensor_copy(out=bsb[:], in_=pp2[:, 0:1]), reads=[("ps", pj)], writes=[("B0_bsb",)])
                    P.op("dve", lambda e, pp=pp: e.tensor_scalar(out=xg[:, 0:255], in0=pp[:, 0:255], scalar1=bsb[:, 0:1], scalar2=None, op0=ALU.add),
                         reads=[("ps", pi), ("B0_bsb",)], writes=[("B0_xg",)])
                    P.op("dve", lambda e: e.tensor_tensor(out=x2[:], in0=xg[:], in1=xg[:], op=ALU.mult), reads=[("B0_xg",)], writes=[("B0_x2",)])
                    P.op("dve", lambda e: e.tensor_scalar(out=x2[:], in0=x2[:], scalar1=0.044715, scalar2=1.0, op0=ALU.mult, op1=ALU.add),
                         reads=[("B0_x2",)], writes=[("B0_x2",)])
                    P.op("dve", lambda e: e.tensor_tensor(out=x2[:], in0=x2[:], in1=xg[:], op=ALU.mult), reads=[("B0_x2",), ("B0_xg",)], writes=[("B0_x2",)])
                    P.op("act", lambda e: e.activation(out=x2[:], in_=x2[:], func=AF.Tanh, scale=0.7978845608028654),
                         reads=[("B0_x2",)], writes=[("B0_x2",)])
                    P.op("dve", lambda e: e.tensor_scalar(out=x2[:], in0=x2[:], scalar1=1.0, scalar2=0.5, op0=ALU.add, op1=ALU.mult),
                         reads=[("B0_x2",)], writes=[("B0_x2",)])
                    P.op("dve", lambda e: e.tensor_tensor(out=hd[:], in0=x2[:], in1=xg[:], op=ALU.mult), reads=[("B0_x2",), ("B0_xg",)], writes=[("B0_hd",)])
                    if kv == 0:
                        pk = self.nextps(); pp3 = ps[pk]
                        P.op("pe", lambda e, pp3=pp3: e.matmul(pp3[:, 0:256], lhsT=w2[:], rhs=hd[:], start=True, stop=True),
                             reads=[("B0_hd",), ("B0_w2",)], writes=[("ps", pk)])
                        P.op("act", lambda e, pp3=pp3, g=g: e.activation(out=kcT[g][:], in_=pp3[:, 0:256], func=AF.Copy),
                             reads=[("ps", pk)], writes=[("B_kcT", g)])
                    else:
                        for nt in range(2):
                            pk = self.nextps(); pp3 = ps[pk]
                            P.op("pe", lambda e, pp3=pp3, nt=nt: e.matmul(pp3[:, 0:128], lhsT=hd[:, nt * 128:(nt + 1) * 128], rhs=w2[:], start=True, stop=True),
                                 reads=[("B0_hd",), ("B0_w2",)], writes=[("ps", pk)])
                            P.op("act", lambda e, pp3=pp3, g=g, nt=nt: e.activation(out=vc[g][:, nt, :], in_=pp3[:, 0:128], func=AF.Copy),
                                 reads=[("ps", pk)], writes=[("B_vc", g)])
        P.barrier()

        def attn(blocks, pv_sets, obanks, zbank):
            nb = len(blocks)
            pend = []
            for bi, blk in enumerate(blocks):
                si = cnt["s"] % 2; cnt["s"] += 1
                S = ps[si]
                ns = len(blk["s"])
                for mi, (lt, rh, keys) in enumerate(blk["s"]):
                    P.op("pe", lambda e, S=S, lt=lt, rh=rh, mi=mi, ns=ns: e.matmul(S[:, :], lhsT=lt, rhs=rh, start=(mi == 0), stop=(mi == ns - 1)),
                         reads=keys, writes=[("ps", si)])
                pidx = cnt["p"] % 4; cnt["p"] += 1
                Pb = Pt[pidx]
                P.op("act", lambda e, S=S, Pb=Pb: e.activation(out=Pb[:], in_=S[:, :], func=AF.Exp),
                     reads=[("ps", si)], writes=[("B_P", pidx)])
                pend.append((bi, blk, Pb, pidx))
                if len(pend) == 2:
                    emit_pv(pend.pop(0), nb, obanks, zbank)
            while pend:
                emit_pv(pend.pop(0), nb, obanks, zbank)

        def emit_pv(item, nb, obanks, zbank):
            bi, blk, Pb, pidx = item
            for oi, ob in enumerate(obanks):
                lt = blk["v"][oi]
                P.op("pe", lambda e, ob=ob, lt=lt, Pb=Pb, bi=bi, nb=nb: e.matmul(ps[ob][:, :], lhsT=lt, rhs=Pb[:], start=(bi == 0), stop=(bi == nb - 1)),
                     reads=[("B_P", pidx)] + blk["vkeys"], writes=[("ps", ob)])
            P.op("pe", lambda e, Pb=Pb, bi=bi, nb=nb: e.matmul(ps[zbank][:, :], lhsT=ones[:], rhs=Pb[:], start=(bi == 0), stop=(bi == nb - 1)),
                 reads=[("B_P", pidx), KO], writes=[("ps", zbank)])

        def next_oz():
            k = cnt["oz"] % 2; cnt["oz"] += 1
            return 2 + k, 4 + k

        def next_ostg():
            k = cnt["o"] % 4; cnt["o"] += 1
            return k

        with contextlib.ExitStack() as st:
            causal = self.sbuf(st, "N_causal", [128, 4, 512], BF16)
            winm = self.sbuf(st, "N_winm", [128, 8, 512], BF16)
            ov = self.sbuf(st, "N_ov", [128, 2, 65], BF16)
            asel = self.sbuf(st, "N_asel", [128, 32, 128], BF16)
            P.dma("sp", causal[:], dr["causal"][:], writes=[("N_causal",)])
            P.dma("sp", winm[:], dr["winmask"][:], writes=[("N_winm",)])
            P.dma("sp", ov[:], dr["ov"][:], writes=[("N_ov",)])
            P.dma("sp", asel[:], dr["asel"][:], writes=[("N_asel",)])
            skT = self.sbuf(st, "N_skT", [128, T], BF16)
            svs = self.sbuf(st, "N_sv", [128, NB, 128], BF16)
            wkT = self.sbuf(st, "N_wkT", [128, T], BF16)
            wvs = self.sbuf(st, "N_wv", [128, NB, 128], BF16)
            selT = self.sbuf(st, "N_selT", [128, T], BF16)
            P.op("pool", lambda e: e.memset(selT[:], 0.0), writes=[("N_selT", q) for q in range(NB)])
            q4 = [self.sbuf(st, f"N_q4_{i}", [128, 4, 512], BF16) for i in range(2)]
            cmk = [self.sbuf(st, f"N_cmk{i}", [128, 2, 512], BF16) for i in range(2)]
            stb = [self.sbuf(st, f"N_stb{i}", [128, 3, 4, 64], F32) for i in range(2)]
            gnt = [self.sbuf(st, f"N_gn{i}", [128, 512], BF16) for i in range(2)]
            for i_ in range(2):
                P.op("pool", lambda e, i_=i_: e.memset(gnt[i_][:], 0.0), writes=[("N_gn", i_)])
            acc = [self.sbuf(st, f"N_acc{i}", [128, 512], F32) for i in range(4)]
            pslc = self.sbuf(st, "N_pslc", [128, 4, 64], F32)
            z4 = self.sbuf(st, "N_z4", [128, 4], F32)
            sc = self.sbuf(st, "N_sc", [128, 64], F32)
            sc2 = self.sbuf(st, "N_sc2", [128, 64], F32)
            m8 = self.sbuf(st, "N_m8", [128, 8], F32)
            m8b = self.sbuf(st, "N_m8b", [128, 8], F32)
            sbb = self.sbuf(st, "N_sbb", [128, 64], BF16)

            def gate_w(row, jb, zb):
                P.op("pe", lambda e, row=row, jb=jb: e.matmul(ps[6][:, :], lhsT=sb["selg"][:, row, :], rhs=gnt[jb][:], start=True, stop=True),
                     reads=[("N_gn", jb), ("c", "selg")], writes=[("ps", 6)])
                P.op("dve", lambda e, zb=zb: e.tensor_scalar(out=zt[:], in0=ps[zb][:, :], scalar1=1e-30, scalar2=None, op0=ALU.add),
                     reads=[("ps", zb)], writes=[("B_zt",)])
                P.op("dve", lambda e: e.reciprocal(out=rz[:], in_=zt[:]), reads=[("B_zt",)], writes=[("B_rz",)])
                P.op("dve", lambda e: e.tensor_tensor(out=wt[:], in0=ps[6][:, :], in1=rz[:], op=ALU.mult),
                     reads=[("ps", 6), ("B_rz",)], writes=[("B_wt",)])

            for g in range(2):
                P.dma("sp", skT[:], dr["skT"][g, :, :], reads=[("skT", g, j) for j in range(NT)], writes=[("N_skT",)])
                P.dma("sp", wkT[:], dr["wkT"][g, :, :], reads=[("wkT", g, j) for j in range(NT)], writes=[("N_wkT",)])
                P.dma("sp", svs[:], dr["sv"][:, g * 128:(g + 1) * 128].rearrange("(b p) d -> p b d", p=128),
                      reads=[("sv", r, 0) for r in range(NB)], writes=[("N_sv",)])
                P.dma("sp", wvs[:], dr["wv"][:, g * 128:(g + 1) * 128].rearrange("(b p) d -> p b d", p=128),
                      reads=[("wv", r, 0) for r in range(NB)], writes=[("N_wv",)])
                for j in range(NT):
                    jb = j % 2
                    t0 = j * TS
                    Q = q4[jb]
                    for hh in range(4):
                        h = 4 * g + hh
                        P.dma("sp", Q[:, hh, :], dr["qnT"][h, :, t0:t0 + TS], reads=[("qnT", h, j)], writes=[("N_q4", jb, hh)])
                    P.dma("sp", cmk[jb][:], dr["cmpmask"][:, j, :, :], writes=[("N_cmk", jb)])
                    P.dma("sp", stb[jb][:], dr["seltab"][:, :, 4 * j:4 * j + 4, :], writes=[("N_stb", jb)])
                    P.dma("sp", gnt[jb][0:24, :], dr["gnT"][:, t0:t0 + TS], reads=[("gnT", j)], writes=[("N_gn", jb)])
                    nts = [0] if j < 4 else [0, 1]
                    for hh in range(4):
                        h = 4 * g + hh
                        qa = Q[:, hh, :]
                        qk = ("N_q4", jb, hh)
                        ob, zb = next_oz()
                        blocks = []
                        for nt in nts:
                            blocks.append(dict(
                                s=[(kcT[g][:, nt * 128:(nt + 1) * 128], qa, [("B_kcT", g), qk]),
                                   (ident[:], cmk[jb][:, nt, :], [KI, ("N_cmk", jb)])],
                                v=[vc[g][:, nt, :]], vkeys=[("B_vc", g)]))
                        p_start = cnt["p"]
                        attn(blocks, 1, [ob], zb)
                        Es = [((p_start + i) % 4) for i in range(len(nts))]
                        for r in range(4):
                            for i, nt in enumerate(nts):
                                Eb = Pt[Es[i]]
                                P.op("pe", lambda e, r=r, Eb=Eb, nt=nt, i=i, n=len(nts): e.matmul(
                                    ps[6][:, r * 128:r * 128 + 65], lhsT=Eb[:, r * 128:(r + 1) * 128], rhs=ov[:, nt, :],
                                    start=(i == 0), stop=(i == n - 1)),
                                    reads=[("B_P", Es[i]), ("N_ov",)], writes=[("ps", 6)])
                        p6 = ps[6][:, :].rearrange("p (r k) -> p r k", k=128)
                        P.op("dve", lambda e, p6=p6: e.tensor_scalar(out=z4[:], in0=p6[:, :, 64], scalar1=1e-30, scalar2=None, op0=ALU.add),
                             reads=[("ps", 6)], writes=[("N_z4",)])
                        P.op("dve", lambda e: e.reciprocal(out=z4[:], in_=z4[:]), reads=[("N_z4",)], writes=[("N_z4",)])
                        for r in range(4):
                            if hh == 0:
                                P.op("dve", lambda e, r=r, p6=p6: e.tensor_scalar(out=pslc[:, r, :], in0=p6[:, r, 0:64], scalar1=z4[:, r:r + 1], scalar2=None, op0=ALU.mult),
                                     reads=[("ps", 6), ("N_z4",)], writes=[("N_pslc",)])
                            else:
                                P.op("dve", lambda e, r=r, p6=p6: e.scalar_tensor_tensor(out=pslc[:, r, :], in0=p6[:, r, 0:64], scalar=z4[:, r:r + 1], in1=pslc[:, r, :],
                                                                                         op0=ALU.mult, op1=ALU.add),
                                     reads=[("ps", 6), ("N_z4",), ("N_pslc",)], writes=[("N_pslc",)])
                        gate_w(0 * 8 + h, jb, zb)
                        P.op("dve", lambda e, ob=ob, hh=hh: e.tensor_tensor(out=acc[hh][:], in0=ps[ob][:, :], in1=wt[:], op=ALU.mult),
                             reads=[("ps", ob), ("B_wt",)], writes=[("N_acc", hh)])
                    self.cast_step()
                    if getattr(self, "dbg", False) and g == 0 and j in (0, 1, 2):
                        dj = j
                        P.dma("sp", dr["d_acc"][dj], acc[0][:], reads=[("N_acc", 0)])
                        P.dma("sp", dr["d_pslc"][dj], pslc[:], reads=[("N_pslc",)])
                        P.dma("sp", dr["d_gn"][dj], gnt[jb][0:24, :], reads=[("N_gn", jb)])
                        P.dma("sp", dr["d_cmk"][dj], cmk[jb][:], reads=[("N_cmk", jb)])
                        P.dma("sp", dr["d_q4"][dj], Q[:], reads=[("N_q4", jb, hh) for hh in range(4)])
                        P.dma("sp", dr["d_stb"][dj], stb[jb][:], reads=[("N_stb", jb)])
                    for r in range(4):
                        qb = 4 * j + r
                        P.op("dve", lambda e, r=r, jb=jb: e.tensor_tensor(out=sc[:], in0=pslc[:, r, :], in1=stb[jb][:, 0, r, :], op=ALU.mult),
                             reads=[("N_pslc",), ("N_stb", jb)], writes=[("N_sc",)])
                        P.op("dve", lambda e, r=r, jb=jb: e.tensor_tensor(out=sc[:], in0=sc[:], in1=stb[jb][:, 1, r, :], op=ALU.add),
                             reads=[("N_sc",), ("N_stb", jb)], writes=[("N_sc",)])
                        P.op("dve", lambda e: e.max(out=m8[:], in_=sc[:]), reads=[("N_sc",)], writes=[("N_m8",)])
                        P.op("dve", lambda e: e.match_replace(out=sc2[:], in_to_replace=m8[:], in_values=sc[:], imm_value=-1e9),
                             reads=[("N_sc",), ("N_m8",)], writes=[("N_sc2",)])
                        P.op("dve", lambda e: e.max(out=m8b[:], in_=sc2[:]), reads=[("N_sc2",)], writes=[("N_m8b",)])
                        P.op("dve", lambda e: e.tensor_scalar(out=sc2[:], in0=sc[:], scalar1=m8b[:, 7:8], scalar2=None, op0=ALU.is_ge),
                             reads=[("N_sc",), ("N_m8b",)], writes=[("N_sc2",)])
                        P.op("dve", lambda e, r=r, jb=jb: e.tensor_tensor(out=sc2[:], in0=sc2[:], in1=stb[jb][:, 2, r, :], op=ALU.mult),
                             reads=[("N_sc2",), ("N_stb", jb)], writes=[("N_sc2",)])
                        P.op("dve", lambda e: e.tensor_scalar(out=sbb[:], in0=sc2[:], scalar1=BIG, scalar2=-BIG, op0=ALU.mult, op1=ALU.add),
                             reads=[("N_sc2",)], writes=[("N_sbb",)])
                        P.op("pe", lambda e: e.transpose(out=self.psb[0:64, 0:128], in_=sbb[:], identity=ident[:]),
                             reads=[("N_sbb",), KI], writes=[("psb",)])
                        P.op("act", lambda e, qb=qb: e.activation(out=selT[0:64, qb * 128:(qb + 1) * 128], in_=self.psb[0:64, 0:128], func=AF.Copy),
                             reads=[("psb",)], writes=[("N_selT", qb)])
                    for hh in range(4):
                        h = 4 * g + hh
                        qa = Q[:, hh, :]
                        qk = ("N_q4", jb, hh)
                        ob, zb = next_oz()
                        blocks = []
                        for kb in range(4 * j + 4):
                            s = [(skT[:, kb * 128:(kb + 1) * 128], qa, [("N_skT",), qk]),
                                 (asel[:, kb, :], selT[:, t0:t0 + TS], [("N_asel",)] + [("N_selT", 4 * j + r) for r in range(4)])]
                            if kb >= 4 * j:
                                s.append((ident[:], causal[:, kb - 4 * j, :], [KI, ("N_causal",)]))
                            blocks.append(dict(s=s, v=[svs[:, kb, :]], vkeys=[("N_sv",)]))
                        attn(blocks, 1, [ob], zb)
                        gate_w(1 * 8 + h, jb, zb)
                        P.op("dve", lambda e, ob=ob: e.tensor_tensor(out=ctr[:], in0=ps[ob][:, :], in1=wt[:], op=ALU.mult),
                             reads=[("ps", ob), ("B_wt",)], writes=[("B_ctr",)])
                        P.op("pool", lambda e, hh=hh: e.tensor_tensor(out=acc[hh][:], in0=acc[hh][:], in1=ctr[:], op=ALU.add),
                             reads=[("N_acc", hh), ("B_ctr",)], writes=[("N_acc", hh)])
                    self.cast_step()
                    for hh in range(4):
                        h = 4 * g + hh
                        qa = Q[:, hh, :]
                        qk = ("N_q4", jb, hh)
                        ob, zb = next_oz()
                        blocks = []
                        for kb in range(max(0, 4 * j - 4), 4 * j + 4):
                            s = [(wkT[:, kb * 128:(kb + 1) * 128], qa, [("N_wkT",), qk]),
                                 (ident[:], winm[:, kb - 4 * j + 4, :], [KI, ("N_winm",)])]
                            blocks.append(dict(s=s, v=[wvs[:, kb, :]], vkeys=[("N_wv",)]))
                        attn(blocks, 1, [ob], zb)
                        gate_w(2 * 8 + h, jb, zb)
                        P.op("dve", lambda e, ob=ob: e.tensor_tensor(out=ctr[:], in0=ps[ob][:, :], in1=wt[:], op=ALU.mult),
                             reads=[("ps", ob), ("B_wt",)], writes=[("B_ctr",)])
                        oi = next_ostg()
                        P.op("pool", lambda e, hh=hh, oi=oi: e.tensor_tensor(out=ostg[oi][:], in0=acc[hh][:], in1=ctr[:], op=ALU.add),
                             reads=[("N_acc", hh), ("B_ctr",)], writes=[("B_ostg", oi)])
                        P.dma("pool", dr["oT"][8 + h, :, t0:t0 + TS], ostg[oi][:], reads=[("B_ostg", oi)], writes=[("oT", 8 + h, j)])
                    self.cast_step()
                if getattr(self, "dbg", False) and g == 0:
                    P.dma("sp", dr["d_selT"][:, :], selT[0:64, :], reads=[("N_selT", q) for q in range(NB)])
        P.barrier()

        with contextlib.ExitStack() as st:
            causal = self.sbuf(st, "F_causal", [128, 4, 512], BF16)
            P.dma("sp", causal[:], dr["causal"][:], writes=[("F_causal",)])
            kT = [self.sbuf(st, f"F_kT{i}", [128, T], BF16) for i in range(2)]
            vs = [self.sbuf(st, f"F_v{i}", [128, NB, 128], BF16) for i in range(2)]
            q6 = [self.sbuf(st, f"F_q6{i}", [128, T], BF16) for i in range(2)]
            k6 = [self.sbuf(st, f"F_k6{i}", [128, T], BF16) for i in range(2)]
            for i_ in range(2):
                P.op("pool", lambda e, i_=i_: e.memset(q6[i_][:], 0.0), writes=[("F_q6", i_)])
                P.op("pool", lambda e, i_=i_: e.memset(k6[i_][:], 0.0), writes=[("F_k6", i_)])
            qt = [self.sbuf(st, f"F_q{i}", [128, 512], BF16) for i in range(2)]
            qc = 0
            for h in range(8):
                hb = h % 2
                P.dma("sp", kT[hb][:], dr["kfT"][h, :, :], reads=[("kfT", h, j) for j in range(NT)], writes=[("F_kT", hb)])
                P.dma("sp", vs[hb][:], dr["vf"][:, h * 128:(h + 1) * 128].rearrange("(b p) d -> p b d", p=128),
                      reads=[("vf", r, h // 4) for r in range(NB)], writes=[("F_v", hb)])
                P.op("pool", lambda e, hb=hb: e.memset(q6[hb][0:6, :], 1.0), writes=[("F_q6", hb)])
                P.op("pool", lambda e, hb=hb: e.memset(k6[hb][0:6, :], 1.0), writes=[("F_k6", hb)])
                P.dma("sp", q6[hb][0:3, :], dr["cs3"][h, 0:3, :], reads=[("cs3",)], writes=[("F_q6", hb)])
                P.dma("sp", k6[hb][3:6, :], dr["cs3"][h, 3:6, :], reads=[("cs3",)], writes=[("F_k6", hb)])
                for j in range(NT):
                    t0 = j * TS
                    qb_ = qc % 2; qc += 1
                    P.dma("sp", qt[qb_][:], dr["qfT"][h, :, t0:t0 + TS], reads=[("qfT", h, j)], writes=[("F_q", qb_)])
                    ob, zb = next_oz()
                    blocks = []
                    for kb in range(4 * j + 4):
                        s = [(kT[hb][:, kb * 128:(kb + 1) * 128], qt[qb_][:], [("F_kT", hb), ("F_q", qb_)]),
                             (k6[hb][:, kb * 128:(kb + 1) * 128], q6[hb][:, t0:t0 + TS], [("F_k6", hb), ("F_q6", hb)])]
                        if kb >= 4 * j:
                            s.append((ident[:], causal[:, kb - 4 * j, :], [KI, ("F_causal",)]))
                        blocks.append(dict(s=s, v=[vs[hb][:, kb, :]], vkeys=[("F_v", hb)]))
                    attn(blocks, 1, [ob], zb)
                    P.op("dve", lambda e, zb=zb: e.reciprocal(out=rz[:], in_=ps[zb][:, :]), reads=[("ps", zb)], writes=[("B_rz",)])
                    oi = next_ostg()
                    P.op("dve", lambda e, ob=ob, oi=oi: e.tensor_tensor(out=ostg[oi][:], in0=ps[ob][:, :], in1=rz[:], op=ALU.mult),
                         reads=[("ps", ob), ("B_rz",)], writes=[("B_ostg", oi)])
                    P.dma("pool", dr["oT"][h, :, t0:t0 + TS], ostg[oi][:], reads=[("B_ostg", oi)], writes=[("oT", h, j)])
                    self.cast_step()
        P.barrier()

        with contextlib.ExitStack() as st:
            mq = [self.sbuf(st, f"M_q{i}", [128, 2, 512], BF16) for i in range(2)]
            qc = 0
            for h in range(4):
                for j in range(NT):
                    t0 = j * TS
                    qb_ = qc % 2; qc += 1
                    for c in range(2):
                        P.dma("sp", mq[qb_][:, c, :], dr["mqT"][h * 2 + c, :, t0:t0 + TS], reads=[("mqT", h * 2 + c, j)], writes=[("M_q", qb_, c)])
                    blocks = []
                    for mt in range(2):
                        s = [(mkT[:, h * 2 + c, mt * 128:(mt + 1) * 128], mq[qb_][:, c, :], [("B_mkT",), ("M_q", qb_, c)]) for c in range(2)]
                        blocks.append(dict(s=s, v=[mv[:, mt, h * 256 + c * 128:h * 256 + (c + 1) * 128] for c in range(2)], vkeys=[("B_mv",)]))
                    attn(blocks, 2, [2, 3], 4)
                    P.op("dve", lambda e: e.reciprocal(out=rz[:], in_=ps[4][:, :]), reads=[("ps", 4)], writes=[("B_rz",)])
                    for c in range(2):
                        oi = next_ostg()
                        P.op("dve", lambda e, c=c, oi=oi: e.tensor_tensor(out=ostg[oi][:], in0=ps[2 + c][:, :], in1=rz[:], op=ALU.mult),
                             reads=[("ps", 2 + c), ("B_rz",)], writes=[("B_ostg", oi)])
                        P.dma("pool", dr["oT"][16 + h * 2 + c, :, t0:t0 + TS], ostg[oi][:], reads=[("B_ostg", oi)], writes=[("oT", 16 + h * 2 + c, j)])
                    self.cast_step()
    P.barrier()


Builder.phase_B = _phase_B


def _phase_C(self, l, src_name, dst_name):
    nc, P, dr, sb = self.nc, self.P, self.dr, self.sb
    vb = l * VPL
    ps = self.ps
    vec = sb["vecs"]
    with contextlib.ExitStack() as st:
        oTt = self.sbuf(st, "C_oT", [128, 24, TS], BF16)
        NG = 4
        gts = [self.sbuf(st, f"C_g{i}", [128, TS], BF16) for i in range(NG)]
        NWB = 4
        Wb = [self.sbuf(st, f"C_W{i}", [128, NCH, 512], BF16) for i in range(NWB)]
        mrg = self.sbuf(st, "C_mrg", [128, NCH, TS], BF16)
        y = self.sbuf(st, "C_y", [128, NCH, TS], F32)
        h = self.sbuf(st, "C_h", [128, NCH, TS], F32)
        ag = self.sbuf(st, "C_ag", [128, NCH, TS], BF16)
        tmp = [self.sbuf(st, f"C_t{i}", [128, TS], F32) for i in range(3)]
        rl = [self.sbuf(st, f"C_rl{i}", [128, TS], BF16) for i in range(2)]
        rstd = self.sbuf(st, "C_rstd", [128, TS], F32)
        ntmp = self.sbuf(st, "C_ntmp", [128, TS], F32)
        wi = [0]
        gi = [0]

        def loadW(dn, idx, nk):
            b = wi[0] % NWB; wi[0] += 1
            P.dma("sp", Wb[b][:, 0:nk, :], dr[dn][idx], reads=[("wc", dn, idx)], writes=[("C_W", b)])
            return b

        def norm_apply(srcbuf, srckey, gcol, dstfn):
            self.rmsnorm_stats(srcbuf, srckey, ag, rstd, ntmp)

        for j in range(NT):
            t0 = j * TS
            P.dma("sp", oTt[:], dr["oT"][:, :, t0:t0 + TS].rearrange("c p t -> p c t"),
                  reads=[("oT", c, j) for c in range(24)], writes=[("C_oT",)])
            for cg in range(4):
                bs = []
                for br, wn in enumerate(("w_up_fox", "w_up_nsa", "w_up_mem")):
                    bs.append(loadW("wcu", br * 4 + cg, 8))
                for cbl in range(4):
                    cb = cg * 4 + cbl
                    gb = []
                    for br in range(3):
                        k = gi[0] % NG; gi[0] += 1
                        P.dma("sp", gts[k][:], dr["bgT"][br * 16 + cb, :, t0:t0 + TS], reads=[("bgT", br * 16 + cb, j)], writes=[("C_g", k)])
                        gb.append(k)
                    pis = []
                    for br in range(3):
                        pi = self.nextps(); pis.append(pi)
                        for k in range(8):
                            P.op("pe", lambda e, pi=pi, br=br, k=k, cbl=cbl, b=bs[br]: e.matmul(
                                ps[pi][:, :], lhsT=Wb[b][:, k, cbl * 128:(cbl + 1) * 128], rhs=oTt[:, br * 8 + k, :],
                                start=(k == 0), stop=(k == 7)),
                                reads=[("C_W", bs[br]), ("C_oT",)], writes=[("ps", pi)])
                    for br in range(3):
                        P.op("dve", lambda e, br=br, pi=pis[br], k=gb[br]: e.tensor_tensor(out=tmp[br][:], in0=ps[pi][:, :], in1=gts[k][:], op=ALU.mult),
                             reads=[("ps", pis[br]), ("C_g", gb[br])], writes=[("C_t", br)])
                    P.op("pool", lambda e: e.tensor_tensor(out=tmp[0][:], in0=tmp[0][:], in1=tmp[1][:], op=ALU.add),
                         reads=[("C_t", 0), ("C_t", 1)], writes=[("C_t", 0)])
                    P.op("pool", lambda e, cb=cb: e.tensor_tensor(out=mrg[:, cb, :], in0=tmp[0][:], in1=tmp[2][:], op=ALU.add),
                         reads=[("C_t", 0), ("C_t", 2)], writes=[("C_mrg", cb)])
            MK = [("C_mrg", c) for c in range(NCH)]
            for cg in range(4):
                b = loadW("wcb", cg, NCH)
                for cbl in range(4):
                    cb = cg * 4 + cbl
                    pi = self.nextps()
                    for k in range(NCH):
                        P.op("pe", lambda e, pi=pi, k=k, cbl=cbl, b=b: e.matmul(
                            ps[pi][:, :], lhsT=Wb[b][:, k, cbl * 128:(cbl + 1) * 128], rhs=mrg[:, k, :],
                            start=(k == 0), stop=(k == NCH - 1)),
                            reads=[("C_W", b)] + MK, writes=[("ps", pi)])
                    P.op("act", lambda e, pi=pi, cb=cb: e.activation(out=y[:, cb, :], in_=ps[pi][:, :], func=AF.Copy),
                         reads=[("ps", pi)], writes=[("C_y", cb)])
            YK = [("C_y", c) for c in range(NCH)]
            P.dma("sp", h[:], dr[src_name][:, :, t0:t0 + TS].rearrange("c p t -> p c t"),
                  reads=[(src_name, j)], writes=[("C_h",)])
            P.op("act", lambda e: e.activation(out=ag[:], in_=y[:], func=AF.Square), reads=YK, writes=[("sq",)] + [("C_ag", c) for c in range(NCH)])
            self._stats_from_sq(ag, rstd, ntmp)
            for cb in range(NCH):
                tb = cb % 2
                P.op("dve", lambda e, cb=cb, tb=tb: e.scalar_tensor_tensor(
                    out=tmp[tb][:], in0=y[:, cb, :], scalar=vec[:, vb + VG_POST_MIX + cb:vb + VG_POST_MIX + cb + 1], in1=rstd[:],
                    op0=ALU.mult, op1=ALU.mult),
                    reads=[("C_y", cb), ("rstd",), ("c", "vecs")], writes=[("C_t", tb)])
                P.op("pool", lambda e, cb=cb, tb=tb: e.tensor_tensor(out=h[:, cb, :], in0=h[:, cb, :], in1=tmp[tb][:], op=ALU.add),
                     reads=[("C_h",), ("C_t", tb)], writes=[("C_h",)])
            P.op("act", lambda e: e.activation(out=ag[:], in_=h[:], func=AF.Square), reads=[("C_h",)], writes=[("sq",)] + [("C_ag", c) for c in range(NCH)])
            self._stats_from_sq(ag, rstd, ntmp)
            for cb in range(NCH):
                P.op("dve", lambda e, cb=cb: e.scalar_tensor_tensor(
                    out=mrg[:, cb, :], in0=h[:, cb, :], scalar=vec[:, vb + VG_PRE_MLP + cb:vb + VG_PRE_MLP + cb + 1], in1=rstd[:],
                    op0=ALU.mult, op1=ALU.mult),
                    reads=[("C_h",), ("rstd",), ("c", "vecs")], writes=[("C_mrg", cb)])
            for g in range(4):
                for wt_ in range(4):
                    b = loadW("wcb", 4 + g * 4 + wt_, NCH)
                    for cbl in range(4):
                        pi = self.nextps()
                        for k in range(NCH):
                            P.op("pe", lambda e, pi=pi, k=k, cbl=cbl, b=b: e.matmul(
                                ps[pi][:, :], lhsT=Wb[b][:, k, cbl * 128:(cbl + 1) * 128], rhs=mrg[:, k, :],
                                start=(k == 0), stop=(k == NCH - 1)),
                                reads=[("C_W", b)] + MK, writes=[("ps", pi)])
                        ri = (wt_ * 4 + cbl) % 2
                        P.op("act", lambda e, pi=pi, ri=ri: e.activation(out=rl[ri][:], in_=ps[pi][:, :], func=AF.Relu),
                             reads=[("ps", pi)], writes=[("C_rl", ri)])
                        ac = wt_ * 4 + cbl
                        P.op("pool", lambda e, ri=ri, ac=ac: e.tensor_tensor(out=ag[:, ac, :], in0=rl[ri][:], in1=rl[ri][:], op=ALU.mult),
                             reads=[("C_rl", ri)], writes=[("C_ag", ac)])
                AK = [("C_ag", c) for c in range(NCH)]
                for cg in range(4):
                    b = loadW("wcb", 20 + g * 4 + cg, NCH)
                    for cbl in range(4):
                        cb = cg * 4 + cbl
                        pi = self.nextps()
                        for k in range(NCH):
                            P.op("pe", lambda e, pi=pi, k=k, cbl=cbl, b=b: e.matmul(
                                ps[pi][:, :], lhsT=Wb[b][:, k, cbl * 128:(cbl + 1) * 128], rhs=ag[:, k, :],
                                start=(k == 0), stop=(k == NCH - 1)),
                                reads=[("C_W", b)] + AK, writes=[("ps", pi)])
                        if g == 0:
                            P.op("act", lambda e, pi=pi, cb=cb: e.activation(out=y[:, cb, :], in_=ps[pi][:, :], func=AF.Copy),
                                 reads=[("ps", pi)], writes=[("C_y", cb)])
                        else:
                            P.op("dve", lambda e, pi=pi, cb=cb: e.tensor_tensor(out=y[:, cb, :], in0=ps[pi][:, :], in1=y[:, cb, :], op=ALU.add),
                                 reads=[("ps", pi), ("C_y", cb)], writes=[("C_y", cb)])
            P.op("act", lambda e: e.activation(out=ag[:], in_=y[:], func=AF.Square), reads=YK, writes=[("sq",)] + AK)
            self._stats_from_sq(ag, rstd, ntmp)
            for cb in range(NCH):
                tb = cb % 2
                P.op("dve", lambda e, cb=cb, tb=tb: e.scalar_tensor_tensor(
                    out=tmp[tb][:], in0=y[:, cb, :], scalar=vec[:, vb + VG_POST_MLP + cb:vb + VG_POST_MLP + cb + 1], in1=rstd[:],
                    op0=ALU.mult, op1=ALU.mult),
                    reads=[("C_y", cb), ("rstd",), ("c", "vecs")], writes=[("C_t", tb)])
                P.op("pool", lambda e, cb=cb, tb=tb: e.tensor_tensor(out=h[:, cb, :], in0=h[:, cb, :], in1=tmp[tb][:], op=ALU.add),
                     reads=[("C_h",), ("C_t", tb)], writes=[("C_h",)])
            d = P.dma("act", dr[dst_name][:, :, t0:t0 + TS].rearrange("c p t -> p c t"), h[:],
                      reads=[("C_h",)], writes=[(dst_name, j)])
            if dst_name == "outT":
                self.final.append(d)
    P.barrier()


def _stats_from_sq(self, sq, rstd, tmp, nfree=512):
    P = self.P
    pi = self.nextps()
    ps = self.ps[pi]
    for c in range(NCH):
        P.op("pe", lambda e, c=c: e.matmul(ps[:, 0:nfree], lhsT=self.sb["ones"][:], rhs=sq[:, c, 0:nfree],
                                           start=(c == 0), stop=(c == NCH - 1)),
             reads=[("sq",), ("c", "ones")], writes=[("ps", pi)])
    P.op("dve", lambda e: e.tensor_scalar(out=tmp[:, 0:nfree], in0=ps[:, 0:nfree], scalar1=1.0 / D, scalar2=EPS,
                                          op0=ALU.mult, op1=ALU.add),
         reads=[("ps", pi)], writes=[("nt",)])
    P.op("act", lambda e: e.activation(out=tmp[:, 0:nfree], in_=tmp[:, 0:nfree], func=AF.Sqrt),
         reads=[("nt",)], writes=[("nt",)])
    P.op("dve", lambda e: e.reciprocal(out=rstd[:, 0:nfree], in_=tmp[:, 0:nfree]),
         reads=[("nt",)], writes=[("rstd",)])


Builder.phase_C = _phase_C
Builder._stats_from_sq = _stats_from_sq


from concourse.bass_utils import run_bass_kernel_spmd

N_ACTIVE = 4


def build_fused():
    B = Builder()
    src = "xT"
    for l in range(NL):
        B.phase_A(l, src)
        B.phase_B(l)
        dst = "outT" if l == NL - 1 else "hT"
        B.phase_C(l, src, dst)
        src = dst
    B.P.emit(final_waits=B.final)
    return B


def kernel(**inputs):
    inp = {k: np.asarray(v) for k, v in inputs.items()}
    consts = host_consts()
    consts["vecs"] = host_vecs(inp)
    B = build_fused()
    in_maps = []
    for b in range(N_ACTIVE):
        m = {"xT": np.ascontiguousarray(inp["x"][b].T.reshape(NCH, 128, T)).astype(np.float32),
             "memT": np.ascontiguousarray(inp["mem"][b].T.reshape(NCH, 128, 256)).astype(np.float32)}
        m.update(consts)
        for k in WEIGHT_SPECS:
            m[k] = np.ascontiguousarray(inp[k], dtype=np.float32)
        in_maps.append(m)
    res = run_bass_kernel_spmd(B.nc, in_maps, core_ids=list(range(N_ACTIVE)))
    out = np.empty((N_ACTIVE, T, D), np.float32)
    for b in range(N_ACTIVE):
        oT = np.asarray(res.results[b]["outT"], dtype=np.float32)
        out[b] = oT.reshape(D, T).T
    return out
```
